# Optimizing a Trainium2 kernel written in Bass

```python
import jax, jax.numpy as jnp
from jax import lax
import numpy as np

D_MODEL = 2048
BATCH = 1
SEQ = 16384
DEPTH = 1

CHUNK = 64
N_LEFT_CHUNKS = 8
BAND = (N_LEFT_CHUNKS + 1) * CHUNK
CONV_CH = D_MODEL
CONV_WIDTH = 31
N_HEADS = 16
HEAD_DIM = 128
ATTN_W = N_HEADS * HEAD_DIM
MAX_REL = 128
N_REL = (CHUNK - 1) + MAX_REL + 1
PEER_HEADS = 8
PEER_KEYS = 128
N_EXPERTS = PEER_KEYS * PEER_KEYS
PEER_QDIM = 256
PEER_HALF = PEER_QDIM // 2
PEER_TOPK = 16
TOKEN_BLOCK = 128
EPS = 1e-6
IN_COLS = 2 * CONV_CH + 3 * ATTN_W + 2 * D_MODEL

kernel_name = "hybrid_conv_chunkattn_peer_block"


def rms_norm(x, g):
    xf = x.astype(jnp.float32)
    y = xf * lax.rsqrt(jnp.mean(xf * xf, axis=-1, keepdims=True) + EPS)
    return (y * g.astype(jnp.float32)).astype(x.dtype)


def layer_norm(x, g, b):
    xf = x.astype(jnp.float32)
    mu = jnp.mean(xf, axis=-1, keepdims=True)
    var = jnp.mean(jnp.square(xf - mu), axis=-1, keepdims=True)
    y = (xf - mu) * lax.rsqrt(var + EPS)
    return (y * g.astype(jnp.float32) + b.astype(jnp.float32)).astype(x.dtype)


def causal_depthwise_conv(x, w, b):
    y = lax.conv_general_dilated(
        x, w[:, None, :], window_strides=(1,), padding=[(CONV_WIDTH - 1, 0)],
        dimension_numbers=('NWC', 'WIO', 'NWC'), feature_group_count=x.shape[-1])
    return y + b


def conformer_conv_branch(a_val, a_gate, dw_w, dw_b, ln_g, ln_b, w_pw2):
    u = a_val * jax.nn.sigmoid(a_gate)
    u = causal_depthwise_conv(u, dw_w, dw_b)
    u = layer_norm(u, ln_g, ln_b)
    u = jax.nn.silu(u)
    return u @ w_pw2


def chunk_band_attention(q, k, v, rel_bias):
    B, S = q.shape[0], q.shape[1]
    n_chunks = S // CHUNK
    left = BAND - CHUNK
    k_pad = jnp.pad(k, ((0, 0), (left, 0), (0, 0), (0, 0)))
    v_pad = jnp.pad(v, ((0, 0), (left, 0), (0, 0), (0, 0)))
    qi = np.arange(CHUNK)[:, None]
    kj = np.arange(BAND)[None, :]
    rel_idx = np.clip(qi - kj + left, -(CHUNK - 1), MAX_REL) + (CHUNK - 1)
    bias = rel_bias[:, rel_idx].astype(jnp.float32)
    scale = HEAD_DIM ** -0.5

    def one_chunk(c):
        start = c * CHUNK
        qc = lax.dynamic_slice_in_dim(q, start, CHUNK, axis=1)
        kc = lax.dynamic_slice_in_dim(k_pad, start, BAND, axis=1)
        vc = lax.dynamic_slice_in_dim(v_pad, start, BAND, axis=1)
        s = jnp.einsum('bqhd,bkhd->bhqk', qc, kc).astype(jnp.float32) * scale + bias[None]
        kpos = start - left + jnp.arange(BAND, dtype=jnp.int32)
        s = jnp.where((kpos >= 0)[None, None, None, :], s, -1e30)
        p = jax.nn.softmax(s, axis=-1).astype(vc.dtype)
        return jnp.einsum('bhqk,bkhd->bqhd', p, vc)

    out = lax.map(one_chunk, jnp.arange(n_chunks, dtype=jnp.int32))
    return jnp.moveaxis(out, 0, 1).reshape(B, S, ATTN_W)


def peer_route(q, k1, k2):
    T = q.shape[0]
    s1 = jnp.einsum('thd,nd->thn', q[..., :PEER_HALF], k1).astype(jnp.float32)
    s2 = jnp.einsum('thd,nd->thn', q[..., PEER_HALF:], k2).astype(jnp.float32)
    v1, i1 = lax.top_k(s1, PEER_TOPK)
    v2, i2 = lax.top_k(s2, PEER_TOPK)
    cand = (v1[..., :, None] + v2[..., None, :]).reshape(T, PEER_HEADS, PEER_TOPK * PEER_TOPK)
    top, flat = lax.top_k(cand, PEER_TOPK)
    e1 = jnp.take_along_axis(i1, flat // PEER_TOPK, axis=-1)
    e2 = jnp.take_along_axis(i2, flat % PEER_TOPK, axis=-1)
    idx = e1 * PEER_KEYS + e2
    gate = jax.nn.softmax(top, axis=-1)
    return idx, gate


def peer_ffn(h, w_query, k1, k2, expert_u, expert_v):
    B, S, D = h.shape
    T = B * S
    ht = h.reshape(T, D)
    q = (ht @ w_query).reshape(T, PEER_HEADS, PEER_QDIM)
    idx, gate = peer_route(q, k1, k2)
    nb = T // TOKEN_BLOCK

    def block(args):
        hb, ib, gb = args
        u = expert_u[ib]
        a = jnp.einsum('td,thkd->thk', hb, u)
        w = gb.astype(hb.dtype) * jax.nn.gelu(a, approximate=False)
        v = expert_v[ib]
        return jnp.einsum('thk,thkd->td', w, v)

    y = lax.map(block, (ht.reshape(nb, TOKEN_BLOCK, D),
                        idx.reshape(nb, TOKEN_BLOCK, PEER_HEADS, PEER_TOPK),
                        gate.reshape(nb, TOKEN_BLOCK, PEER_HEADS, PEER_TOPK)))
    return y.reshape(B, S, D)


def setup_inputs(seed: int = 0) -> dict:
    key = jax.random.key(seed)
    ks = jax.random.split(key, 20)
    L, D, C, A = DEPTH, D_MODEL, CONV_CH, ATTN_W
    nrm = lambda k, shape, s: jax.random.normal(k, shape, jnp.float32) * s
    return {
        "x": nrm(ks[0], (BATCH, SEQ, D), 1.0),
        "norm1_g": 1.0 + nrm(ks[1], (L, D), 0.02),
        "w_in": nrm(ks[2], (L, D, IN_COLS), D ** -0.5),
        "conv_dw_w": nrm(ks[3], (L, CONV_WIDTH, C), CONV_WIDTH ** -0.5),
        "conv_dw_b": nrm(ks[4], (L, C), 0.02),
        "conv_ln_g": 1.0 + nrm(ks[5], (L, C), 0.02),
        "conv_ln_b": nrm(ks[6], (L, C), 0.02),
        "w_conv_out": nrm(ks[7], (L, C, D), C ** -0.5),
        "q_norm_g": 1.0 + nrm(ks[8], (L, HEAD_DIM), 0.02),
        "k_norm_g": 1.0 + nrm(ks[9], (L, HEAD_DIM), 0.02),
        "rel_bias": nrm(ks[10], (L, N_HEADS, N_REL), 0.1),
        "w_attn_o": nrm(ks[11], (L, A, D), A ** -0.5),
        "w_out": nrm(ks[12], (L, D, D), D ** -0.5),
        "norm2_g": 1.0 + nrm(ks[13], (L, D), 0.02),
        "w_query": nrm(ks[14], (L, D, PEER_HEADS * PEER_QDIM), D ** -0.5),
        "sub_keys_1": nrm(ks[15], (L, PEER_KEYS, PEER_HALF), PEER_HALF ** -0.5),
        "sub_keys_2": nrm(ks[16], (L, PEER_KEYS, PEER_HALF), PEER_HALF ** -0.5),
        "expert_u": nrm(ks[17], (L, N_EXPERTS, D), D ** -0.5),
        "expert_v": nrm(ks[18], (L, N_EXPERTS, D), PEER_HEADS ** -0.5),
    }


def reference(x, norm1_g, w_in, conv_dw_w, conv_dw_b, conv_ln_g, conv_ln_b, w_conv_out,
              q_norm_g, k_norm_g, rel_bias, w_attn_o, w_out, norm2_g, w_query,
              sub_keys_1, sub_keys_2, expert_u, expert_v):
    B, S, D = x.shape
    splits = np.cumsum([CONV_CH, CONV_CH, ATTN_W, ATTN_W, ATTN_W, D_MODEL]).tolist()
    for l in range(DEPTH):
        h = rms_norm(x, norm1_g[l])
        proj = h @ w_in[l]
        a_val, a_gate, q, k, v, g_conv, g_attn = jnp.split(proj, splits, axis=-1)
        conv_out = conformer_conv_branch(a_val, a_gate, conv_dw_w[l], conv_dw_b[l],
                                         conv_ln_g[l], conv_ln_b[l], w_conv_out[l])
        q = rms_norm(q.reshape(B, S, N_HEADS, HEAD_DIM), q_norm_g[l])
        k = rms_norm(k.reshape(B, S, N_HEADS, HEAD_DIM), k_norm_g[l])
        v = v.reshape(B, S, N_HEADS, HEAD_DIM)
        attn_out = chunk_band_attention(q, k, v, rel_bias[l]) @ w_attn_o[l]
        merged = jax.nn.sigmoid(g_conv) * conv_out + jax.nn.sigmoid(g_attn) * attn_out
        x = x + merged @ w_out[l]
        h2 = rms_norm(x, norm2_g[l])
        x = x + peer_ffn(h2, w_query[l], sub_keys_1[l], sub_keys_2[l], expert_u[l], expert_v[l])
    return x
```

```python
import numpy as np
from contextlib import ExitStack
import concourse.bass as bass
import concourse.mybir as mybir
from concourse.bass_utils import run_bass_kernel_spmd

F32 = mybir.dt.float32
BF16 = mybir.dt.bfloat16
U32 = mybir.dt.uint32
ALU = mybir.AluOpType
AF = mybir.ActivationFunctionType
AX = mybir.AxisListType

D = 2048
NCORE = 8
TOK_CORE = 2048
TT = 256
HALO_T = 2
NT_OWN = TOK_CORE // TT
EPS = 1e-6
NEXP_CH = 128
EG = 32
CG = 4


class Sched:
    ENG = ['pe', 'act', 'dve', 'pool', 'sp']
    EPOCH = 12000

    def __init__(self, nc, stack):
        self.nc = nc
        self.stack = stack
        self.streams = {e: [] for e in self.ENG}
        self.sem = {}
        self.cnt = {}
        self.nsem = 0
        for e in self.ENG:
            self._new_epoch(e)
        self.known = {e: {} for e in self.ENG}
        self.wr = {}
        self.rd = {}
        self.dma_sems = {}

    def _new_sem(self, name):
        self.nsem += 1
        return self.stack.enter_context(self.nc.semaphore(f"{name}_{self.nsem}"))

    def _new_epoch(self, e):
        self.sem[e] = self._new_sem(f"s_{e}")
        self.cnt[e] = 0

    def _deps(self, eng, reads, writes):
        deps = {}

        def need(ev, same_ok):
            sem, val, src = ev
            k = id(sem)
            if k not in deps or deps[k][1] < val:
                deps[k] = (sem, val)

        for (a, lo, hi) in reads:
            for (l2, h2, ev) in self.wr.get(a, ()):
                if l2 < hi and lo < h2:
                    need(ev, False)
        for (a, lo, hi) in writes:
            for (l2, h2, ev) in self.wr.get(a, ()):
                if l2 < hi and lo < h2:
                    need(ev, True)
            for (l2, h2, ev) in self.rd.get(a, ()):
                if l2 < hi and lo < h2:
                    need(ev, True)
        waits = []
        kn = self.known[eng]
        for k, (sem, val) in deps.items():
            if kn.get(k, 0) < val:
                kn[k] = val
                waits.append((sem, val))
        return waits

    def _commit(self, ev, reads, writes):
        for (a, lo, hi) in writes:
            self.wr[a] = [t for t in self.wr.get(a, ()) if not (t[0] >= lo and t[1] <= hi)]
            self.rd[a] = [t for t in self.rd.get(a, ()) if not (t[0] >= lo and t[1] <= hi)]
            self.wr[a].append((lo, hi, ev))
        for (a, lo, hi) in reads:
            lst = self.rd.setdefault(a, [])
            for i, t in enumerate(lst):
                if t[0] == lo and t[1] == hi and t[2][0] is ev[0]:
                    lst[i] = (lo, hi, ev)
                    break
            else:
                lst.append((lo, hi, ev))

    def op(self, eng, fn, reads=(), writes=()):
        reads = list(reads)
        writes = list(writes)
        waits = self._deps(eng, reads, writes)
        if self.cnt[eng] >= self.EPOCH:
            self._new_epoch(eng)
        self.cnt[eng] += 1
        mysem = self.sem[eng]
        ev = (mysem, self.cnt[eng], eng)

        def emit(e):
            for sem, val in waits:
                e.wait_ge(sem, val)
            ins = fn(e)
            ins.then_inc(mysem, 1)

        self.streams[eng].append(emit)
        self._commit(ev, reads, writes)
        return ev

    def dma(self, queue, out, in_, reads=(), writes=(), key=None):
        reads = list(reads)
        writes = list(writes)
        waits = self._deps(queue, reads, writes)
        if key not in self.dma_sems:
            self.dma_sems[key] = [self._new_sem("d"), 0]
        ent = self.dma_sems[key]
        ent[1] += 16
        sem = ent[0]
        ev = (sem, ent[1], None)

        def emit(e):
            for s, val in waits:
                e.wait_ge(s, val)
            e.dma_start(out=out, in_=in_).then_inc(sem, 16)

        self.streams[queue].append(emit)
        self._commit(ev, reads, writes)
        return ev

    def finish(self, eng='sp'):
        waits = [(ent[0], ent[1]) for ent in self.dma_sems.values()]
        for e in self.ENG:
            if e != eng and self.cnt[e] > 0:
                waits.append((self.sem[e], self.cnt[e]))

        def emit(e):
            for s, val in waits:
                e.wait_ge(s, val)

        self.streams[eng].append(emit)

    def emit_all(self):
        with self.nc.Block() as block:
            @block.tensor
            def _(e):
                for f in self.streams['pe']:
                    f(e)

            @block.scalar
            def _(e):
                for f in self.streams['act']:
                    f(e)

            @block.vector
            def _(e):
                for f in self.streams['dve']:
                    f(e)

            @block.gpsimd
            def _(e):
                for f in self.streams['pool']:
                    f(e)

            @block.sync
            def _(e):
                for f in self.streams['sp']:
                    f(e)


class Buf:
    def __init__(self, arena_name, ap, off, dt_bytes):
        self.an = arena_name
        self.ap = ap
        self.off = off
        self.eb = dt_bytes
        sh = ap.shape[1:]
        self.n = int(np.prod(sh))
        self.inner = int(np.prod(sh[1:])) if len(sh) > 1 else 1

    def k(self, i=None, j=None):
        if i is None:
            return (self.an, self.off, self.off + self.n * self.eb)
        if j is None:
            j = i + 1
        return (self.an, self.off + i * self.inner * self.eb, self.off + j * self.inner * self.eb)

    def ke(self, lo, hi):
        return (self.an, self.off + lo * self.eb, self.off + hi * self.eb)


def build_nc(nt_own=NT_OWN, debug=False):
    nc = bass.Bass("TRN2", target_bir_lowering=False)
    NTILE = HALO_T + nt_own
    NTOK = NTILE * TT
    xT = nc.dram_tensor("xT", [D, (HALO_T + NT_OWN) * TT], F32, kind="ExternalInput").ap()
    w_in = nc.dram_tensor("w_in", [D, 14336], F32, kind="ExternalInput").ap()
    w_co = nc.dram_tensor("w_co", [D, D], F32, kind="ExternalInput").ap()
    w_ao = nc.dram_tensor("w_ao", [D, D], F32, kind="ExternalInput").ap()
    w_out = nc.dram_tensor("w_out", [D, D], F32, kind="ExternalInput").ap()
    w_q = nc.dram_tensor("w_q", [D, D], F32, kind="ExternalInput").ap()
    uT = nc.dram_tensor("uT", [D, 16384], F32, kind="ExternalInput").ap()
    ev_d = nc.dram_tensor("ev", [16384, D], F32, kind="ExternalInput").ap()
    vecs_d = nc.dram_tensor("vecs", [128, 578], F32, kind="ExternalInput").ap()
    tb_d = nc.dram_tensor("tb", [128, 16, 640], F32, kind="ExternalInput").ap()
    mb_d = nc.dram_tensor("mb", [128, 20], F32, kind="ExternalInput").ap()
    k12_d = nc.dram_tensor("k12", [128, 256], F32, kind="ExternalInput").ap()
    yT = nc.dram_tensor("yT", [D, NT_OWN * TT], F32, kind="ExternalOutput").ap()
    NBLK = 28 + 16 + 64
    wsc = nc.dram_tensor("wsc", [NBLK, 128, 8192], BF16, kind="Internal").ap()
    if debug:
        dbg_mid = nc.dram_tensor("dbg_mid", [D, TT], F32, kind="ExternalOutput").ap()

    with ExitStack() as st:
        S = Sched(nc, st)

        def sb(name, shape, dt):
            t = st.enter_context(nc.sbuf_tensor("s_" + name, shape, dt))
            eb = 4 if dt in (F32, U32) else 2
            return Buf(name, t[:], 0, eb)

        xt = sb("xt", [128, 16, TT], F32)
        hT = sb("hT", [128, 16, TT], BF16)
        knT = sb("knT", [128, 16, 3, TT], BF16)
        vtok = sb("vtok", [128, 3, 2, D], BF16)
        Etab2 = [sb(f"Etab{i}", [128, 640], BF16) for i in range(2)]
        NWB = 3
        wbt = [sb(f"wb{i}", [128, 8192], BF16) for i in range(NWB)]
        ubuf = sb("ubuf", [128, 16, 30 + TT], BF16)
        vecs = sb("vecs", [128, 578], F32)
        mb = sb("mb", [128, 20], F32)
        k12 = sb("k12", [128, 256], BF16)
        ones_f = sb("ones_f", [128, 128], F32)
        ones_b = sb("ones_b", [128, 128], BF16)
        ident = sb("ident", [128, 128], F32)
        iota128 = sb("iota128", [128, 128], F32)
        iota16 = sb("iota16", [128, 16], F32)
        g1s = sb("g1s", [128, 16], F32)
        g2s = sb("g2s", [128, 16], F32)

        SCR_BYTES = 60 * 1024
        scr_t = st.enter_context(nc.sbuf_tensor("scr", [128, SCR_BYTES // 2], BF16))

        def scr(off, shape, dt):
            eb = 4 if dt in (F32, U32) else 2
            n = int(np.prod(shape))
            assert off % 4 == 0 and off + n * eb <= SCR_BYTES, (off, shape)
            ap = scr_t[:, off // 2: off // 2 + n * eb // 2]
            if dt != BF16:
                ap = ap.bitcast(dt)
            if len(shape) == 2:
                ap = ap.rearrange("p (a b) -> p a b", a=shape[0])
            elif len(shape) == 3:
                ap = ap.rearrange("p (a b c) -> p a b c", a=shape[0], b=shape[1])
            return Buf("scr", ap, off, eb)

        K = 1024
        ybuf = scr(0, [16, TT], F32)
        qnT = scr(0, [16, TT], BF16)
        attnT = scr(8 * K, [16, TT], BF16)
        su = scr(16 * K, [16, TT], BF16)
        merged = scr(16 * K, [16, TT], BF16)
        mc = scr(24 * K, [16, TT], BF16)
        PT = [scr(32 * K + i * K, [TT], F32) for i in range(2)]
        PT2 = [scr(34 * K + i * 3 * K, [6, TT], BF16) for i in range(2)]
        tmpf = [scr(40 * K + i * K, [TT], F32) for i in range(6)]
        sqt = [scr(46 * K + i * 4 * K, [4, TT], F32) for i in range(2)]
        etmp = [scr(54 * K + i * 2560, [640], F32) for i in range(2)]
        pqT = scr(0, [16, TT], BF16)
        sc = scr(8 * K, [16, 128], F32)
        Gsub = scr(16 * K, [EG, TT], BF16)
        Gsub2 = scr(0, [EG, TT], BF16)
        cand = scr(32 * K, [8, 256], F32)
        wk = [scr(40 * K + i * K, [256], F32) for i in range(2)]
        gel = [scr(42 * K + i * K, [TT], F32) for i in range(2)]
        v1 = scr(44 * K, [16, 16], F32)
        i1u = scr(45 * K, [16, 16], U32)
        i1f = scr(46 * K, [16, 16], F32)
        topv = scr(47 * K, [8, 16], F32)
        flatu = scr(47 * K + 512, [8, 16], U32)
        af = scr(48 * K, [8, 16], F32)
        bf_ = scr(48 * K + 512, [8, 16], F32)
        oh4 = scr(49 * K, [8, 16, 16], F32)
        e1f = scr(57 * K, [8, 16], F32)
        e2f = scr(57 * K + 512, [8, 16], F32)
        gat = scr(58 * K, [8, 16], F32)
        gsum = scr(58 * K + 512, [8], F32)
        au = scr(58 * K + 512 + 64, [8, 16], U32)
        bu = scr(59 * K + 64, [8, 16], U32)
        Ism = sb("Ism", [128, TT], BF16)
        Jsm = sb("Jsm", [128, TT], BF16)
        iota128b = sb("iota128b", [128, 128], BF16)
        gsm = sb("gsm", [128, TT], F32)
        OJb = [scr(32 * K + i * 4 * K, [16, 128], BF16) for i in range(2)]
        Cb = [scr(44 * K + i * K, [16, 32], BF16) for i in range(2)]
        Cf = [scr(46 * K + i * 2 * K, [16, 32], BF16) for i in range(2)]
        wTb = [scr(50 * K + i * 2 * K, [CG, TT], BF16) for i in range(2)]

        pst = [st.enter_context(nc.psum_tensor(f"ps{i}", [128, 512], F32)) for i in range(8)]

        class PBuf(Buf):
            def __init__(self, bank, ap):
                Buf.__init__(self, "ps", ap, bank * 2048, 4)
                self.bank = bank

            def k(self, i=None, j=None):
                return ("ps", self.bank * 2048, (self.bank + 1) * 2048)

        def psh(bank, half):
            return PBuf(bank, pst[bank][:, half * 256:half * 256 + 256])

        def psf(b):
            return PBuf(b, pst[b][:, :])

        class Rot:
            def __init__(self, items):
                self.items = items
                self.i = 0

            def next(self):
                r = self.items[self.i % len(self.items)]
                self.i += 1
                return r

        pj = Rot([psh(0, 0), psh(1, 0), psh(2, 0)])
        pstat = Rot([psh(3, 0), psh(3, 1)])
        pscore = Rot([psh(4, 0), psh(5, 0)])
        ppv = psh(6, 0)
        pden = psh(6, 1)
        pfull = Rot([psf(6), psf(7)])
        pfull2 = psf(5)
        ppy = Rot([psh(3, 0), psh(4, 0)])
        wrot = Rot(list(range(NWB)))

        V = lambda c0, n=1: vecs.ap[:, c0:c0 + n]
        N1G, N2G, DWB, LNG, LNB, QG, KG, DWW = 0, 16, 32, 48, 64, 80, 81, 82

        S.dma('sp', vecs.ap, vecs_d, writes=[vecs.k()], key='c0')
        S.dma('sp', mb.ap, mb_d, writes=[mb.k()], key='c1')
        S.dma('pool', k12.ap, k12_d, writes=[k12.k()], key='c2')
        S.op('dve', lambda e: e.memset(ones_f.ap, 1.0), writes=[ones_f.k()])
        S.op('dve', lambda e: e.memset(ones_b.ap, 1.0), writes=[ones_b.k()])
        S.op('dve', lambda e: e.memset(ubuf.ap, 0.0), writes=[ubuf.k()])
        S.op('pool', lambda e: e.iota(ident.ap, pattern=[[1, 128]], base=0, channel_multiplier=-1,
                                      allow_small_or_imprecise_dtypes=True), writes=[ident.k()])
        S.op('dve', lambda e: e.tensor_scalar(out=ident.ap, in0=ident.ap, scalar1=0.0, scalar2=None,
                                              op0=ALU.is_equal), reads=[ident.k()], writes=[ident.k()])
        S.op('pool', lambda e: e.iota(iota128.ap, pattern=[[1, 128]], base=0, channel_multiplier=0,
                                      allow_small_or_imprecise_dtypes=True), writes=[iota128.k()])
        S.op('dve', lambda e: e.tensor_copy(out=iota128b.ap, in_=iota128.ap), reads=[iota128.k()], writes=[iota128b.k()])
        S.op('pool', lambda e: e.iota(iota16.ap, pattern=[[1, 16]], base=0, channel_multiplier=0,
                                      allow_small_or_imprecise_dtypes=True), writes=[iota16.k()])
        S.op('dve', lambda e: e.tensor_scalar(out=g1s.ap, in0=V(N1G, 16), scalar1=float(np.sqrt(D)), scalar2=None,
                                              op0=ALU.mult), reads=[vecs.k()], writes=[g1s.k()])
        S.op('dve', lambda e: e.tensor_scalar(out=g2s.ap, in0=V(N2G, 16), scalar1=float(np.sqrt(D)), scalar2=None,
                                              op0=ALU.mult), reads=[vecs.k()], writes=[g2s.k()])
        blk_id = {}
        NCVSEM = 56

        def conv_blk(name, src_ap, j, pattern):
            b = len(blk_id)
            blk_id[(name, j)] = b
            c = 16 if pattern == 'kc' else 4
            S.dma('pool', wsc[b].rearrange("p (c n) -> p c n", c=c), src_ap.rearrange("(c p) n -> p c n", p=128),
                  writes=[('wsc', b, b + 1), ('cvslot', b % NCVSEM, b % NCVSEM + 1)], key=('cv', b % NCVSEM))

        def conv_in(j0):
            for j in range(4):
                conv_blk('w_in', w_in[:, j0 + 512 * j:j0 + 512 * (j + 1)], (j0 // 512) + j, 'kc')

        conv_in(6144)
        conv_in(8192)
        for j in range(4):
            conv_blk('w_in', w_in[:, 512 * j:512 * (j + 1)], j, 'kc')
            conv_blk('w_in', w_in[:, 2048 + 512 * j:2048 + 512 * (j + 1)], 4 + j, 'kc')
        for j in range(4):
            conv_blk('w_co', w_co[:, 512 * j:512 * (j + 1)], j, 'kc')
            conv_blk('w_in', w_in[:, 10240 + 512 * j:10240 + 512 * (j + 1)], 20 + j, 'kc')
        conv_in(4096)
        for j in range(4):
            conv_blk('w_ao', w_ao[:, 512 * j:512 * (j + 1)], j, 'kc')
            conv_blk('w_in', w_in[:, 12288 + 512 * j:12288 + 512 * (j + 1)], 24 + j, 'kc')
        for j in range(4):
            conv_blk('w_out', w_out[:, 512 * j:512 * (j + 1)], j, 'kc')
        for j in range(4):
            conv_blk('w_q', w_q[:, 512 * j:512 * (j + 1)], j, 'kc')
        for j in range(NEXP_CH // CG):
            conv_blk('uT', uT[:, 512 * j:512 * (j + 1)], j, 'kc')
            conv_blk('ev', ev_d[512 * j:512 * (j + 1), :], j, 'ec')
        assert len(blk_id) == NBLK

        def load_w(name, j, rows_pattern):
            wi = wrot.next()
            wb = wbt[wi]
            b = blk_id[(name, j)]
            c = 16 if rows_pattern == 'kc' else 4
            view = Buf(wb.an, wb.ap.rearrange("p (c n) -> p c n", c=c), 0, 2)
            S.dma('sp', wb.ap, wsc[b], reads=[('wsc', b, b + 1)], writes=[wb.k()], key=('wb', wi))
            return view

        def proj(wv, sub, rhsbuf, ps):
            def f(e):
                for c in range(16):
                    ins = e.matmul(ps.ap, lhsT=wv.ap[:, c, sub * 128:(sub + 1) * 128], rhs=rhsbuf.ap[:, c, :],
                                   start=(c == 0), stop=(c == 15))
                return ins
            S.op('pe', f, reads=[wv.k(), rhsbuf.k()], writes=[ps.k()])

        def rsqrt_chain(dst, src_ap, src_key, addc):
            S.op('dve', lambda e: e.tensor_scalar(out=dst.ap, in0=src_ap, scalar1=float(addc), scalar2=None, op0=ALU.add),
                 reads=[src_key], writes=[dst.k()])
            S.op('act', lambda e: e.activation(out=dst.ap, in_=dst.ap, func=AF.Sqrt), reads=[dst.k()], writes=[dst.k()])
            S.op('dve', lambda e: e.reciprocal(out=dst.ap, in_=dst.ap), reads=[dst.k()], writes=[dst.k()])

        def rmsnorm_to_hT(gs):
            pss = pstat.next()
            for q4 in range(4):
                sq = sqt[q4 % 2]
                S.op('act', lambda e, q4=q4, sq=sq: e.activation(out=sq.ap, in_=xt.ap[:, 4 * q4:4 * q4 + 4, :], func=AF.Square),
                     reads=[xt.k(4 * q4, 4 * q4 + 4)], writes=[sq.k()])

                def f(e, q4=q4, sq=sq):
                    for j in range(4):
                        ins = e.matmul(pss.ap, lhsT=ones_f.ap, rhs=sq.ap[:, j, :], start=(q4 == 0 and j == 0),
                                       stop=(q4 == 3 and j == 3))
                    return ins
                S.op('pe', f, reads=[sq.k(), ones_f.k()], writes=[pss.k()])
            rs = tmpf[0]
            rsqrt_chain(rs, pss.ap, pss.k(), D * EPS)
            for c in range(16):
                S.op('dve', lambda e, c=c: e.scalar_tensor_tensor(out=hT.ap[:, c, :], in0=xt.ap[:, c, :], scalar=gs.ap[:, c:c + 1],
                                                                 in1=rs.ap, op0=ALU.mult, op1=ALU.mult),
                     reads=[xt.k(c), rs.k(), gs.k()], writes=[hT.k(c)])

        def qk_norm(ps, gcol, out_ap, out_key):
            sq = tmpf[1]
            S.op('act', lambda e: e.activation(out=sq.ap, in_=ps.ap, func=AF.Square), reads=[ps.k()], writes=[sq.k()])
            pss = pstat.next()
            S.op('pe', lambda e: e.matmul(pss.ap, lhsT=ones_f.ap, rhs=sq.ap, start=True, stop=True),
                 reads=[sq.k(), ones_f.k()], writes=[pss.k()])
            rs = tmpf[2]
            rsqrt_chain(rs, pss.ap, pss.k(), 128 * EPS)
            S.op('dve', lambda e: e.scalar_tensor_tensor(out=out_ap, in0=ps.ap, scalar=V(gcol), in1=rs.ap,
                                                         op0=ALU.mult, op1=ALU.mult),
                 reads=[ps.k(), rs.k(), vecs.k()], writes=[out_key])

        def do_tile(ti):
            own = ti >= HALO_T
            slot = ti % 3
            S.dma('sp', xt.ap, xT[:, ti * TT:(ti + 1) * TT].rearrange("(c p) t -> p c t", p=128),
                  writes=[xt.k()], key='xt')
            rmsnorm_to_hT(g1s)

            if ti >= 1:
                for j in range(4):
                    wa = load_w('w_in', j, 'kc')
                    wg = load_w('w_in', 4 + j, 'kc')
                    for sub in range(4):
                        c = 4 * j + sub
                        pa = pj.next()
                        pg = pj.next()
                        proj(wa, sub, hT, pa)
                        proj(wg, sub, hT, pg)
                        sg = tmpf[3 + (c % 2)]
                        S.op('act', lambda e, sg=sg, pg=pg: e.activation(out=sg.ap, in_=pg.ap, func=AF.Sigmoid),
                             reads=[pg.k()], writes=[sg.k()])
                        S.op('dve', lambda e, c=c, sg=sg, pa=pa: e.tensor_tensor(out=ubuf.ap[:, c, 30:30 + TT], in0=pa.ap, in1=sg.ap, op=ALU.mult),
                             reads=[pa.k(), sg.k()], writes=[ubuf.k(c)])
            conv_q = []
            if own:
                for g4 in range(4):
                    cs = [4 * g4 + i for i in range(4)]
                    for c in cs:
                        conv_q.append(lambda c=c: S.op('dve', lambda e: e.tensor_scalar(
                            out=ybuf.ap[:, c, :], in0=ubuf.ap[:, c, 0:TT], scalar1=V(DWW + c * 31),
                            scalar2=V(DWB + c), op0=ALU.mult, op1=ALU.add),
                            reads=[ubuf.k(c), vecs.k()], writes=[ybuf.k(c)]))
                    for j in range(1, 31):
                        for c in cs:
                            conv_q.append(lambda c=c, j=j: S.op('dve', lambda e: e.scalar_tensor_tensor(
                                out=ybuf.ap[:, c, :], in0=ubuf.ap[:, c, j:j + TT],
                                scalar=V(DWW + c * 31 + j), in1=ybuf.ap[:, c, :], op0=ALU.mult, op1=ALU.add),
                                reads=[ubuf.k(c), vecs.k(), ybuf.k(c)], writes=[ybuf.k(c)]))
            conv_q.reverse()

            def conv_emit(n):
                for _ in range(n):
                    if conv_q:
                        conv_q.pop()()

            for j in range(4):
                wv = load_w('w_in', 12 + j, 'kc')
                for sub in range(4):
                    h = 4 * j + sub
                    ps = pj.next()
                    proj(wv, sub, hT, ps)
                    qk_norm(ps, KG, knT.ap[:, h, slot, :], knT.ke((h * 3 + slot) * TT, (h * 3 + slot + 1) * TT))
                    conv_emit(24)
            for j in range(4):
                wv = load_w('w_in', 16 + j, 'kc')
                for t2 in range(2):
                    pf = pfull.next()

                    def f(e, wv=wv, t2=t2, pf=pf):
                        for c in range(16):
                            ins = e.matmul(pf.ap, lhsT=hT.ap[:, c, t2 * 128:(t2 + 1) * 128], rhs=wv.ap[:, c, :],
                                           start=(c == 0), stop=(c == 15))
                        return ins
                    S.op('pe', f, reads=[wv.k(), hT.k()], writes=[pf.k()])
                    S.op('act', lambda e, t2=t2, j=j, pf=pf: e.activation(out=vtok.ap[:, slot, t2, 512 * j:512 * (j + 1)], in_=pf.ap, func=AF.Copy),
                         reads=[pf.k()], writes=[vtok.ke((slot * 2 + t2) * D + 512 * j, (slot * 2 + t2) * D + 512 * (j + 1))])
                    conv_emit(14)
            conv_emit(10000)
            if ti == 0:
                return
            if not own:
                S.op('pool', lambda e: e.tensor_copy(out=ubuf.ap[:, :, 0:30], in_=ubuf.ap[:, :, TT:TT + 30]),
                     reads=[ubuf.k()], writes=[ubuf.k()])
                return
            S.op('pool', lambda e: e.tensor_copy(out=ubuf.ap[:, :, 0:30], in_=ubuf.ap[:, :, TT:TT + 30]),
                 reads=[ubuf.k()], writes=[ubuf.k()])
            ps1 = pstat.next()
            ps2 = pstat.next()

            def f(e):
                for c in range(16):
                    ins = e.matmul(ps1.ap, lhsT=ones_f.ap, rhs=ybuf.ap[:, c, :], start=(c == 0), stop=(c == 15))
                return ins
            S.op('pe', f, reads=[ybuf.k(), ones_f.k()], writes=[ps1.k()])
            for q4 in range(4):
                sq = sqt[q4 % 2]
                S.op('act', lambda e, q4=q4, sq=sq: e.activation(out=sq.ap, in_=ybuf.ap[:, 4 * q4:4 * q4 + 4, :], func=AF.Square),
                     reads=[ybuf.k(4 * q4, 4 * q4 + 4)], writes=[sq.k()])

                def f(e, q4=q4, sq=sq):
                    for j in range(4):
                        ins = e.matmul(ps2.ap, lhsT=ones_f.ap, rhs=sq.ap[:, j, :], start=(q4 == 0 and j == 0),
                                       stop=(q4 == 3 and j == 3))
                    return ins
                S.op('pe', f, reads=[sq.k(), ones_f.k()], writes=[ps2.k()])
            mean, msq, rstd, nmr = tmpf[0], tmpf[1], tmpf[2], tmpf[3]
            S.op('dve', lambda e: e.tensor_scalar(out=mean.ap, in0=ps1.ap, scalar1=1.0 / D, scalar2=None, op0=ALU.mult),
                 reads=[ps1.k()], writes=[mean.k()])
            S.op('dve', lambda e: e.tensor_tensor(out=msq.ap, in0=mean.ap, in1=mean.ap, op=ALU.mult),
                 reads=[mean.k()], writes=[msq.k()])
            S.op('dve', lambda e: e.scalar_tensor_tensor(out=rstd.ap, in0=ps2.ap, scalar=1.0 / D, in1=msq.ap,
                                                         op0=ALU.mult, op1=ALU.subtract),
                 reads=[ps2.k(), msq.k()], writes=[rstd.k()])
            rsqrt_chain(rstd, rstd.ap, rstd.k(), EPS)
            S.op('dve', lambda e: e.scalar_tensor_tensor(out=nmr.ap, in0=mean.ap, scalar=-1.0, in1=rstd.ap,
                                                         op0=ALU.mult, op1=ALU.mult),
                 reads=[mean.k(), rstd.k()], writes=[nmr.k()])
            for c in range(16):
                S.op('pool', lambda e, c=c: e.tensor_tensor(out=ybuf.ap[:, c, :], in0=ybuf.ap[:, c, :], in1=rstd.ap, op=ALU.mult),
                     reads=[ybuf.k(c), rstd.k()], writes=[ybuf.k(c)])
            for c in range(16):
                S.op('pool', lambda e, c=c: e.tensor_tensor(out=ybuf.ap[:, c, :], in0=ybuf.ap[:, c, :], in1=nmr.ap, op=ALU.add),
                     reads=[ybuf.k(c), nmr.k()], writes=[ybuf.k(c)])
            for c in range(16):
                S.op('act', lambda e, c=c: e.activation(out=su.ap[:, c, :], in_=ybuf.ap[:, c, :], func=AF.Silu,
                                                        bias=V(LNB + c), scale=V(LNG + c)),
                     reads=[ybuf.k(c), vecs.k()], writes=[su.k(c)])
            for j in range(4):
                wc = load_w('w_co', j, 'kc')
                wg = load_w('w_in', 20 + j, 'kc')
                for sub in range(4):
                    dc = 4 * j + sub
                    pc = pj.next()
                    pg = pj.next()
                    proj(wc, sub, su, pc)
                    proj(wg, sub, hT, pg)
                    sg = tmpf[4 + (dc % 2)]
                    S.op('act', lambda e, sg=sg, pg=pg: e.activation(out=sg.ap, in_=pg.ap, func=AF.Sigmoid),
                         reads=[pg.k()], writes=[sg.k()])
                    S.op('dve', lambda e, dc=dc, sg=sg, pc=pc: e.tensor_tensor(out=mc.ap[:, dc, :], in0=pc.ap, in1=sg.ap, op=ALU.mult),
                         reads=[pc.k(), sg.k()], writes=[mc.k(dc)])
            for j in range(4):
                wv = load_w('w_in', 8 + j, 'kc')
                for sub in range(4):
                    h = 4 * j + sub
                    ps = pj.next()
                    proj(wv, sub, hT, ps)
                    qk_norm(ps, QG, qnT.ap[:, h, :], qnT.k(h))
            RQ = [(0, 128, 512), (0, 256, 384), (0, 256, 256), (0, 256, 128), (0, 256, 0), (128, 128, 0)]
            for h in range(16):
                pt2 = PT2[h % 2]
                et = etmp[h % 2]
                Eh = Etab2[h % 2]
                S.dma('sp', et.ap, tb_d[:, h, :], writes=[et.k()], key=('et', h % 2))
                S.op('act', lambda e, et=et, Eh=Eh: e.activation(out=Eh.ap, in_=et.ap, func=AF.Exp),
                     reads=[et.k()], writes=[Eh.k()])
                S.op('dve', lambda e, Eh=Eh: e.memset(Eh.ap[0:64, 576:640], 0.0), writes=[Eh.k()])
                S.op('dve', lambda e, Eh=Eh: e.memset(Eh.ap[64:128, 0:64], 0.0), writes=[Eh.k()])
                for r in range(6):
                    q0, nq, qrel0 = RQ[r]
                    tsrc = ti - 2 + r // 2
                    ksl = tsrc % 3
                    kh = r % 2
                    gidx = 2 * (ti - 2) + r
                    ps = pscore.next()
                    kkey = knT.ke((h * 3 + ksl) * TT + kh * 128, (h * 3 + ksl) * TT + kh * 128 + 128)
                    S.op('pe', lambda e, ps=ps, h=h, ksl=ksl, kh=kh, q0=q0, nq=nq: e.matmul(
                        ps.ap[:, 0:nq], lhsT=knT.ap[:, h, ksl, kh * 128:(kh + 1) * 128], rhs=qnT.ap[:, h, q0:q0 + nq],
                        start=True, stop=True), reads=[kkey, qnT.k(h)], writes=[ps.k()])
                    ptb = PT[r % 2]
                    S.op('act', lambda e, ps=ps, ptb=ptb, nq=nq, gidx=gidx: e.activation(
                        out=ptb.ap[:, 0:nq], in_=ps.ap[:, 0:nq], func=AF.Exp, bias=mb.ap[:, gidx:gidx + 1],
                        scale=float(np.sqrt(128.0))), reads=[ps.k(), mb.k()], writes=[ptb.k()])
                    S.op('dve', lambda e, ptb=ptb, pt2=pt2, r=r, q0=q0, nq=nq, qrel0=qrel0, Eh=Eh: e.tensor_tensor(
                        out=pt2.ap[:, r, q0:q0 + nq], in0=ptb.ap[:, 0:nq], in1=Eh.ap[:, qrel0:qrel0 + nq], op=ALU.mult),
                        reads=[ptb.k(), Eh.k()], writes=[pt2.k(r)])

                def f(e, h=h, pt2=pt2):
                    for b in range(2):
                        for r in range(b, b + 5):
                            tsrc = ti - 2 + r // 2
                            e.matmul(ppv.ap[:, b * 128:(b + 1) * 128], lhsT=vtok.ap[:, tsrc % 3, r % 2, h * 128:(h + 1) * 128],
                                     rhs=pt2.ap[:, r, b * 128:(b + 1) * 128], start=(r == b), stop=(r == b + 4))
                    for b in range(2):
                        for r in range(b, b + 5):
                            ins = e.matmul(pden.ap[:, b * 128:(b + 1) * 128], lhsT=ones_b.ap,
                                           rhs=pt2.ap[:, r, b * 128:(b + 1) * 128], start=(r == b), stop=(r == b + 4))
                    return ins
                S.op('pe', f, reads=[vtok.k(), pt2.k(), ones_b.k()], writes=[ppv.k(), pden.k()])
                rec = tmpf[0]
                S.op('dve', lambda e: e.reciprocal(out=rec.ap, in_=pden.ap), reads=[pden.k()], writes=[rec.k()])
                S.op('dve', lambda e, h=h: e.tensor_tensor(out=attnT.ap[:, h, :], in0=ppv.ap, in1=rec.ap, op=ALU.mult),
                     reads=[ppv.k(), rec.k()], writes=[attnT.k(h)])
            for j in range(4):
                wa = load_w('w_ao', j, 'kc')
                wg = load_w('w_in', 24 + j, 'kc')
                for sub in range(4):
                    dc = 4 * j + sub
                    pa = pj.next()
                    pg = pj.next()
                    proj(wa, sub, attnT, pa)
                    proj(wg, sub, hT, pg)
                    sg = tmpf[4 + (dc % 2)]
                    tt_ = tmpf[2 + (dc % 2)]
                    S.op('act', lambda e, sg=sg, pg=pg: e.activation(out=sg.ap, in_=pg.ap, func=AF.Sigmoid),
                         reads=[pg.k()], writes=[sg.k()])
                    S.op('dve', lambda e, tt_=tt_, sg=sg, pa=pa: e.tensor_tensor(out=tt_.ap, in0=pa.ap, in1=sg.ap, op=ALU.mult),
                         reads=[pa.k(), sg.k()], writes=[tt_.k()])
                    S.op('pool', lambda e, dc=dc, tt_=tt_: e.tensor_tensor(out=merged.ap[:, dc, :], in0=tt_.ap, in1=mc.ap[:, dc, :], op=ALU.add),
                         reads=[tt_.k(), mc.k(dc)], writes=[merged.k(dc)])
            for j in range(4):
                wo = load_w('w_out', j, 'kc')
                for sub in range(4):
                    dc = 4 * j + sub
                    po = pj.next()
                    proj(wo, sub, merged, po)
                    S.op('dve', lambda e, dc=dc, po=po: e.tensor_tensor(out=xt.ap[:, dc, :], in0=po.ap, in1=xt.ap[:, dc, :], op=ALU.add),
                         reads=[po.k(), xt.k(dc)], writes=[xt.k(dc)])
            if debug and ti == HALO_T:
                S.dma('sp', dbg_mid.rearrange("(c p) t -> p c t", p=128), xt.ap, reads=[xt.k()], key='dbg')

            rmsnorm_to_hT(g2s)
            for j in range(4):
                wq = load_w('w_q', j, 'kc')
                for sub in range(4):
                    cc = 4 * j + sub
                    pq = pj.next()
                    proj(wq, sub, hT, pq)
                    S.op('act', lambda e, cc=cc, pq=pq: e.activation(out=pqT.ap[:, cc, :], in_=pq.ap, func=AF.Copy),
                         reads=[pq.k()], writes=[pqT.k(cc)])
            v1v = v1.ap.rearrange("p (h two) k -> p h two k", two=2)
            i1v = i1f.ap.rearrange("p (h two) k -> p h two k", two=2)
            for t2 in range(2):
                for g4 in range(4):
                    pf = pfull.next()

                    def f(e, g4=g4, pf=pf, t2=t2):
                        for i in range(4):
                            cc = 4 * g4 + i
                            ins = e.matmul(pf.ap[:, i * 128:(i + 1) * 128], lhsT=pqT.ap[:, cc, t2 * 128:(t2 + 1) * 128],
                                           rhs=k12.ap[:, (cc % 2) * 128:(cc % 2) * 128 + 128], start=True, stop=True)
                        return ins
                    S.op('pe', f, reads=[pqT.k(4 * g4, 4 * g4 + 4), k12.k()], writes=[pf.k()])
                    S.op('act', lambda e, g4=g4, pf=pf: e.activation(out=sc.ap[:, 4 * g4:4 * g4 + 4, :],
                                                                     in_=pf.ap.rearrange("p (a b) -> p a b", a=4), func=AF.Copy),
                         reads=[pf.k()], writes=[sc.k(4 * g4, 4 * g4 + 4)])
                for cc in range(16):
                    S.op('dve', lambda e, cc=cc: e.max(out=v1.ap[:, cc, 0:8], in_=sc.ap[:, cc, :]),
                         reads=[sc.k(cc)], writes=[v1.ke(cc * 16, cc * 16 + 8)])
                for cc in range(16):
                    S.op('dve', lambda e, cc=cc: e.max_index(out=i1u.ap[:, cc, 0:8], in_max=v1.ap[:, cc, 0:8], in_values=sc.ap[:, cc, :]),
                         reads=[sc.k(cc), v1.ke(cc * 16, cc * 16 + 8)], writes=[i1u.ke(cc * 16, cc * 16 + 8)])
                for cc in range(16):
                    S.op('dve', lambda e, cc=cc: e.match_replace(out=sc.ap[:, cc, :], in_to_replace=v1.ap[:, cc, 0:8],
                                                                 in_values=sc.ap[:, cc, :], imm_value=-1e30),
                         reads=[sc.k(cc), v1.ke(cc * 16, cc * 16 + 8)], writes=[sc.k(cc)])
                for cc in range(16):
                    S.op('dve', lambda e, cc=cc: e.max(out=v1.ap[:, cc, 8:16], in_=sc.ap[:, cc, :]),
                         reads=[sc.k(cc)], writes=[v1.ke(cc * 16 + 8, cc * 16 + 16)])
                for cc in range(16):
                    S.op('dve', lambda e, cc=cc: e.max_index(out=i1u.ap[:, cc, 8:16], in_max=v1.ap[:, cc, 8:16], in_values=sc.ap[:, cc, :]),
                         reads=[sc.k(cc), v1.ke(cc * 16 + 8, cc * 16 + 16)], writes=[i1u.ke(cc * 16 + 8, cc * 16 + 16)])
                S.op('dve', lambda e: e.tensor_copy(out=i1f.ap, in_=i1u.ap), reads=[i1u.k()], writes=[i1f.k()])
                cand4 = cand.ap.rearrange("p h (a b) -> p h a b", a=16)
                S.op('dve', lambda e: e.tensor_tensor(out=cand4, in0=v1v[:, :, 0, :].unsqueeze(3).to_broadcast([128, 8, 16, 16]),
                                                      in1=v1v[:, :, 1, :].unsqueeze(2).to_broadcast([128, 8, 16, 16]), op=ALU.add),
                     reads=[v1.k()], writes=[cand.k()])
                for h in range(8):
                    S.op('dve', lambda e, h=h: e.max(out=topv.ap[:, h, 0:8], in_=cand.ap[:, h, :]),
                         reads=[cand.k(h)], writes=[topv.ke(h * 16, h * 16 + 8)])
                for h in range(8):
                    S.op('dve', lambda e, h=h: e.max_index(out=flatu.ap[:, h, 0:8], in_max=topv.ap[:, h, 0:8], in_values=cand.ap[:, h, :]),
                         reads=[cand.k(h), topv.ke(h * 16, h * 16 + 8)], writes=[flatu.ke(h * 16, h * 16 + 8)])
                for h in range(8):
                    S.op('dve', lambda e, h=h: e.match_replace(out=cand.ap[:, h, :], in_to_replace=topv.ap[:, h, 0:8],
                                                               in_values=cand.ap[:, h, :], imm_value=-1e30),
                         reads=[cand.k(h), topv.ke(h * 16, h * 16 + 8)], writes=[cand.k(h)])
                for h in range(8):
                    S.op('dve', lambda e, h=h: e.max(out=topv.ap[:, h, 8:16], in_=cand.ap[:, h, :]),
                         reads=[cand.k(h)], writes=[topv.ke(h * 16 + 8, h * 16 + 16)])
                for h in range(8):
                    S.op('dve', lambda e, h=h: e.max_index(out=flatu.ap[:, h, 8:16], in_max=topv.ap[:, h, 8:16], in_values=cand.ap[:, h, :]),
                         reads=[cand.k(h), topv.ke(h * 16 + 8, h * 16 + 16)], writes=[flatu.ke(h * 16 + 8, h * 16 + 16)])
                S.op('dve', lambda e: e.tensor_single_scalar(out=au.ap, in_=flatu.ap, scalar=4, op=ALU.logical_shift_right),
                     reads=[flatu.k()], writes=[au.k()])
                S.op('dve', lambda e: e.tensor_single_scalar(out=bu.ap, in_=flatu.ap, scalar=15, op=ALU.bitwise_and),
                     reads=[flatu.k()], writes=[bu.k()])
                S.op('dve', lambda e: e.tensor_copy(out=af.ap, in_=au.ap), reads=[au.k()], writes=[af.k()])
                S.op('dve', lambda e: e.tensor_copy(out=bf_.ap, in_=bu.ap), reads=[bu.k()], writes=[bf_.k()])
                io4 = iota16.ap.unsqueeze(1).unsqueeze(1).to_broadcast([128, 8, 16, 16])
                for (src, which, dst) in ((af, 0, e1f), (bf_, 1, e2f)):
                    S.op('dve', lambda e, src=src: e.tensor_tensor(out=oh4.ap, in0=src.ap.unsqueeze(3).to_broadcast([128, 8, 16, 16]),
                                                                   in1=io4, op=ALU.is_equal),
                         reads=[src.k(), iota16.k()], writes=[oh4.k()])
                    S.op('dve', lambda e, which=which: e.tensor_tensor(out=oh4.ap, in0=oh4.ap,
                                                                       in1=i1v[:, :, which, :].unsqueeze(2).to_broadcast([128, 8, 16, 16]),
                                                                       op=ALU.mult),
                         reads=[oh4.k(), i1f.k()], writes=[oh4.k()])
                    S.op('dve', lambda e, dst=dst: e.tensor_reduce(out=dst.ap, in_=oh4.ap, axis=AX.X, op=ALU.add),
                         reads=[oh4.k()], writes=[dst.k()])
                S.op('dve', lambda e: e.tensor_tensor(out=gat.ap, in0=topv.ap, in1=topv.ap[:, :, 0:1].to_broadcast([128, 8, 16]),
                                                      op=ALU.subtract), reads=[topv.k()], writes=[gat.k()])
                S.op('act', lambda e: e.activation(out=gat.ap, in_=gat.ap, func=AF.Exp), reads=[gat.k()], writes=[gat.k()])
                S.op('dve', lambda e: e.tensor_reduce(out=gsum.ap, in_=gat.ap, axis=AX.X, op=ALU.add),
                     reads=[gat.k()], writes=[gsum.k()])
                S.op('dve', lambda e: e.reciprocal(out=gsum.ap, in_=gsum.ap), reads=[gsum.k()], writes=[gsum.k()])
                S.op('dve', lambda e: e.tensor_tensor(out=gat.ap, in0=gat.ap, in1=gsum.ap.unsqueeze(2).to_broadcast([128, 8, 16]),
                                                      op=ALU.mult), reads=[gat.k(), gsum.k()], writes=[gat.k()])
                ptr = pfull2

                def f(e):
                    e.transpose(out=ptr.ap[:, 0:128], in_=e1f.ap.rearrange("p h k -> p (h k)"), identity=ident.ap)
                    e.transpose(out=ptr.ap[:, 128:256], in_=e2f.ap.rearrange("p h k -> p (h k)"), identity=ident.ap)
                    return e.transpose(out=ptr.ap[:, 256:384], in_=gat.ap.rearrange("p h k -> p (h k)"), identity=ident.ap)
                S.op('pe', f, reads=[e1f.k(), e2f.k(), gat.k(), ident.k()], writes=[ptr.k()])
                for i, dstb in enumerate((Ism, Jsm, gsm)):
                    S.op('act', lambda e, i=i, dstb=dstb, t2=t2: e.activation(out=dstb.ap[:, t2 * 128:(t2 + 1) * 128],
                                                                              in_=ptr.ap[:, i * 128:(i + 1) * 128], func=AF.Copy),
                         reads=[ptr.k()], writes=[dstb.ke(t2 * 128, (t2 + 1) * 128)])

            Gs = [Gsub, Gsub2]

            def gbuild_batch(eg, tb, G):
                oj, cb, cf = OJb[tb % 2], Cb[tb % 2], Cf[tb % 2]
                S.op('dve', lambda e: e.tensor_tensor(
                    out=oj.ap, in0=iota128b.ap.unsqueeze(1).to_broadcast([128, 16, 128]),
                    in1=Jsm.ap[:, tb * 16:(tb + 1) * 16].unsqueeze(2).to_broadcast([128, 16, 128]), op=ALU.is_equal),
                    reads=[iota128b.k(), Jsm.k()], writes=[oj.k()])
                S.op('dve', lambda e: e.tensor_tensor(
                    out=cf.ap, in0=iota128b.ap[:, eg * EG:(eg + 1) * EG].unsqueeze(1).to_broadcast([128, 16, EG]),
                    in1=Ism.ap[:, tb * 16:(tb + 1) * 16].unsqueeze(2).to_broadcast([128, 16, EG]), op=ALU.is_equal),
                    reads=[iota128b.k(), Ism.k()], writes=[cf.k()])
                S.op('pool', lambda e: e.tensor_tensor(
                    out=cb.ap, in0=cf.ap, in1=gsm.ap[:, tb * 16:(tb + 1) * 16].unsqueeze(2).to_broadcast([128, 16, EG]),
                    op=ALU.mult), reads=[cf.k(), gsm.k()], writes=[cb.k()])
                pf = pfull.next()

                def f(e):
                    for t in range(16):
                        ins = e.matmul(pf.ap[:, t * EG:(t + 1) * EG], lhsT=oj.ap[:, t, :], rhs=cb.ap[:, t, :],
                                       start=True, stop=True)
                    return ins
                S.op('pe', f, reads=[oj.k(), cb.k()], writes=[pf.k()])
                S.op('act', lambda e: e.activation(out=G.ap[:, :, tb * 16:(tb + 1) * 16],
                                                   in_=pf.ap.rearrange("p (t e) -> p e t", e=EG), func=AF.Copy),
                     reads=[pf.k()], writes=[G.k()])

            def dense_cg(eg, cg, G):
                wu = load_w('uT', eg * (EG // CG) + cg, 'kc')
                wvv = load_w('ev', eg * (EG // CG) + cg, 'ec')
                wt = wTb[cg % 2]
                for cc in range(CG):
                    pa = pj.next()
                    proj(wu, cc, hT, pa)
                    ge = gel[cc % 2]
                    S.op('act', lambda e, ge=ge, pa=pa: e.activation(out=ge.ap, in_=pa.ap, func=AF.Gelu),
                         reads=[pa.k()], writes=[ge.k()])
                    S.op('dve', lambda e, ge=ge, cc=cc: e.tensor_tensor(
                        out=wt.ap[:, cc, :], in0=ge.ap, in1=G.ap[:, cg * CG + cc, :], op=ALU.mult),
                        reads=[ge.k(), G.k(cg * CG + cc)], writes=[wt.k(cc)])
                for dc in range(16):
                    py = ppy.next()

                    def f(e, py=py, dc=dc):
                        for cc in range(CG):
                            ins = e.matmul(py.ap, lhsT=wvv.ap[:, cc, dc * 128:(dc + 1) * 128], rhs=wt.ap[:, cc, :],
                                           start=(cc == 0), stop=(cc == CG - 1))
                        return ins
                    S.op('pe', f, reads=[wvv.k(), wt.k()], writes=[py.k()])
                    S.op('dve', lambda e, dc=dc, py=py: e.tensor_tensor(out=xt.ap[:, dc, :], in0=py.ap, in1=xt.ap[:, dc, :], op=ALU.add),
                         reads=[py.k(), xt.k(dc)], writes=[xt.k(dc)])

            NEG = NEXP_CH // EG
            NB = TT // 16
            for tb in range(NB):
                gbuild_batch(0, tb, Gs[0])
            per = NB // (EG // CG)
            for eg in range(NEG):
                for cg in range(EG // CG):
                    dense_cg(eg, cg, Gs[eg % 2])
                    if eg + 1 < NEG:
                        for tb in range(cg * per, (cg + 1) * per):
                            gbuild_batch(eg + 1, tb, Gs[(eg + 1) % 2])
            S.dma('sp', yT[:, (ti - HALO_T) * TT:(ti - HALO_T + 1) * TT].rearrange("(c p) t -> p c t", p=128), xt.ap,
                  reads=[xt.k()], key='yst')
        for ti_ in range(NTILE):
            do_tile(ti_)
        S.finish('sp')
        S.emit_all()
    return nc


def _prep_shared(inputs):
    g = lambda k: np.asarray(inputs[k], dtype=np.float32)[0]
    sh = {}
    sh["w_in"] = np.ascontiguousarray(g("w_in"))
    sh["w_co"] = np.ascontiguousarray(g("w_conv_out"))
    sh["w_ao"] = np.ascontiguousarray(g("w_attn_o"))
    sh["w_out"] = np.ascontiguousarray(g("w_out"))
    sh["w_q"] = np.ascontiguousarray(g("w_query"))
    sh["uT"] = np.ascontiguousarray(g("expert_u").T)
    sh["ev"] = np.ascontiguousarray(g("expert_v"))
    fm = lambda v: v.reshape(16, 128).T
    vecs = np.zeros((128, 578), np.float32)
    vecs[:, 0:16] = fm(g("norm1_g"))
    vecs[:, 16:32] = fm(g("norm2_g"))
    vecs[:, 32:48] = fm(g("conv_dw_b"))
    vecs[:, 48:64] = fm(g("conv_ln_g"))
    vecs[:, 64:80] = fm(g("conv_ln_b"))
    vecs[:, 80] = g("q_norm_g")
    vecs[:, 81] = g("k_norm_g")
    dw = g("conv_dw_w")
    vecs[:, 82:] = dw.reshape(31, 16, 128).transpose(2, 1, 0).reshape(128, 16 * 31)
    sh["vecs"] = vecs
    kk = np.arange(128)[:, None]
    mm = np.arange(640)[None, :]
    idx = np.clip(mm - kk, -63, 128) + 63
    rb = g("rel_bias")
    sh["tb"] = np.ascontiguousarray(rb[:, idx].transpose(1, 0, 2))
    sh["k12"] = np.ascontiguousarray(np.concatenate([g("sub_keys_1").T, g("sub_keys_2").T], axis=1))
    return sh


_NC_CACHE = {}


def kernel(**inputs):
    x = np.asarray(inputs["x"], dtype=np.float32)[0]
    xTf = np.ascontiguousarray(x.T)
    sh = _prep_shared(inputs)
    halo = HALO_T * TT
    in_maps = []
    for c in range(NCORE):
        xc = np.zeros((D, halo + TOK_CORE), np.float32)
        xc[:, halo:] = xTf[:, c * TOK_CORE:(c + 1) * TOK_CORE]
        mbc = np.zeros((128, 20), np.float32)
        if c > 0:
            xc[:, :halo] = xTf[:, c * TOK_CORE - halo:c * TOK_CORE]
        else:
            mbc[:, 0:4] = -30000.0
        m = dict(sh)
        m["xT"] = xc
        m["mb"] = mbc
        in_maps.append(m)
    if "nc" not in _NC_CACHE:
        _NC_CACHE["nc"] = build_nc()
    res = run_bass_kernel_spmd(_NC_CACHE["nc"], in_maps, core_ids=list(range(NCORE)))
    out = np.empty((1, NCORE * TOK_CORE, D), np.float32)
    for c in range(NCORE):
        out[0, c * TOK_CORE:(c + 1) * TOK_CORE, :] = res.results[c]["yT"].T
    return out
```

```python
import numpy as np
from contextlib import ExitStack
import concourse.bass as bass
import concourse.mybir as mybir
from concourse.bass_utils import run_bass_kernel_spmd

F32 = mybir.dt.float32
BF16 = mybir.dt.bfloat16
U32 = mybir.dt.uint32
ALU = mybir.AluOpType
AF = mybir.ActivationFunctionType
AX = mybir.AxisListType

D = 2048
NCORE = 8
TOK_CORE = 2048
TT = 256
HALO_T = 2
NT_OWN = TOK_CORE // TT
EPS = 1e-6
NEXP_CH = 128
EG = 32
CG = 4


class Sched:
    ENG = ['pe', 'act', 'dve', 'pool', 'sp']
    EPOCH = 12000

    def __init__(self, nc, stack):
        self.nc = nc
        self.stack = stack
        self.streams = {e: [] for e in self.ENG}
        self.sem = {}
        self.cnt = {}
        self.nsem = 0
        for e in self.ENG:
            self._new_epoch(e)
        self.known = {e: {} for e in self.ENG}
        self.wr = {}
        self.rd = {}
        self.dma_sems = {}

    def _new_sem(self, name):
        self.nsem += 1
        return self.stack.enter_context(self.nc.semaphore(f"{name}_{self.nsem}"))

    def _new_epoch(self, e):
        self.sem[e] = self._new_sem(f"s_{e}")
        self.cnt[e] = 0

    def _deps(self, eng, reads, writes):
        deps = {}

        def need(ev, same_ok):
            sem, val, src = ev
            k = id(sem)
            if k not in deps or deps[k][1] < val:
                deps[k] = (sem, val)

        for (a, lo, hi) in reads:
            for (l2, h2, ev) in self.wr.get(a, ()):
                if l2 < hi and lo < h2:
                    need(ev, False)
        for (a, lo, hi) in writes:
            for (l2, h2, ev) in self.wr.get(a, ()):
                if l2 < hi and lo < h2:
                    need(ev, True)
            for (l2, h2, ev) in self.rd.get(a, ()):
                if l2 < hi and lo < h2:
                    need(ev, True)
        waits = []
        kn = self.known[eng]
        for k, (sem, val) in deps.items():
            if kn.get(k, 0) < val:
                kn[k] = val
                waits.append((sem, val))
        return waits

    def _commit(self, ev, reads, writes):
        for (a, lo, hi) in writes:
            self.wr[a] = [t for t in self.wr.get(a, ()) if not (t[0] >= lo and t[1] <= hi)]
            self.rd[a] = [t for t in self.rd.get(a, ()) if not (t[0] >= lo and t[1] <= hi)]
            self.wr[a].append((lo, hi, ev))
        for (a, lo, hi) in reads:
            lst = self.rd.setdefault(a, [])
            for i, t in enumerate(lst):
                if t[0] == lo and t[1] == hi and t[2][0] is ev[0]:
                    lst[i] = (lo, hi, ev)
                    break
            else:
                lst.append((lo, hi, ev))

    def op(self, eng, fn, reads=(), writes=()):
        reads = list(reads)
        writes = list(writes)
        waits = self._deps(eng, reads, writes)
        if self.cnt[eng] >= self.EPOCH:
            self._new_epoch(eng)
        self.cnt[eng] += 1
        mysem = self.sem[eng]
        ev = (mysem, self.cnt[eng], eng)

        def emit(e):
            for sem, val in waits:
                e.wait_ge(sem, val)
            ins = fn(e)
            ins.then_inc(mysem, 1)

        self.streams[eng].append(emit)
        self._commit(ev, reads, writes)
        return ev

    def dma(self, queue, out, in_, reads=(), writes=(), key=None):
        reads = list(reads)
        writes = list(writes)
        waits = self._deps(queue, reads, writes)
        if key not in self.dma_sems:
            self.dma_sems[key] = [self._new_sem("d"), 0]
        ent = self.dma_sems[key]
        ent[1] += 16
        sem = ent[0]
        ev = (sem, ent[1], None)

        def emit(e):
            for s, val in waits:
                e.wait_ge(s, val)
            e.dma_start(out=out, in_=in_).then_inc(sem, 16)

        self.streams[queue].append(emit)
        self._commit(ev, reads, writes)
        return ev

    def finish(self, eng='sp'):
        waits = [(ent[0], ent[1]) for ent in self.dma_sems.values()]
        for e in self.ENG:
            if e != eng and self.cnt[e] > 0:
                waits.append((self.sem[e], self.cnt[e]))

        def emit(e):
            for s, val in waits:
                e.wait_ge(s, val)

        self.streams[eng].append(emit)

    def emit_all(self):
        with self.nc.Block() as block:
            @block.tensor
            def _(e):
                for f in self.streams['pe']:
                    f(e)

            @block.scalar
            def _(e):
                for f in self.streams['act']:
                    f(e)

            @block.vector
            def _(e):
                for f in self.streams['dve']:
                    f(e)

            @block.gpsimd
            def _(e):
                for f in self.streams['pool']:
                    f(e)

            @block.sync
            def _(e):
                for f in self.streams['sp']:
                    f(e)


class Buf:
    def __init__(self, arena_name, ap, off, dt_bytes):
        self.an = arena_name
        self.ap = ap
        self.off = off
        self.eb = dt_bytes
        sh = ap.shape[1:]
        self.n = int(np.prod(sh))
        self.inner = int(np.prod(sh[1:])) if len(sh) > 1 else 1

    def k(self, i=None, j=None):
        if i is None:
            return (self.an, self.off, self.off + self.n * self.eb)
        if j is None:
            j = i + 1
        return (self.an, self.off + i * self.inner * self.eb, self.off + j * self.inner * self.eb)

    def ke(self, lo, hi):
        return (self.an, self.off + lo * self.eb, self.off + hi * self.eb)


def build_nc(nt_own=NT_OWN, debug=False):
    nc = bass.Bass("TRN2", target_bir_lowering=False)
    NTILE = HALO_T + nt_own
    NTOK = NTILE * TT
    xT = nc.dram_tensor("xT", [D, (HALO_T + NT_OWN) * TT], F32, kind="ExternalInput").ap()
    w_in = nc.dram_tensor("w_in", [D, 14336], F32, kind="ExternalInput").ap()
    w_co = nc.dram_tensor("w_co", [D, D], F32, kind="ExternalInput").ap()
    w_ao = nc.dram_tensor("w_ao", [D, D], F32, kind="ExternalInput").ap()
    w_out = nc.dram_tensor("w_out", [D, D], F32, kind="ExternalInput").ap()
    w_q = nc.dram_tensor("w_q", [D, D], F32, kind="ExternalInput").ap()
    uT = nc.dram_tensor("uT", [D, 16384], F32, kind="ExternalInput").ap()
    ev_d = nc.dram_tensor("ev", [16384, D], F32, kind="ExternalInput").ap()
    vecs_d = nc.dram_tensor("vecs", [128, 578], F32, kind="ExternalInput").ap()
    tb_d = nc.dram_tensor("tb", [128, 16, 640], F32, kind="ExternalInput").ap()
    mb_d = nc.dram_tensor("mb", [128, 20], F32, kind="ExternalInput").ap()
    k12_d = nc.dram_tensor("k12", [128, 256], F32, kind="ExternalInput").ap()
    yT = nc.dram_tensor("yT", [D, NT_OWN * TT], F32, kind="ExternalOutput").ap()
    NBLK = 28 + 16 + 64
    wsc = nc.dram_tensor("wsc", [NBLK, 128, 8192], BF16, kind="Internal").ap()
    if debug:
        dbg_mid = nc.dram_tensor("dbg_mid", [D, TT], F32, kind="ExternalOutput").ap()

    with ExitStack() as st:
        S = Sched(nc, st)

        def sb(name, shape, dt):
            t = st.enter_context(nc.sbuf_tensor("s_" + name, shape, dt))
            eb = 4 if dt in (F32, U32) else 2
            return Buf(name, t[:], 0, eb)

        xt = sb("xt", [128, 16, TT], F32)
        hT = sb("hT", [128, 16, TT], BF16)
        knT = sb("knT", [128, 16, 3, TT], BF16)
        vtok = sb("vtok", [128, 3, 2, D], BF16)
        Etab2 = [sb(f"Etab{i}", [128, 640], BF16) for i in range(2)]
        NWB = 3
        wbt = [sb(f"wb{i}", [128, 8192], BF16) for i in range(NWB)]
        ubuf = sb("ubuf", [128, 16, 30 + TT], BF16)
        vecs = sb("vecs", [128, 578], F32)
        mb = sb("mb", [128, 20], F32)
        k12 = sb("k12", [128, 256], BF16)
        ones_f = sb("ones_f", [128, 128], F32)
        ones_b = sb("ones_b", [128, 128], BF16)
        ident = sb("ident", [128, 128], F32)
        iota128 = sb("iota128", [128, 128], F32)
        iota16 = sb("iota16", [128, 16], F32)
        g1s = sb("g1s", [128, 16], F32)
        g2s = sb("g2s", [128, 16], F32)

        SCR_BYTES = 60 * 1024
        scr_t = st.enter_context(nc.sbuf_tensor("scr", [128, SCR_BYTES // 2], BF16))

        def scr(off, shape, dt):
            eb = 4 if dt in (F32, U32) else 2
            n = int(np.prod(shape))
            assert off % 4 == 0 and off + n * eb <= SCR_BYTES, (off, shape)
            ap = scr_t[:, off // 2: off // 2 + n * eb // 2]
            if dt != BF16:
                ap = ap.bitcast(dt)
            if len(shape) == 2:
                ap = ap.rearrange("p (a b) -> p a b", a=shape[0])
            elif len(shape) == 3:
                ap = ap.rearrange("p (a b c) -> p a b c", a=shape[0], b=shape[1])
            return Buf("scr", ap, off, eb)

        K = 1024
        ybuf = scr(0, [16, TT], F32)
        qnT = scr(0, [16, TT], BF16)
        attnT = scr(8 * K, [16, TT], BF16)
        su = scr(16 * K, [16, TT], BF16)
        merged = scr(16 * K, [16, TT], BF16)
        mc = scr(24 * K, [16, TT], BF16)
        PT = [scr(32 * K + i * K, [TT], F32) for i in range(2)]
        PT2 = [scr(34 * K + i * 3 * K, [6, TT], BF16) for i in range(2)]
        tmpf = [scr(40 * K + i * K, [TT], F32) for i in range(6)]
        sqt = [scr(46 * K + i * 4 * K, [4, TT], F32) for i in range(2)]
        etmp = [scr(54 * K + i * 2560, [640], F32) for i in range(2)]
        pqT = scr(0, [16, TT], BF16)
        sc = scr(8 * K, [16, 128], F32)
        Gsub = scr(16 * K, [EG, TT], BF16)
        Gsub2 = scr(0, [EG, TT], BF16)
        cand = scr(32 * K, [8, 256], F32)
        wk = [scr(40 * K + i * K, [256], F32) for i in range(2)]
        gel = [scr(42 * K + i * K, [TT], F32) for i in range(2)]
        v1 = scr(44 * K, [16, 16], F32)
        i1u = scr(45 * K, [16, 16], U32)
        i1f = scr(46 * K, [16, 16], F32)
        topv = scr(47 * K, [8, 16], F32)
        flatu = scr(47 * K + 512, [8, 16], U32)
        af = scr(48 * K, [8, 16], F32)
        bf_ = scr(48 * K + 512, [8, 16], F32)
        oh4 = scr(49 * K, [8, 16, 16], F32)
        e1f = scr(57 * K, [8, 16], F32)
        e2f = scr(57 * K + 512, [8, 16], F32)
        gat = scr(58 * K, [8, 16], F32)
        gsum = scr(58 * K + 512, [8], F32)
        au = scr(58 * K + 512 + 64, [8, 16], U32)
        bu = scr(59 * K + 64, [8, 16], U32)
        ytmp = [scr(54 * K + i * K, [TT], F32) for i in range(4)]
        Ism = sb("Ism", [128, TT], BF16)
        Jsm = sb("Jsm", [128, TT], BF16)
        iota128b = sb("iota128b", [128, 128], BF16)
        gsm = sb("gsm", [128, TT], F32)
        OJb = [scr(32 * K + i * 4 * K, [16, 128], BF16) for i in range(2)]
        Cb = [scr(44 * K + i * K, [16, 32], BF16) for i in range(2)]
        Cf = [scr(46 * K + i * 2 * K, [16, 32], BF16) for i in range(2)]
        wTb = [scr(50 * K + i * 2 * K, [CG, TT], BF16) for i in range(2)]

        pst = [st.enter_context(nc.psum_tensor(f"ps{i}", [128, 512], F32)) for i in range(8)]

        class PBuf(Buf):
            def __init__(self, bank, ap):
                Buf.__init__(self, "ps", ap, bank * 2048, 4)
                self.bank = bank

            def k(self, i=None, j=None):
                return ("ps", self.bank * 2048, (self.bank + 1) * 2048)

        def psh(bank, half):
            return PBuf(bank, pst[bank][:, half * 256:half * 256 + 256])

        def psf(b):
            return PBuf(b, pst[b][:, :])

        class Rot:
            def __init__(self, items):
                self.items = items
                self.i = 0

            def next(self):
                r = self.items[self.i % len(self.items)]
                self.i += 1
                return r

        pj = Rot([psh(0, 0), psh(1, 0), psh(2, 0)])
        pstat = Rot([psh(3, 0), psh(3, 1)])
        pscore = Rot([psh(4, 0), psh(5, 0)])
        ppv = psh(6, 0)
        pden = psh(6, 1)
        pfull = Rot([psf(6), psf(7)])
        pfull2 = psf(5)
        ppy = Rot([psh(3, 0), psh(4, 0), psh(5, 0)])
        wrot = Rot(list(range(NWB)))

        V = lambda c0, n=1: vecs.ap[:, c0:c0 + n]
        N1G, N2G, DWB, LNG, LNB, QG, KG, DWW = 0, 16, 32, 48, 64, 80, 81, 82

        S.dma('sp', vecs.ap, vecs_d, writes=[vecs.k()], key='c0')
        S.dma('sp', mb.ap, mb_d, writes=[mb.k()], key='c1')
        S.dma('pool', k12.ap, k12_d, writes=[k12.k()], key='c2')
        S.op('dve', lambda e: e.memset(ones_f.ap, 1.0), writes=[ones_f.k()])
        S.op('dve', lambda e: e.memset(ones_b.ap, 1.0), writes=[ones_b.k()])
        S.op('dve', lambda e: e.memset(ubuf.ap, 0.0), writes=[ubuf.k()])
        S.op('pool', lambda e: e.iota(ident.ap, pattern=[[1, 128]], base=0, channel_multiplier=-1,
                                      allow_small_or_imprecise_dtypes=True), writes=[ident.k()])
        S.op('dve', lambda e: e.tensor_scalar(out=ident.ap, in0=ident.ap, scalar1=0.0, scalar2=None,
                                              op0=ALU.is_equal), reads=[ident.k()], writes=[ident.k()])
        S.op('pool', lambda e: e.iota(iota128.ap, pattern=[[1, 128]], base=0, channel_multiplier=0,
                                      allow_small_or_imprecise_dtypes=True), writes=[iota128.k()])
        S.op('dve', lambda e: e.tensor_copy(out=iota128b.ap, in_=iota128.ap), reads=[iota128.k()], writes=[iota128b.k()])
        S.op('pool', lambda e: e.iota(iota16.ap, pattern=[[1, 16]], base=0, channel_multiplier=0,
                                      allow_small_or_imprecise_dtypes=True), writes=[iota16.k()])
        S.op('dve', lambda e: e.tensor_scalar(out=g1s.ap, in0=V(N1G, 16), scalar1=float(np.sqrt(D)), scalar2=None,
                                              op0=ALU.mult), reads=[vecs.k()], writes=[g1s.k()])
        S.op('dve', lambda e: e.tensor_scalar(out=g2s.ap, in0=V(N2G, 16), scalar1=float(np.sqrt(D)), scalar2=None,
                                              op0=ALU.mult), reads=[vecs.k()], writes=[g2s.k()])
        blk_id = {}
        NCVSEM = 56
        CV_AHEAD = 4

        def conv_blk(name, src_ap, j, pattern):
            b = len(blk_id)
            blk_id[(name, j)] = b
            c = 16 if pattern == 'kc' else 4
            S.dma('pool', wsc[b].rearrange("p (c n) -> p c n", c=c), src_ap.rearrange("(c p) n -> p c n", p=128),
                  reads=([('wsc', b - CV_AHEAD, b - CV_AHEAD + 1)] if b >= CV_AHEAD else []),
                  writes=[('wsc', b, b + 1), ('cvslot', b % NCVSEM, b % NCVSEM + 1)], key=('cv', b % NCVSEM))

        def conv_in(j0):
            for j in range(4):
                conv_blk('w_in', w_in[:, j0 + 512 * j:j0 + 512 * (j + 1)], (j0 // 512) + j, 'kc')

        conv_in(6144)
        conv_in(8192)
        for j in range(4):
            conv_blk('w_in', w_in[:, 512 * j:512 * (j + 1)], j, 'kc')
            conv_blk('w_in', w_in[:, 2048 + 512 * j:2048 + 512 * (j + 1)], 4 + j, 'kc')
        for j in range(4):
            conv_blk('w_co', w_co[:, 512 * j:512 * (j + 1)], j, 'kc')
            conv_blk('w_in', w_in[:, 10240 + 512 * j:10240 + 512 * (j + 1)], 20 + j, 'kc')
        conv_in(4096)
        for j in range(4):
            conv_blk('w_ao', w_ao[:, 512 * j:512 * (j + 1)], j, 'kc')
            conv_blk('w_in', w_in[:, 12288 + 512 * j:12288 + 512 * (j + 1)], 24 + j, 'kc')
        for j in range(4):
            conv_blk('w_out', w_out[:, 512 * j:512 * (j + 1)], j, 'kc')
        for j in range(4):
            conv_blk('w_q', w_q[:, 512 * j:512 * (j + 1)], j, 'kc')
        for j in range(NEXP_CH // CG):
            conv_blk('uT', uT[:, 512 * j:512 * (j + 1)], j, 'kc')
            conv_blk('ev', ev_d[512 * j:512 * (j + 1), :], j, 'ec')
        assert len(blk_id) == NBLK

        def load_w(name, j, rows_pattern, wi=None):
            if wi is None:
                wi = wrot.next()
            wb = wbt[wi]
            b = blk_id[(name, j)]
            c = 16 if rows_pattern == 'kc' else 4
            view = Buf(wb.an, wb.ap.rearrange("p (c n) -> p c n", c=c), 0, 2)
            S.dma('sp', wb.ap, wsc[b], reads=[('wsc', b, b + 1)], writes=[wb.k()], key=('wb', wi))
            return view

        def proj(wv, sub, rhsbuf, ps):
            def f(e):
                for c in range(16):
                    ins = e.matmul(ps.ap, lhsT=wv.ap[:, c, sub * 128:(sub + 1) * 128], rhs=rhsbuf.ap[:, c, :],
                                   start=(c == 0), stop=(c == 15))
                return ins
            S.op('pe', f, reads=[wv.k(), rhsbuf.k()], writes=[ps.k()])

        def rsqrt_chain(dst, src_ap, src_key, addc):
            S.op('dve', lambda e: e.tensor_scalar(out=dst.ap, in0=src_ap, scalar1=float(addc), scalar2=None, op0=ALU.add),
                 reads=[src_key], writes=[dst.k()])
            S.op('act', lambda e: e.activation(out=dst.ap, in_=dst.ap, func=AF.Sqrt), reads=[dst.k()], writes=[dst.k()])
            S.op('dve', lambda e: e.reciprocal(out=dst.ap, in_=dst.ap), reads=[dst.k()], writes=[dst.k()])

        def rmsnorm_to_hT(gs):
            pss = pstat.next()
            for q4 in range(4):
                sq = sqt[q4 % 2]
                S.op('act', lambda e, q4=q4, sq=sq: e.activation(out=sq.ap, in_=xt.ap[:, 4 * q4:4 * q4 + 4, :], func=AF.Square),
                     reads=[xt.k(4 * q4, 4 * q4 + 4)], writes=[sq.k()])

                def f(e, q4=q4, sq=sq):
                    for j in range(4):
                        ins = e.matmul(pss.ap, lhsT=ones_f.ap, rhs=sq.ap[:, j, :], start=(q4 == 0 and j == 0),
                                       stop=(q4 == 3 and j == 3))
                    return ins
                S.op('pe', f, reads=[sq.k(), ones_f.k()], writes=[pss.k()])
            rs = tmpf[0]
            rsqrt_chain(rs, pss.ap, pss.k(), D * EPS)
            for c in range(16):
                S.op('dve', lambda e, c=c: e.scalar_tensor_tensor(out=hT.ap[:, c, :], in0=xt.ap[:, c, :], scalar=gs.ap[:, c:c + 1],
                                                                 in1=rs.ap, op0=ALU.mult, op1=ALU.mult),
                     reads=[xt.k(c), rs.k(), gs.k()], writes=[hT.k(c)])

        def qk_norm(ps, gcol, out_ap, out_key):
            sq = tmpf[1]
            S.op('act', lambda e: e.activation(out=sq.ap, in_=ps.ap, func=AF.Square), reads=[ps.k()], writes=[sq.k()])
            pss = pstat.next()
            S.op('pe', lambda e: e.matmul(pss.ap, lhsT=ones_f.ap, rhs=sq.ap, start=True, stop=True),
                 reads=[sq.k(), ones_f.k()], writes=[pss.k()])
            rs = tmpf[2]
            rsqrt_chain(rs, pss.ap, pss.k(), 128 * EPS)
            S.op('dve', lambda e: e.scalar_tensor_tensor(out=out_ap, in0=ps.ap, scalar=V(gcol), in1=rs.ap,
                                                         op0=ALU.mult, op1=ALU.mult),
                 reads=[ps.k(), rs.k(), vecs.k()], writes=[out_key])

        def do_tile(ti):
            own = ti >= HALO_T
            slot = ti % 3
            S.dma('sp', xt.ap, xT[:, ti * TT:(ti + 1) * TT].rearrange("(c p) t -> p c t", p=128),
                  writes=[xt.k()], key='xt')
            rmsnorm_to_hT(g1s)

            if ti >= 1:
                for j in range(4):
                    wa = load_w('w_in', j, 'kc')
                    wg = load_w('w_in', 4 + j, 'kc')
                    for sub in range(4):
                        c = 4 * j + sub
                        pa = pj.next()
                        pg = pj.next()
                        proj(wa, sub, hT, pa)
                        proj(wg, sub, hT, pg)
                        sg = tmpf[3 + (c % 2)]
                        S.op('act', lambda e, sg=sg, pg=pg: e.activation(out=sg.ap, in_=pg.ap, func=AF.Sigmoid),
                             reads=[pg.k()], writes=[sg.k()])
                        S.op('dve', lambda e, c=c, sg=sg, pa=pa: e.tensor_tensor(out=ubuf.ap[:, c, 30:30 + TT], in0=pa.ap, in1=sg.ap, op=ALU.mult),
                             reads=[pa.k(), sg.k()], writes=[ubuf.k(c)])
            conv_q = []
            if own:
                for g4 in range(4):
                    cs = [4 * g4 + i for i in range(4)]
                    for c in cs:
                        conv_q.append(lambda c=c: S.op('dve', lambda e: e.tensor_scalar(
                            out=ybuf.ap[:, c, :], in0=ubuf.ap[:, c, 0:TT], scalar1=V(DWW + c * 31),
                            scalar2=V(DWB + c), op0=ALU.mult, op1=ALU.add),
                            reads=[ubuf.k(c), vecs.k()], writes=[ybuf.k(c)]))
                    for j in range(1, 31):
                        for c in cs:
                            conv_q.append(lambda c=c, j=j: S.op('dve', lambda e: e.scalar_tensor_tensor(
                                out=ybuf.ap[:, c, :], in0=ubuf.ap[:, c, j:j + TT],
                                scalar=V(DWW + c * 31 + j), in1=ybuf.ap[:, c, :], op0=ALU.mult, op1=ALU.add),
                                reads=[ubuf.k(c), vecs.k(), ybuf.k(c)], writes=[ybuf.k(c)]))
            conv_q.reverse()

            def conv_emit(n):
                for _ in range(n):
                    if conv_q:
                        conv_q.pop()()

            for j in range(4):
                wv = load_w('w_in', 12 + j, 'kc')
                for sub in range(4):
                    h = 4 * j + sub
                    ps = pj.next()
                    proj(wv, sub, hT, ps)
                    qk_norm(ps, KG, knT.ap[:, h, slot, :], knT.ke((h * 3 + slot) * TT, (h * 3 + slot + 1) * TT))
                    conv_emit(24)
            for j in range(4):
                wv = load_w('w_in', 16 + j, 'kc')
                for t2 in range(2):
                    pf = pfull.next()

                    def f(e, wv=wv, t2=t2, pf=pf):
                        for c in range(16):
                            ins = e.matmul(pf.ap, lhsT=hT.ap[:, c, t2 * 128:(t2 + 1) * 128], rhs=wv.ap[:, c, :],
                                           start=(c == 0), stop=(c == 15))
                        return ins
                    S.op('pe', f, reads=[wv.k(), hT.k()], writes=[pf.k()])
                    S.op('act', lambda e, t2=t2, j=j, pf=pf: e.activation(out=vtok.ap[:, slot, t2, 512 * j:512 * (j + 1)], in_=pf.ap, func=AF.Copy),
                         reads=[pf.k()], writes=[vtok.ke((slot * 2 + t2) * D + 512 * j, (slot * 2 + t2) * D + 512 * (j + 1))])
                    conv_emit(14)
            conv_emit(10000)
            if ti == 0:
                return
            if not own:
                S.op('pool', lambda e: e.tensor_copy(out=ubuf.ap[:, :, 0:30], in_=ubuf.ap[:, :, TT:TT + 30]),
                     reads=[ubuf.k()], writes=[ubuf.k()])
                return
            S.op('pool', lambda e: e.tensor_copy(out=ubuf.ap[:, :, 0:30], in_=ubuf.ap[:, :, TT:TT + 30]),
                 reads=[ubuf.k()], writes=[ubuf.k()])
            ps1 = pstat.next()
            ps2 = pstat.next()

            def f(e):
                for c in range(16):
                    ins = e.matmul(ps1.ap, lhsT=ones_f.ap, rhs=ybuf.ap[:, c, :], start=(c == 0), stop=(c == 15))
                return ins
            S.op('pe', f, reads=[ybuf.k(), ones_f.k()], writes=[ps1.k()])
            for q4 in range(4):
                sq = sqt[q4 % 2]
                S.op('act', lambda e, q4=q4, sq=sq: e.activation(out=sq.ap, in_=ybuf.ap[:, 4 * q4:4 * q4 + 4, :], func=AF.Square),
                     reads=[ybuf.k(4 * q4, 4 * q4 + 4)], writes=[sq.k()])

                def f(e, q4=q4, sq=sq):
                    for j in range(4):
                        ins = e.matmul(ps2.ap, lhsT=ones_f.ap, rhs=sq.ap[:, j, :], start=(q4 == 0 and j == 0),
                                       stop=(q4 == 3 and j == 3))
                    return ins
                S.op('pe', f, reads=[sq.k(), ones_f.k()], writes=[ps2.k()])
            mean, msq, rstd, nmr = tmpf[0], tmpf[1], tmpf[2], tmpf[3]
            S.op('dve', lambda e: e.tensor_scalar(out=mean.ap, in0=ps1.ap, scalar1=1.0 / D, scalar2=None, op0=ALU.mult),
                 reads=[ps1.k()], writes=[mean.k()])
            S.op('dve', lambda e: e.tensor_tensor(out=msq.ap, in0=mean.ap, in1=mean.ap, op=ALU.mult),
                 reads=[mean.k()], writes=[msq.k()])
            S.op('dve', lambda e: e.scalar_tensor_tensor(out=rstd.ap, in0=ps2.ap, scalar=1.0 / D, in1=msq.ap,
                                                         op0=ALU.mult, op1=ALU.subtract),
                 reads=[ps2.k(), msq.k()], writes=[rstd.k()])
            rsqrt_chain(rstd, rstd.ap, rstd.k(), EPS)
            S.op('dve', lambda e: e.scalar_tensor_tensor(out=nmr.ap, in0=mean.ap, scalar=-1.0, in1=rstd.ap,
                                                         op0=ALU.mult, op1=ALU.mult),
                 reads=[mean.k(), rstd.k()], writes=[nmr.k()])
            for c in range(16):
                S.op('pool', lambda e, c=c: e.tensor_tensor(out=ybuf.ap[:, c, :], in0=ybuf.ap[:, c, :], in1=rstd.ap, op=ALU.mult),
                     reads=[ybuf.k(c), rstd.k()], writes=[ybuf.k(c)])
            for c in range(16):
                S.op('pool', lambda e, c=c: e.tensor_tensor(out=ybuf.ap[:, c, :], in0=ybuf.ap[:, c, :], in1=nmr.ap, op=ALU.add),
                     reads=[ybuf.k(c), nmr.k()], writes=[ybuf.k(c)])
            for c in range(16):
                S.op('act', lambda e, c=c: e.activation(out=su.ap[:, c, :], in_=ybuf.ap[:, c, :], func=AF.Silu,
                                                        bias=V(LNB + c), scale=V(LNG + c)),
                     reads=[ybuf.k(c), vecs.k()], writes=[su.k(c)])
            for j in range(4):
                wc = load_w('w_co', j, 'kc')
                wg = load_w('w_in', 20 + j, 'kc')
                for sub in range(4):
                    dc = 4 * j + sub
                    pc = pj.next()
                    pg = pj.next()
                    proj(wc, sub, su, pc)
                    proj(wg, sub, hT, pg)
                    sg = tmpf[4 + (dc % 2)]
                    S.op('act', lambda e, sg=sg, pg=pg: e.activation(out=sg.ap, in_=pg.ap, func=AF.Sigmoid),
                         reads=[pg.k()], writes=[sg.k()])
                    S.op('dve', lambda e, dc=dc, sg=sg, pc=pc: e.tensor_tensor(out=mc.ap[:, dc, :], in0=pc.ap, in1=sg.ap, op=ALU.mult),
                         reads=[pc.k(), sg.k()], writes=[mc.k(dc)])
            for j in range(4):
                wv = load_w('w_in', 8 + j, 'kc')
                for sub in range(4):
                    h = 4 * j + sub
                    ps = pj.next()
                    proj(wv, sub, hT, ps)
                    qk_norm(ps, QG, qnT.ap[:, h, :], qnT.k(h))
            RQ = [(0, 128, 512), (0, 256, 384), (0, 256, 256), (0, 256, 128), (0, 256, 0), (128, 128, 0)]
            for h in range(16):
                pt2 = PT2[h % 2]
                et = etmp[h % 2]
                Eh = Etab2[h % 2]
                S.dma('sp', et.ap, tb_d[:, h, :], writes=[et.k()], key=('et', h % 2))
                S.op('act', lambda e, et=et, Eh=Eh: e.activation(out=Eh.ap, in_=et.ap, func=AF.Exp),
                     reads=[et.k()], writes=[Eh.k()])
                S.op('dve', lambda e, Eh=Eh: e.memset(Eh.ap[0:64, 576:640], 0.0), writes=[Eh.k()])
                S.op('dve', lambda e, Eh=Eh: e.memset(Eh.ap[64:128, 0:64], 0.0), writes=[Eh.k()])
                for r in range(6):
                    q0, nq, qrel0 = RQ[r]
                    tsrc = ti - 2 + r // 2
                    ksl = tsrc % 3
                    kh = r % 2
                    gidx = 2 * (ti - 2) + r
                    ps = pscore.next()
                    kkey = knT.ke((h * 3 + ksl) * TT + kh * 128, (h * 3 + ksl) * TT + kh * 128 + 128)
                    S.op('pe', lambda e, ps=ps, h=h, ksl=ksl, kh=kh, q0=q0, nq=nq: e.matmul(
                        ps.ap[:, 0:nq], lhsT=knT.ap[:, h, ksl, kh * 128:(kh + 1) * 128], rhs=qnT.ap[:, h, q0:q0 + nq],
                        start=True, stop=True), reads=[kkey, qnT.k(h)], writes=[ps.k()])
                    ptb = PT[r % 2]
                    S.op('act', lambda e, ps=ps, ptb=ptb, nq=nq, gidx=gidx: e.activation(
                        out=ptb.ap[:, 0:nq], in_=ps.ap[:, 0:nq], func=AF.Exp, bias=mb.ap[:, gidx:gidx + 1],
                        scale=float(np.sqrt(128.0))), reads=[ps.k(), mb.k()], writes=[ptb.k()])
                    S.op('dve', lambda e, ptb=ptb, pt2=pt2, r=r, q0=q0, nq=nq, qrel0=qrel0, Eh=Eh: e.tensor_tensor(
                        out=pt2.ap[:, r, q0:q0 + nq], in0=ptb.ap[:, 0:nq], in1=Eh.ap[:, qrel0:qrel0 + nq], op=ALU.mult),
                        reads=[ptb.k(), Eh.k()], writes=[pt2.k(r)])

                def f(e, h=h, pt2=pt2):
                    for b in range(2):
                        for r in range(b, b + 5):
                            tsrc = ti - 2 + r // 2
                            e.matmul(ppv.ap[:, b * 128:(b + 1) * 128], lhsT=vtok.ap[:, tsrc % 3, r % 2, h * 128:(h + 1) * 128],
                                     rhs=pt2.ap[:, r, b * 128:(b + 1) * 128], start=(r == b), stop=(r == b + 4))
                    for b in range(2):
                        for r in range(b, b + 5):
                            ins = e.matmul(pden.ap[:, b * 128:(b + 1) * 128], lhsT=ones_b.ap,
                                           rhs=pt2.ap[:, r, b * 128:(b + 1) * 128], start=(r == b), stop=(r == b + 4))
                    return ins
                S.op('pe', f, reads=[vtok.k(), pt2.k(), ones_b.k()], writes=[ppv.k(), pden.k()])
                rec = tmpf[0]
                S.op('dve', lambda e: e.reciprocal(out=rec.ap, in_=pden.ap), reads=[pden.k()], writes=[rec.k()])
                S.op('dve', lambda e, h=h: e.tensor_tensor(out=attnT.ap[:, h, :], in0=ppv.ap, in1=rec.ap, op=ALU.mult),
                     reads=[ppv.k(), rec.k()], writes=[attnT.k(h)])
            for j in range(4):
                wa = load_w('w_ao', j, 'kc')
                wg = load_w('w_in', 24 + j, 'kc')
                for sub in range(4):
                    dc = 4 * j + sub
                    pa = pj.next()
                    pg = pj.next()
                    proj(wa, sub, attnT, pa)
                    proj(wg, sub, hT, pg)
                    sg = tmpf[4 + (dc % 2)]
                    tt_ = tmpf[2 + (dc % 2)]
                    S.op('act', lambda e, sg=sg, pg=pg: e.activation(out=sg.ap, in_=pg.ap, func=AF.Sigmoid),
                         reads=[pg.k()], writes=[sg.k()])
                    S.op('dve', lambda e, tt_=tt_, sg=sg, pa=pa: e.tensor_tensor(out=tt_.ap, in0=pa.ap, in1=sg.ap, op=ALU.mult),
                         reads=[pa.k(), sg.k()], writes=[tt_.k()])
                    S.op('pool', lambda e, dc=dc, tt_=tt_: e.tensor_tensor(out=merged.ap[:, dc, :], in0=tt_.ap, in1=mc.ap[:, dc, :], op=ALU.add),
                         reads=[tt_.k(), mc.k(dc)], writes=[merged.k(dc)])
            for j in range(4):
                wo = load_w('w_out', j, 'kc')
                for sub in range(4):
                    dc = 4 * j + sub
                    po = pj.next()
                    proj(wo, sub, merged, po)
                    S.op('dve', lambda e, dc=dc, po=po: e.tensor_tensor(out=xt.ap[:, dc, :], in0=po.ap, in1=xt.ap[:, dc, :], op=ALU.add),
                         reads=[po.k(), xt.k(dc)], writes=[xt.k(dc)])
            if debug and ti == HALO_T:
                S.dma('sp', dbg_mid.rearrange("(c p) t -> p c t", p=128), xt.ap, reads=[xt.k()], key='dbg')

            rmsnorm_to_hT(g2s)
            for j in range(4):
                wq = load_w('w_q', j, 'kc')
                for sub in range(4):
                    cc = 4 * j + sub
                    pq = pj.next()
                    proj(wq, sub, hT, pq)
                    S.op('act', lambda e, cc=cc, pq=pq: e.activation(out=pqT.ap[:, cc, :], in_=pq.ap, func=AF.Copy),
                         reads=[pq.k()], writes=[pqT.k(cc)])
            v1v = v1.ap.rearrange("p (h two) k -> p h two k", two=2)
            i1v = i1f.ap.rearrange("p (h two) k -> p h two k", two=2)
            for t2 in range(2):
                for g4 in range(4):
                    pf = pfull.next()

                    def f(e, g4=g4, pf=pf, t2=t2):
                        for i in range(4):
                            cc = 4 * g4 + i
                            ins = e.matmul(pf.ap[:, i * 128:(i + 1) * 128], lhsT=pqT.ap[:, cc, t2 * 128:(t2 + 1) * 128],
                                           rhs=k12.ap[:, (cc % 2) * 128:(cc % 2) * 128 + 128], start=True, stop=True)
                        return ins
                    S.op('pe', f, reads=[pqT.k(4 * g4, 4 * g4 + 4), k12.k()], writes=[pf.k()])
                    S.op('act', lambda e, g4=g4, pf=pf: e.activation(out=sc.ap[:, 4 * g4:4 * g4 + 4, :],
                                                                     in_=pf.ap.rearrange("p (a b) -> p a b", a=4), func=AF.Copy),
                         reads=[pf.k()], writes=[sc.k(4 * g4, 4 * g4 + 4)])
                for cc in range(16):
                    S.op('dve', lambda e, cc=cc: e.max(out=v1.ap[:, cc, 0:8], in_=sc.ap[:, cc, :]),
                         reads=[sc.k(cc)], writes=[v1.ke(cc * 16, cc * 16 + 8)])
                for cc in range(16):
                    S.op('dve', lambda e, cc=cc: e.max_index(out=i1u.ap[:, cc, 0:8], in_max=v1.ap[:, cc, 0:8], in_values=sc.ap[:, cc, :]),
                         reads=[sc.k(cc), v1.ke(cc * 16, cc * 16 + 8)], writes=[i1u.ke(cc * 16, cc * 16 + 8)])
                for cc in range(16):
                    S.op('dve', lambda e, cc=cc: e.match_replace(out=sc.ap[:, cc, :], in_to_replace=v1.ap[:, cc, 0:8],
                                                                 in_values=sc.ap[:, cc, :], imm_value=-1e30),
                         reads=[sc.k(cc), v1.ke(cc * 16, cc * 16 + 8)], writes=[sc.k(cc)])
                for cc in range(16):
                    S.op('dve', lambda e, cc=cc: e.max(out=v1.ap[:, cc, 8:16], in_=sc.ap[:, cc, :]),
                         reads=[sc.k(cc)], writes=[v1.ke(cc * 16 + 8, cc * 16 + 16)])
                for cc in range(16):
                    S.op('dve', lambda e, cc=cc: e.max_index(out=i1u.ap[:, cc, 8:16], in_max=v1.ap[:, cc, 8:16], in_values=sc.ap[:, cc, :]),
                         reads=[sc.k(cc), v1.ke(cc * 16 + 8, cc * 16 + 16)], writes=[i1u.ke(cc * 16 + 8, cc * 16 + 16)])
                S.op('dve', lambda e: e.tensor_copy(out=i1f.ap, in_=i1u.ap), reads=[i1u.k()], writes=[i1f.k()])
                cand4 = cand.ap.rearrange("p h (a b) -> p h a b", a=16)
                S.op('dve', lambda e: e.tensor_tensor(out=cand4, in0=v1v[:, :, 0, :].unsqueeze(3).to_broadcast([128, 8, 16, 16]),
                                                      in1=v1v[:, :, 1, :].unsqueeze(2).to_broadcast([128, 8, 16, 16]), op=ALU.add),
                     reads=[v1.k()], writes=[cand.k()])
                for h in range(8):
                    S.op('dve', lambda e, h=h: e.max(out=topv.ap[:, h, 0:8], in_=cand.ap[:, h, :]),
                         reads=[cand.k(h)], writes=[topv.ke(h * 16, h * 16 + 8)])
                for h in range(8):
                    S.op('dve', lambda e, h=h: e.max_index(out=flatu.ap[:, h, 0:8], in_max=topv.ap[:, h, 0:8], in_values=cand.ap[:, h, :]),
                         reads=[cand.k(h), topv.ke(h * 16, h * 16 + 8)], writes=[flatu.ke(h * 16, h * 16 + 8)])
                for h in range(8):
                    S.op('dve', lambda e, h=h: e.match_replace(out=cand.ap[:, h, :], in_to_replace=topv.ap[:, h, 0:8],
                                                               in_values=cand.ap[:, h, :], imm_value=-1e30),
                         reads=[cand.k(h), topv.ke(h * 16, h * 16 + 8)], writes=[cand.k(h)])
                for h in range(8):
                    S.op('dve', lambda e, h=h: e.max(out=topv.ap[:, h, 8:16], in_=cand.ap[:, h, :]),
                         reads=[cand.k(h)], writes=[topv.ke(h * 16 + 8, h * 16 + 16)])
                for h in range(8):
                    S.op('dve', lambda e, h=h: e.max_index(out=flatu.ap[:, h, 8:16], in_max=topv.ap[:, h, 8:16], in_values=cand.ap[:, h, :]),
                         reads=[cand.k(h), topv.ke(h * 16 + 8, h * 16 + 16)], writes=[flatu.ke(h * 16 + 8, h * 16 + 16)])
                S.op('dve', lambda e: e.tensor_single_scalar(out=au.ap, in_=flatu.ap, scalar=4, op=ALU.logical_shift_right),
                     reads=[flatu.k()], writes=[au.k()])
                S.op('dve', lambda e: e.tensor_single_scalar(out=bu.ap, in_=flatu.ap, scalar=15, op=ALU.bitwise_and),
                     reads=[flatu.k()], writes=[bu.k()])
                S.op('dve', lambda e: e.tensor_copy(out=af.ap, in_=au.ap), reads=[au.k()], writes=[af.k()])
                S.op('dve', lambda e: e.tensor_copy(out=bf_.ap, in_=bu.ap), reads=[bu.k()], writes=[bf_.k()])
                io4 = iota16.ap.unsqueeze(1).unsqueeze(1).to_broadcast([128, 8, 16, 16])
                for (src, which, dst) in ((af, 0, e1f), (bf_, 1, e2f)):
                    S.op('dve', lambda e, src=src: e.tensor_tensor(out=oh4.ap, in0=src.ap.unsqueeze(3).to_broadcast([128, 8, 16, 16]),
                                                                   in1=io4, op=ALU.is_equal),
                         reads=[src.k(), iota16.k()], writes=[oh4.k()])
                    S.op('dve', lambda e, which=which: e.tensor_tensor(out=oh4.ap, in0=oh4.ap,
                                                                       in1=i1v[:, :, which, :].unsqueeze(2).to_broadcast([128, 8, 16, 16]),
                                                                       op=ALU.mult),
                         reads=[oh4.k(), i1f.k()], writes=[oh4.k()])
                    S.op('dve', lambda e, dst=dst: e.tensor_reduce(out=dst.ap, in_=oh4.ap, axis=AX.X, op=ALU.add),
                         reads=[oh4.k()], writes=[dst.k()])
                S.op('dve', lambda e: e.tensor_tensor(out=gat.ap, in0=topv.ap, in1=topv.ap[:, :, 0:1].to_broadcast([128, 8, 16]),
                                                      op=ALU.subtract), reads=[topv.k()], writes=[gat.k()])
                S.op('act', lambda e: e.activation(out=gat.ap, in_=gat.ap, func=AF.Exp), reads=[gat.k()], writes=[gat.k()])
                S.op('dve', lambda e: e.tensor_reduce(out=gsum.ap, in_=gat.ap, axis=AX.X, op=ALU.add),
                     reads=[gat.k()], writes=[gsum.k()])
                S.op('dve', lambda e: e.reciprocal(out=gsum.ap, in_=gsum.ap), reads=[gsum.k()], writes=[gsum.k()])
                S.op('dve', lambda e: e.tensor_tensor(out=gat.ap, in0=gat.ap, in1=gsum.ap.unsqueeze(2).to_broadcast([128, 8, 16]),
                                                      op=ALU.mult), reads=[gat.k(), gsum.k()], writes=[gat.k()])
                ptr = pfull2

                def f(e):
                    e.transpose(out=ptr.ap[:, 0:128], in_=e1f.ap.rearrange("p h k -> p (h k)"), identity=ident.ap)
                    e.transpose(out=ptr.ap[:, 128:256], in_=e2f.ap.rearrange("p h k -> p (h k)"), identity=ident.ap)
                    return e.transpose(out=ptr.ap[:, 256:384], in_=gat.ap.rearrange("p h k -> p (h k)"), identity=ident.ap)
                S.op('pe', f, reads=[e1f.k(), e2f.k(), gat.k(), ident.k()], writes=[ptr.k()])
                for i, dstb in enumerate((Ism, Jsm, gsm)):
                    S.op('act', lambda e, i=i, dstb=dstb, t2=t2: e.activation(out=dstb.ap[:, t2 * 128:(t2 + 1) * 128],
                                                                              in_=ptr.ap[:, i * 128:(i + 1) * 128], func=AF.Copy),
                         reads=[ptr.k()], writes=[dstb.ke(t2 * 128, (t2 + 1) * 128)])

            Gs = [Gsub, Gsub2]

            def gbuild_batch(eg, tb, G):
                oj, cb, cf = OJb[tb % 2], Cb[tb % 2], Cf[tb % 2]
                S.op('dve', lambda e: e.tensor_tensor(
                    out=oj.ap, in0=iota128b.ap.unsqueeze(1).to_broadcast([128, 16, 128]),
                    in1=Jsm.ap[:, tb * 16:(tb + 1) * 16].unsqueeze(2).to_broadcast([128, 16, 128]), op=ALU.is_equal),
                    reads=[iota128b.k(), Jsm.k()], writes=[oj.k()])
                S.op('dve', lambda e: e.tensor_tensor(
                    out=cf.ap, in0=iota128b.ap[:, eg * EG:(eg + 1) * EG].unsqueeze(1).to_broadcast([128, 16, EG]),
                    in1=Ism.ap[:, tb * 16:(tb + 1) * 16].unsqueeze(2).to_broadcast([128, 16, EG]), op=ALU.is_equal),
                    reads=[iota128b.k(), Ism.k()], writes=[cf.k()])
                S.op('pool', lambda e: e.tensor_tensor(
                    out=cb.ap, in0=cf.ap, in1=gsm.ap[:, tb * 16:(tb + 1) * 16].unsqueeze(2).to_broadcast([128, 16, EG]),
                    op=ALU.mult), reads=[cf.k(), gsm.k()], writes=[cb.k()])
                pf = pfull.next()

                def f(e):
                    for t in range(16):
                        ins = e.matmul(pf.ap[:, t * EG:(t + 1) * EG], lhsT=oj.ap[:, t, :], rhs=cb.ap[:, t, :],
                                       start=True, stop=True)
                    return ins
                S.op('pe', f, reads=[oj.k(), cb.k()], writes=[pf.k()])
                S.op('act', lambda e: e.activation(out=G.ap[:, :, tb * 16:(tb + 1) * 16],
                                                   in_=pf.ap.rearrange("p (t e) -> p e t", e=EG), func=AF.Copy),
                     reads=[pf.k()], writes=[G.k()])

            NEG = NEXP_CH // EG
            NB = TT // 16
            NCGE = EG // CG
            NCG = NEG * NCGE

            def dense_A(i):
                eg, cg = divmod(i, NCGE)
                G = Gs[eg % 2]
                wu = load_w('uT', i, 'kc', wi=0)
                wvv = load_w('ev', i, 'ec', wi=1 + (i % 2))
                wt = wTb[i % 2]
                for cc in range(CG):
                    pa = pj.next()
                    proj(wu, cc, hT, pa)
                    ge = gel[cc % 2]
                    S.op('act', lambda e, ge=ge, pa=pa: e.activation(out=ge.ap, in_=pa.ap, func=AF.Gelu),
                         reads=[pa.k()], writes=[ge.k()])
                    S.op('dve', lambda e, ge=ge, cc=cc: e.tensor_tensor(
                        out=wt.ap[:, cc, :], in0=ge.ap, in1=G.ap[:, cg * CG + cc, :], op=ALU.mult),
                        reads=[ge.k(), G.k(cg * CG + cc)], writes=[wt.k(cc)])
                return wvv, wt

            def dense_Y(wvv, wt):
                for dc in range(16):
                    py = ppy.next()

                    def f(e, py=py, dc=dc):
                        for cc in range(CG):
                            ins = e.matmul(py.ap, lhsT=wvv.ap[:, cc, dc * 128:(dc + 1) * 128], rhs=wt.ap[:, cc, :],
                                           start=(cc == 0), stop=(cc == CG - 1))
                        return ins
                    S.op('pe', f, reads=[wvv.k(), wt.k()], writes=[py.k()])
                    if dc % 2 == 0:
                        S.op('dve', lambda e, dc=dc, py=py: e.tensor_tensor(out=xt.ap[:, dc, :], in0=py.ap, in1=xt.ap[:, dc, :], op=ALU.add),
                             reads=[py.k(), xt.k(dc)], writes=[xt.k(dc)])
                    else:
                        yt = ytmp[(dc // 2) % 4]
                        S.op('act', lambda e, yt=yt, py=py: e.activation(out=yt.ap, in_=py.ap, func=AF.Copy),
                             reads=[py.k()], writes=[yt.k()])
                        S.op('pool', lambda e, dc=dc, yt=yt: e.tensor_tensor(out=xt.ap[:, dc, :], in0=yt.ap, in1=xt.ap[:, dc, :], op=ALU.add),
                             reads=[yt.k(), xt.k(dc)], writes=[xt.k(dc)])

            for tb in range(NB):
                gbuild_batch(0, tb, Gs[0])
            per = NB // NCGE
            cur = dense_A(0)
            for i in range(NCG):
                eg, cg = divmod(i, NCGE)
                if eg + 1 < NEG:
                    for tb in range(cg * per, (cg + 1) * per):
                        gbuild_batch(eg + 1, tb, Gs[(eg + 1) % 2])
                nxt = dense_A(i + 1) if i + 1 < NCG else None
                dense_Y(*cur)
                cur = nxt
            S.dma('sp', yT[:, (ti - HALO_T) * TT:(ti - HALO_T + 1) * TT].rearrange("(c p) t -> p c t", p=128), xt.ap,
                  reads=[xt.k()], key='yst')
        for ti_ in range(NTILE):
            do_tile(ti_)
        S.finish('sp')
        S.emit_all()
    return nc


def _prep_shared(inputs):
    g = lambda k: np.asarray(inputs[k], dtype=np.float32)[0]
    sh = {}
    sh["w_in"] = np.ascontiguousarray(g("w_in"))
    sh["w_co"] = np.ascontiguousarray(g("w_conv_out"))
    sh["w_ao"] = np.ascontiguousarray(g("w_attn_o"))
    sh["w_out"] = np.ascontiguousarray(g("w_out"))
    sh["w_q"] = np.ascontiguousarray(g("w_query"))
    sh["uT"] = np.ascontiguousarray(g("expert_u").T)
    sh["ev"] = np.ascontiguousarray(g("expert_v"))
    fm = lambda v: v.reshape(16, 128).T
    vecs = np.zeros((128, 578), np.float32)
    vecs[:, 0:16] = fm(g("norm1_g"))
    vecs[:, 16:32] = fm(g("norm2_g"))
    vecs[:, 32:48] = fm(g("conv_dw_b"))
    vecs[:, 48:64] = fm(g("conv_ln_g"))
    vecs[:, 64:80] = fm(g("conv_ln_b"))
    vecs[:, 80] = g("q_norm_g")
    vecs[:, 81] = g("k_norm_g")
    dw = g("conv_dw_w")
    vecs[:, 82:] = dw.reshape(31, 16, 128).transpose(2, 1, 0).reshape(128, 16 * 31)
    sh["vecs"] = vecs
    kk = np.arange(128)[:, None]
    mm = np.arange(640)[None, :]
    idx = np.clip(mm - kk, -63, 128) + 63
    rb = g("rel_bias")
    sh["tb"] = np.ascontiguousarray(rb[:, idx].transpose(1, 0, 2))
    sh["k12"] = np.ascontiguousarray(np.concatenate([g("sub_keys_1").T, g("sub_keys_2").T], axis=1))
    return sh


_NC_CACHE = {}


def kernel(**inputs):
    x = np.asarray(inputs["x"], dtype=np.float32)[0]
    xTf = np.ascontiguousarray(x.T)
    sh = _prep_shared(inputs)
    halo = HALO_T * TT
    in_maps = []
    for c in range(NCORE):
        xc = np.zeros((D, halo + TOK_CORE), np.float32)
        xc[:, halo:] = xTf[:, c * TOK_CORE:(c + 1) * TOK_CORE]
        mbc = np.zeros((128, 20), np.float32)
        if c > 0:
            xc[:, :halo] = xTf[:, c * TOK_CORE - halo:c * TOK_CORE]
        else:
            mbc[:, 0:4] = -30000.0
        m = dict(sh)
        m["xT"] = xc
        m["mb"] = mbc
        in_maps.append(m)
    if "nc" not in _NC_CACHE:
        _NC_CACHE["nc"] = build_nc()
    res = run_bass_kernel_spmd(_NC_CACHE["nc"], in_maps, core_ids=list(range(NCORE)))
    out = np.empty((1, NCORE * TOK_CORE, D), np.float32)
    for c in range(NCORE):
        out[0, c * TOK_CORE:(c + 1) * TOK_CORE, :] = res.results[c]["yT"].T
    return out
```

```python
import numpy as np
from contextlib import ExitStack
import concourse.bass as bass
import concourse.mybir as mybir
from concourse.bass_utils import run_bass_kernel_spmd

F32 = mybir.dt.float32
BF16 = mybir.dt.bfloat16
U32 = mybir.dt.uint32
ALU = mybir.AluOpType
AF = mybir.ActivationFunctionType
AX = mybir.AxisListType

D = 2048
NCORE = 8
TOK_CORE = 2048
TT = 256
HALO_T = 2
NT_OWN = TOK_CORE // TT
EPS = 1e-6
NEXP_CH = 128
EG = 32
CG = 4


class Sched:
    ENG = ['pe', 'act', 'dve', 'pool', 'sp']
    EPOCH = 12000

    def __init__(self, nc, stack):
        self.nc = nc
        self.stack = stack
        self.streams = {e: [] for e in self.ENG}
        self.sem = {}
        self.cnt = {}
        self.nsem = 0
        for e in self.ENG:
            self._new_epoch(e)
        self.known = {e: {} for e in self.ENG}
        self.wr = {}
        self.rd = {}
        self.dma_sems = {}

    def _new_sem(self, name):
        self.nsem += 1
        return self.stack.enter_context(self.nc.semaphore(f"{name}_{self.nsem}"))

    def _new_epoch(self, e):
        self.sem[e] = self._new_sem(f"s_{e}")
        self.cnt[e] = 0

    def _deps(self, eng, reads, writes):
        deps = {}

        def need(ev, same_ok):
            sem, val, src = ev
            k = id(sem)
            if k not in deps or deps[k][1] < val:
                deps[k] = (sem, val)

        for (a, lo, hi) in reads:
            for (l2, h2, ev) in self.wr.get(a, ()):
                if l2 < hi and lo < h2:
                    need(ev, False)
        for (a, lo, hi) in writes:
            for (l2, h2, ev) in self.wr.get(a, ()):
                if l2 < hi and lo < h2:
                    need(ev, True)
            for (l2, h2, ev) in self.rd.get(a, ()):
                if l2 < hi and lo < h2:
                    need(ev, True)
        waits = []
        kn = self.known[eng]
        for k, (sem, val) in deps.items():
            if kn.get(k, 0) < val:
                kn[k] = val
                waits.append((sem, val))
        return waits

    def _commit(self, ev, reads, writes):
        for (a, lo, hi) in writes:
            self.wr[a] = [t for t in self.wr.get(a, ()) if not (t[0] >= lo and t[1] <= hi)]
            self.rd[a] = [t for t in self.rd.get(a, ()) if not (t[0] >= lo and t[1] <= hi)]
            self.wr[a].append((lo, hi, ev))
        for (a, lo, hi) in reads:
            lst = self.rd.setdefault(a, [])
            for i, t in enumerate(lst):
                if t[0] == lo and t[1] == hi and t[2][0] is ev[0]:
                    lst[i] = (lo, hi, ev)
                    break
            else:
                lst.append((lo, hi, ev))

    def op(self, eng, fn, reads=(), writes=()):
        reads = list(reads)
        writes = list(writes)
        waits = self._deps(eng, reads, writes)
        if self.cnt[eng] >= self.EPOCH:
            self._new_epoch(eng)
        self.cnt[eng] += 1
        mysem = self.sem[eng]
        ev = (mysem, self.cnt[eng], eng)

        def emit(e):
            for sem, val in waits:
                e.wait_ge(sem, val)
            ins = fn(e)
            ins.then_inc(mysem, 1)

        self.streams[eng].append(emit)
        self._commit(ev, reads, writes)
        return ev

    def dma(self, queue, out, in_, reads=(), writes=(), key=None):
        reads = list(reads)
        writes = list(writes)
        waits = self._deps(queue, reads, writes)
        if key not in self.dma_sems:
            self.dma_sems[key] = [self._new_sem("d"), 0]
        ent = self.dma_sems[key]
        ent[1] += 16
        sem = ent[0]
        ev = (sem, ent[1], None)

        def emit(e):
            for s, val in waits:
                e.wait_ge(s, val)
            e.dma_start(out=out, in_=in_).then_inc(sem, 16)

        self.streams[queue].append(emit)
        self._commit(ev, reads, writes)
        return ev

    def finish(self, eng='sp'):
        waits = [(ent[0], ent[1]) for ent in self.dma_sems.values()]
        for e in self.ENG:
            if e != eng and self.cnt[e] > 0:
                waits.append((self.sem[e], self.cnt[e]))

        def emit(e):
            for s, val in waits:
                e.wait_ge(s, val)

        self.streams[eng].append(emit)

    def emit_all(self):
        with self.nc.Block() as block:
            @block.tensor
            def _(e):
                for f in self.streams['pe']:
                    f(e)

            @block.scalar
            def _(e):
                for f in self.streams['act']:
                    f(e)

            @block.vector
            def _(e):
                for f in self.streams['dve']:
                    f(e)

            @block.gpsimd
            def _(e):
                for f in self.streams['pool']:
                    f(e)

            @block.sync
            def _(e):
                for f in self.streams['sp']:
                    f(e)


class Buf:
    def __init__(self, arena_name, ap, off, dt_bytes):
        self.an = arena_name
        self.ap = ap
        self.off = off
        self.eb = dt_bytes
        sh = ap.shape[1:]
        self.n = int(np.prod(sh))
        self.inner = int(np.prod(sh[1:])) if len(sh) > 1 else 1

    def k(self, i=None, j=None):
        if i is None:
            return (self.an, self.off, self.off + self.n * self.eb)
        if j is None:
            j = i + 1
        return (self.an, self.off + i * self.inner * self.eb, self.off + j * self.inner * self.eb)

    def ke(self, lo, hi):
        return (self.an, self.off + lo * self.eb, self.off + hi * self.eb)


def build_nc(nt_own=NT_OWN, debug=False):
    nc = bass.Bass("TRN2", target_bir_lowering=False)
    NTILE = HALO_T + nt_own
    NTOK = NTILE * TT
    xT = nc.dram_tensor("xT", [D, (HALO_T + NT_OWN) * TT], F32, kind="ExternalInput").ap()
    w_in = nc.dram_tensor("w_in", [D, 14336], F32, kind="ExternalInput").ap()
    w_co = nc.dram_tensor("w_co", [D, D], F32, kind="ExternalInput").ap()
    w_ao = nc.dram_tensor("w_ao", [D, D], F32, kind="ExternalInput").ap()
    w_out = nc.dram_tensor("w_out", [D, D], F32, kind="ExternalInput").ap()
    w_q = nc.dram_tensor("w_q", [D, D], F32, kind="ExternalInput").ap()
    uT = nc.dram_tensor("uT", [D, 16384], F32, kind="ExternalInput").ap()
    ev_d = nc.dram_tensor("ev", [16384, D], F32, kind="ExternalInput").ap()
    vecs_d = nc.dram_tensor("vecs", [128, 578], F32, kind="ExternalInput").ap()
    tb_d = nc.dram_tensor("tb", [128, 16, 640], F32, kind="ExternalInput").ap()
    mb_d = nc.dram_tensor("mb", [128, 20], F32, kind="ExternalInput").ap()
    k12_d = nc.dram_tensor("k12", [128, 256], F32, kind="ExternalInput").ap()
    yT = nc.dram_tensor("yT", [D, NT_OWN * TT], F32, kind="ExternalOutput").ap()
    NBLK = 28 + 16 + 64
    wsc = nc.dram_tensor("wsc", [NBLK, 128, 8192], BF16, kind="Internal").ap()
    if debug:
        dbg_mid = nc.dram_tensor("dbg_mid", [D, TT], F32, kind="ExternalOutput").ap()

    with ExitStack() as st:
        S = Sched(nc, st)

        def sb(name, shape, dt):
            t = st.enter_context(nc.sbuf_tensor("s_" + name, shape, dt))
            eb = 4 if dt in (F32, U32) else 2
            return Buf(name, t[:], 0, eb)

        xt = sb("xt", [128, 16, TT], F32)
        hT = sb("hT", [128, 16, TT], BF16)
        knT = sb("knT", [128, 16, 3, TT], BF16)
        vtok = sb("vtok", [128, 3, 2, D], BF16)
        Etab2 = [sb(f"Etab{i}", [128, 640], BF16) for i in range(2)]
        NWB = 3
        wbt = [sb(f"wb{i}", [128, 8192], BF16) for i in range(NWB)]
        ubuf = sb("ubuf", [128, 16, 30 + TT], BF16)
        vecs = sb("vecs", [128, 578], F32)
        mb = sb("mb", [128, 20], F32)
        k12 = sb("k12", [128, 256], BF16)
        ones_f = sb("ones_f", [128, 128], F32)
        ones_b = sb("ones_b", [128, 128], BF16)
        ident = sb("ident", [128, 128], F32)
        iota128 = sb("iota128", [128, 128], F32)
        iota16 = sb("iota16", [128, 16], F32)
        g1s = sb("g1s", [128, 16], F32)
        g2s = sb("g2s", [128, 16], F32)

        SCR_BYTES = 60 * 1024
        scr_t = st.enter_context(nc.sbuf_tensor("scr", [128, SCR_BYTES // 2], BF16))

        def scr(off, shape, dt):
            eb = 4 if dt in (F32, U32) else 2
            n = int(np.prod(shape))
            assert off % 4 == 0 and off + n * eb <= SCR_BYTES, (off, shape)
            ap = scr_t[:, off // 2: off // 2 + n * eb // 2]
            if dt != BF16:
                ap = ap.bitcast(dt)
            if len(shape) == 2:
                ap = ap.rearrange("p (a b) -> p a b", a=shape[0])
            elif len(shape) == 3:
                ap = ap.rearrange("p (a b c) -> p a b c", a=shape[0], b=shape[1])
            return Buf("scr", ap, off, eb)

        K = 1024
        ybuf = scr(0, [16, TT], F32)
        qnT = scr(0, [16, TT], BF16)
        attnT = scr(8 * K, [16, TT], BF16)
        su = scr(16 * K, [16, TT], BF16)
        merged = scr(16 * K, [16, TT], BF16)
        mc = scr(24 * K, [16, TT], BF16)
        PT = [scr(32 * K + i * K, [TT], F32) for i in range(2)]
        PT2 = [scr(34 * K + i * 3 * K, [6, TT], BF16) for i in range(2)]
        tmpf = [scr(40 * K + i * K, [TT], F32) for i in range(6)]
        sqt = [scr(46 * K + i * 4 * K, [4, TT], F32) for i in range(2)]
        etmp = [scr(54 * K + i * 2560, [640], F32) for i in range(2)]
        pqT = scr(0, [16, TT], BF16)
        sc = scr(8 * K, [16, 128], F32)
        Gsub = scr(16 * K, [EG, TT], BF16)
        Gsub2 = scr(0, [EG, TT], BF16)
        cand = scr(32 * K, [8, 256], F32)
        wk = [scr(40 * K + i * K, [256], F32) for i in range(2)]
        gel = [scr(42 * K + i * K, [TT], F32) for i in range(2)]
        v1 = scr(44 * K, [16, 16], F32)
        i1u = scr(45 * K, [16, 16], U32)
        i1f = scr(46 * K, [16, 16], F32)
        topv = scr(47 * K, [8, 16], F32)
        flatu = scr(47 * K + 512, [8, 16], U32)
        af = scr(48 * K, [8, 16], F32)
        bf_ = scr(48 * K + 512, [8, 16], F32)
        oh4 = scr(49 * K, [8, 16, 16], F32)
        e1f = scr(57 * K, [8, 16], F32)
        e2f = scr(57 * K + 512, [8, 16], F32)
        gat = scr(58 * K, [8, 16], F32)
        gsum = scr(58 * K + 512, [8], F32)
        au = scr(58 * K + 512 + 64, [8, 16], U32)
        bu = scr(59 * K + 64, [8, 16], U32)
        ytmp = [scr(54 * K + i * K, [TT], F32) for i in range(4)]
        Ism = sb("Ism", [128, TT], BF16)
        Jsm = sb("Jsm", [128, TT], BF16)
        iota128b = sb("iota128b", [128, 128], BF16)
        gsm = sb("gsm", [128, TT], F32)
        OJb = [scr(32 * K + i * 4 * K, [16, 128], BF16) for i in range(2)]
        Cb = [scr(44 * K + i * K, [16, 32], BF16) for i in range(2)]
        Cf = [scr(46 * K + i * 2 * K, [16, 32], BF16) for i in range(2)]
        wTb = [scr(50 * K + i * 2 * K, [CG, TT], BF16) for i in range(2)]

        pst = [st.enter_context(nc.psum_tensor(f"ps{i}", [128, 512], F32)) for i in range(8)]

        class PBuf(Buf):
            def __init__(self, bank, ap):
                Buf.__init__(self, "ps", ap, bank * 2048, 4)
                self.bank = bank

            def k(self, i=None, j=None):
                return ("ps", self.bank * 2048, (self.bank + 1) * 2048)

        def psh(bank, half):
            return PBuf(bank, pst[bank][:, half * 256:half * 256 + 256])

        def psf(b):
            return PBuf(b, pst[b][:, :])

        class Rot:
            def __init__(self, items):
                self.items = items
                self.i = 0

            def next(self):
                r = self.items[self.i % len(self.items)]
                self.i += 1
                return r

        pj = Rot([psh(0, 0), psh(1, 0), psh(2, 0)])
        pstat = Rot([psh(3, 0), psh(3, 1)])
        pscore = Rot([psh(4, 0), psh(5, 0)])
        ppv = psh(6, 0)
        pden = psh(6, 1)
        pfull = Rot([psf(6), psf(7)])
        pfull2 = psf(5)
        ppy = Rot([psh(3, 0), psh(4, 0), psh(5, 0)])
        wrot = Rot(list(range(NWB)))

        V = lambda c0, n=1: vecs.ap[:, c0:c0 + n]
        N1G, N2G, DWB, LNG, LNB, QG, KG, DWW = 0, 16, 32, 48, 64, 80, 81, 82

        S.dma('sp', vecs.ap, vecs_d, writes=[vecs.k()], key='c0')
        S.dma('sp', mb.ap, mb_d, writes=[mb.k()], key='c1')
        S.dma('pool', k12.ap, k12_d, writes=[k12.k()], key='c2')
        S.op('dve', lambda e: e.memset(ones_f.ap, 1.0), writes=[ones_f.k()])
        S.op('dve', lambda e: e.memset(ones_b.ap, 1.0), writes=[ones_b.k()])
        S.op('dve', lambda e: e.memset(ubuf.ap, 0.0), writes=[ubuf.k()])
        S.op('pool', lambda e: e.iota(ident.ap, pattern=[[1, 128]], base=0, channel_multiplier=-1,
                                      allow_small_or_imprecise_dtypes=True), writes=[ident.k()])
        S.op('dve', lambda e: e.tensor_scalar(out=ident.ap, in0=ident.ap, scalar1=0.0, scalar2=None,
                                              op0=ALU.is_equal), reads=[ident.k()], writes=[ident.k()])
        S.op('pool', lambda e: e.iota(iota128.ap, pattern=[[1, 128]], base=0, channel_multiplier=0,
                                      allow_small_or_imprecise_dtypes=True), writes=[iota128.k()])
        S.op('dve', lambda e: e.tensor_copy(out=iota128b.ap, in_=iota128.ap), reads=[iota128.k()], writes=[iota128b.k()])
        S.op('pool', lambda e: e.iota(iota16.ap, pattern=[[1, 16]], base=0, channel_multiplier=0,
                                      allow_small_or_imprecise_dtypes=True), writes=[iota16.k()])
        S.op('dve', lambda e: e.tensor_scalar(out=g1s.ap, in0=V(N1G, 16), scalar1=float(np.sqrt(D)), scalar2=None,
                                              op0=ALU.mult), reads=[vecs.k()], writes=[g1s.k()])
        S.op('dve', lambda e: e.tensor_scalar(out=g2s.ap, in0=V(N2G, 16), scalar1=float(np.sqrt(D)), scalar2=None,
                                              op0=ALU.mult), reads=[vecs.k()], writes=[g2s.k()])
        blk_id = {}
        NCVSEM = 56
        CV_AHEAD = 4
        CV_LOOK = 8

        conv_list = []
        conv_done = [0]

        def conv_blk(name, src_ap, j, pattern):
            b = len(blk_id)
            blk_id[(name, j)] = b
            conv_list.append((src_ap, pattern))

        def ensure_conv(upto):
            while conv_done[0] < min(upto, len(conv_list)):
                b = conv_done[0]
                src_ap, pattern = conv_list[b]
                c = 16 if pattern == 'kc' else 4
                S.dma('pool', wsc[b].rearrange("p (c n) -> p c n", c=c), src_ap.rearrange("(c p) n -> p c n", p=128),
                      reads=([('wsc', b - CV_AHEAD, b - CV_AHEAD + 1)] if b >= CV_AHEAD else []),
                      writes=[('wsc', b, b + 1), ('cvslot', b % NCVSEM, b % NCVSEM + 1)], key=('cv', b % NCVSEM))
                conv_done[0] += 1

        def conv_in(j0):
            for j in range(4):
                conv_blk('w_in', w_in[:, j0 + 512 * j:j0 + 512 * (j + 1)], (j0 // 512) + j, 'kc')

        conv_in(6144)
        conv_in(8192)
        for j in range(4):
            conv_blk('w_in', w_in[:, 512 * j:512 * (j + 1)], j, 'kc')
            conv_blk('w_in', w_in[:, 2048 + 512 * j:2048 + 512 * (j + 1)], 4 + j, 'kc')
        for j in range(4):
            conv_blk('w_co', w_co[:, 512 * j:512 * (j + 1)], j, 'kc')
            conv_blk('w_in', w_in[:, 10240 + 512 * j:10240 + 512 * (j + 1)], 20 + j, 'kc')
        conv_in(4096)
        for j in range(4):
            conv_blk('w_ao', w_ao[:, 512 * j:512 * (j + 1)], j, 'kc')
            conv_blk('w_in', w_in[:, 12288 + 512 * j:12288 + 512 * (j + 1)], 24 + j, 'kc')
        for j in range(4):
            conv_blk('w_out', w_out[:, 512 * j:512 * (j + 1)], j, 'kc')
        for j in range(4):
            conv_blk('w_q', w_q[:, 512 * j:512 * (j + 1)], j, 'kc')
        for j in range(NEXP_CH // CG):
            conv_blk('uT', uT[:, 512 * j:512 * (j + 1)], j, 'kc')
            conv_blk('ev', ev_d[512 * j:512 * (j + 1), :], j, 'ec')
        assert len(blk_id) == NBLK

        def load_w(name, j, rows_pattern, wi=None):
            if wi is None:
                wi = wrot.next()
            wb = wbt[wi]
            b = blk_id[(name, j)]
            ensure_conv(b + 1 + CV_LOOK)
            c = 16 if rows_pattern == 'kc' else 4
            view = Buf(wb.an, wb.ap.rearrange("p (c n) -> p c n", c=c), 0, 2)
            S.dma('sp', wb.ap, wsc[b], reads=[('wsc', b, b + 1)], writes=[wb.k()], key=('wb', wi))
            return view

        def proj(wv, sub, rhsbuf, ps):
            def f(e):
                for c in range(16):
                    ins = e.matmul(ps.ap, lhsT=wv.ap[:, c, sub * 128:(sub + 1) * 128], rhs=rhsbuf.ap[:, c, :],
                                   start=(c == 0), stop=(c == 15))
                return ins
            S.op('pe', f, reads=[wv.k(), rhsbuf.k()], writes=[ps.k()])

        def rsqrt_chain(dst, src_ap, src_key, addc):
            S.op('dve', lambda e: e.tensor_scalar(out=dst.ap, in0=src_ap, scalar1=float(addc), scalar2=None, op0=ALU.add),
                 reads=[src_key], writes=[dst.k()])
            S.op('act', lambda e: e.activation(out=dst.ap, in_=dst.ap, func=AF.Sqrt), reads=[dst.k()], writes=[dst.k()])
            S.op('dve', lambda e: e.reciprocal(out=dst.ap, in_=dst.ap), reads=[dst.k()], writes=[dst.k()])

        def rmsnorm_to_hT(gs):
            pss = pstat.next()
            for q4 in range(4):
                sq = sqt[q4 % 2]
                S.op('act', lambda e, q4=q4, sq=sq: e.activation(out=sq.ap, in_=xt.ap[:, 4 * q4:4 * q4 + 4, :], func=AF.Square),
                     reads=[xt.k(4 * q4, 4 * q4 + 4)], writes=[sq.k()])

                def f(e, q4=q4, sq=sq):
                    for j in range(4):
                        ins = e.matmul(pss.ap, lhsT=ones_f.ap, rhs=sq.ap[:, j, :], start=(q4 == 0 and j == 0),
                                       stop=(q4 == 3 and j == 3))
                    return ins
                S.op('pe', f, reads=[sq.k(), ones_f.k()], writes=[pss.k()])
            rs = tmpf[0]
            rsqrt_chain(rs, pss.ap, pss.k(), D * EPS)
            for c in range(16):
                S.op('dve', lambda e, c=c: e.scalar_tensor_tensor(out=hT.ap[:, c, :], in0=xt.ap[:, c, :], scalar=gs.ap[:, c:c + 1],
                                                                 in1=rs.ap, op0=ALU.mult, op1=ALU.mult),
                     reads=[xt.k(c), rs.k(), gs.k()], writes=[hT.k(c)])

        def qk_norm(ps, gcol, out_ap, out_key):
            sq = tmpf[1]
            S.op('act', lambda e: e.activation(out=sq.ap, in_=ps.ap, func=AF.Square), reads=[ps.k()], writes=[sq.k()])
            pss = pstat.next()
            S.op('pe', lambda e: e.matmul(pss.ap, lhsT=ones_f.ap, rhs=sq.ap, start=True, stop=True),
                 reads=[sq.k(), ones_f.k()], writes=[pss.k()])
            rs = tmpf[2]
            rsqrt_chain(rs, pss.ap, pss.k(), 128 * EPS)
            S.op('dve', lambda e: e.scalar_tensor_tensor(out=out_ap, in0=ps.ap, scalar=V(gcol), in1=rs.ap,
                                                         op0=ALU.mult, op1=ALU.mult),
                 reads=[ps.k(), rs.k(), vecs.k()], writes=[out_key])

        def do_tile(ti):
            own = ti >= HALO_T
            slot = ti % 3
            S.dma('sp', xt.ap, xT[:, ti * TT:(ti + 1) * TT].rearrange("(c p) t -> p c t", p=128),
                  writes=[xt.k()], key='xt')
            rmsnorm_to_hT(g1s)

            if ti >= 1:
                for j in range(4):
                    wa = load_w('w_in', j, 'kc')
                    wg = load_w('w_in', 4 + j, 'kc')
                    for sub in range(4):
                        c = 4 * j + sub
                        pa = pj.next()
                        pg = pj.next()
                        proj(wa, sub, hT, pa)
                        proj(wg, sub, hT, pg)
                        sg = tmpf[3 + (c % 2)]
                        S.op('act', lambda e, sg=sg, pg=pg: e.activation(out=sg.ap, in_=pg.ap, func=AF.Sigmoid),
                             reads=[pg.k()], writes=[sg.k()])
                        S.op('dve', lambda e, c=c, sg=sg, pa=pa: e.tensor_tensor(out=ubuf.ap[:, c, 30:30 + TT], in0=pa.ap, in1=sg.ap, op=ALU.mult),
                             reads=[pa.k(), sg.k()], writes=[ubuf.k(c)])
            conv_q = []
            if own:
                for g4 in range(4):
                    cs = [4 * g4 + i for i in range(4)]
                    for c in cs:
                        conv_q.append(lambda c=c: S.op('dve', lambda e: e.tensor_scalar(
                            out=ybuf.ap[:, c, :], in0=ubuf.ap[:, c, 0:TT], scalar1=V(DWW + c * 31),
                            scalar2=V(DWB + c), op0=ALU.mult, op1=ALU.add),
                            reads=[ubuf.k(c), vecs.k()], writes=[ybuf.k(c)]))
                    for j in range(1, 31):
                        for c in cs:
                            conv_q.append(lambda c=c, j=j: S.op('dve', lambda e: e.scalar_tensor_tensor(
                                out=ybuf.ap[:, c, :], in0=ubuf.ap[:, c, j:j + TT],
                                scalar=V(DWW + c * 31 + j), in1=ybuf.ap[:, c, :], op0=ALU.mult, op1=ALU.add),
                                reads=[ubuf.k(c), vecs.k(), ybuf.k(c)], writes=[ybuf.k(c)]))
            conv_q.reverse()

            def conv_emit(n):
                for _ in range(n):
                    if conv_q:
                        conv_q.pop()()

            for j in range(4):
                wv = load_w('w_in', 12 + j, 'kc')
                for sub in range(4):
                    h = 4 * j + sub
                    ps = pj.next()
                    proj(wv, sub, hT, ps)
                    qk_norm(ps, KG, knT.ap[:, h, slot, :], knT.ke((h * 3 + slot) * TT, (h * 3 + slot + 1) * TT))
                    conv_emit(24)
            for j in range(4):
                wv = load_w('w_in', 16 + j, 'kc')
                for t2 in range(2):
                    pf = pfull.next()

                    def f(e, wv=wv, t2=t2, pf=pf):
                        for c in range(16):
                            ins = e.matmul(pf.ap, lhsT=hT.ap[:, c, t2 * 128:(t2 + 1) * 128], rhs=wv.ap[:, c, :],
                                           start=(c == 0), stop=(c == 15))
                        return ins
                    S.op('pe', f, reads=[wv.k(), hT.k()], writes=[pf.k()])
                    S.op('act', lambda e, t2=t2, j=j, pf=pf: e.activation(out=vtok.ap[:, slot, t2, 512 * j:512 * (j + 1)], in_=pf.ap, func=AF.Copy),
                         reads=[pf.k()], writes=[vtok.ke((slot * 2 + t2) * D + 512 * j, (slot * 2 + t2) * D + 512 * (j + 1))])
                    conv_emit(14)
            conv_emit(10000)
            if ti == 0:
                return
            if not own:
                S.op('pool', lambda e: e.tensor_copy(out=ubuf.ap[:, :, 0:30], in_=ubuf.ap[:, :, TT:TT + 30]),
                     reads=[ubuf.k()], writes=[ubuf.k()])
                return
            S.op('pool', lambda e: e.tensor_copy(out=ubuf.ap[:, :, 0:30], in_=ubuf.ap[:, :, TT:TT + 30]),
                 reads=[ubuf.k()], writes=[ubuf.k()])
            ps1 = pstat.next()
            ps2 = pstat.next()

            def f(e):
                for c in range(16):
                    ins = e.matmul(ps1.ap, lhsT=ones_f.ap, rhs=ybuf.ap[:, c, :], start=(c == 0), stop=(c == 15))
                return ins
            S.op('pe', f, reads=[ybuf.k(), ones_f.k()], writes=[ps1.k()])
            for q4 in range(4):
                sq = sqt[q4 % 2]
                S.op('act', lambda e, q4=q4, sq=sq: e.activation(out=sq.ap, in_=ybuf.ap[:, 4 * q4:4 * q4 + 4, :], func=AF.Square),
                     reads=[ybuf.k(4 * q4, 4 * q4 + 4)], writes=[sq.k()])

                def f(e, q4=q4, sq=sq):
                    for j in range(4):
                        ins = e.matmul(ps2.ap, lhsT=ones_f.ap, rhs=sq.ap[:, j, :], start=(q4 == 0 and j == 0),
                                       stop=(q4 == 3 and j == 3))
                    return ins
                S.op('pe', f, reads=[sq.k(), ones_f.k()], writes=[ps2.k()])
            mean, msq, rstd, nmr = tmpf[0], tmpf[1], tmpf[2], tmpf[3]
            S.op('dve', lambda e: e.tensor_scalar(out=mean.ap, in0=ps1.ap, scalar1=1.0 / D, scalar2=None, op0=ALU.mult),
                 reads=[ps1.k()], writes=[mean.k()])
            S.op('dve', lambda e: e.tensor_tensor(out=msq.ap, in0=mean.ap, in1=mean.ap, op=ALU.mult),
                 reads=[mean.k()], writes=[msq.k()])
            S.op('dve', lambda e: e.scalar_tensor_tensor(out=rstd.ap, in0=ps2.ap, scalar=1.0 / D, in1=msq.ap,
                                                         op0=ALU.mult, op1=ALU.subtract),
                 reads=[ps2.k(), msq.k()], writes=[rstd.k()])
            rsqrt_chain(rstd, rstd.ap, rstd.k(), EPS)
            S.op('dve', lambda e: e.scalar_tensor_tensor(out=nmr.ap, in0=mean.ap, scalar=-1.0, in1=rstd.ap,
                                                         op0=ALU.mult, op1=ALU.mult),
                 reads=[mean.k(), rstd.k()], writes=[nmr.k()])
            for c in range(16):
                S.op('pool', lambda e, c=c: e.tensor_tensor(out=ybuf.ap[:, c, :], in0=ybuf.ap[:, c, :], in1=rstd.ap, op=ALU.mult),
                     reads=[ybuf.k(c), rstd.k()], writes=[ybuf.k(c)])
            for c in range(16):
                S.op('pool', lambda e, c=c: e.tensor_tensor(out=ybuf.ap[:, c, :], in0=ybuf.ap[:, c, :], in1=nmr.ap, op=ALU.add),
                     reads=[ybuf.k(c), nmr.k()], writes=[ybuf.k(c)])
            for c in range(16):
                S.op('act', lambda e, c=c: e.activation(out=su.ap[:, c, :], in_=ybuf.ap[:, c, :], func=AF.Silu,
                                                        bias=V(LNB + c), scale=V(LNG + c)),
                     reads=[ybuf.k(c), vecs.k()], writes=[su.k(c)])
            for j in range(4):
                wc = load_w('w_co', j, 'kc')
                wg = load_w('w_in', 20 + j, 'kc')
                for sub in range(4):
                    dc = 4 * j + sub
                    pc = pj.next()
                    pg = pj.next()
                    proj(wc, sub, su, pc)
                    proj(wg, sub, hT, pg)
                    sg = tmpf[4 + (dc % 2)]
                    S.op('act', lambda e, sg=sg, pg=pg: e.activation(out=sg.ap, in_=pg.ap, func=AF.Sigmoid),
                         reads=[pg.k()], writes=[sg.k()])
                    S.op('dve', lambda e, dc=dc, sg=sg, pc=pc: e.tensor_tensor(out=mc.ap[:, dc, :], in0=pc.ap, in1=sg.ap, op=ALU.mult),
                         reads=[pc.k(), sg.k()], writes=[mc.k(dc)])
            for j in range(4):
                wv = load_w('w_in', 8 + j, 'kc')
                for sub in range(4):
                    h = 4 * j + sub
                    ps = pj.next()
                    proj(wv, sub, hT, ps)
                    qk_norm(ps, QG, qnT.ap[:, h, :], qnT.k(h))
            RQ = [(0, 128, 512), (0, 256, 384), (0, 256, 256), (0, 256, 128), (0, 256, 0), (128, 128, 0)]
            for h in range(16):
                pt2 = PT2[h % 2]
                et = etmp[h % 2]
                Eh = Etab2[h % 2]
                S.dma('sp', et.ap, tb_d[:, h, :], writes=[et.k()], key=('et', h % 2))
                S.op('act', lambda e, et=et, Eh=Eh: e.activation(out=Eh.ap, in_=et.ap, func=AF.Exp),
                     reads=[et.k()], writes=[Eh.k()])
                S.op('dve', lambda e, Eh=Eh: e.memset(Eh.ap[0:64, 576:640], 0.0), writes=[Eh.k()])
                S.op('dve', lambda e, Eh=Eh: e.memset(Eh.ap[64:128, 0:64], 0.0), writes=[Eh.k()])
                for r in range(6):
                    q0, nq, qrel0 = RQ[r]
                    tsrc = ti - 2 + r // 2
                    ksl = tsrc % 3
                    kh = r % 2
                    gidx = 2 * (ti - 2) + r
                    ps = pscore.next()
                    kkey = knT.ke((h * 3 + ksl) * TT + kh * 128, (h * 3 + ksl) * TT + kh * 128 + 128)
                    S.op('pe', lambda e, ps=ps, h=h, ksl=ksl, kh=kh, q0=q0, nq=nq: e.matmul(
                        ps.ap[:, 0:nq], lhsT=knT.ap[:, h, ksl, kh * 128:(kh + 1) * 128], rhs=qnT.ap[:, h, q0:q0 + nq],
                        start=True, stop=True), reads=[kkey, qnT.k(h)], writes=[ps.k()])
                    ptb = PT[r % 2]
                    S.op('act', lambda e, ps=ps, ptb=ptb, nq=nq, gidx=gidx: e.activation(
                        out=ptb.ap[:, 0:nq], in_=ps.ap[:, 0:nq], func=AF.Exp, bias=mb.ap[:, gidx:gidx + 1],
                        scale=float(np.sqrt(128.0))), reads=[ps.k(), mb.k()], writes=[ptb.k()])
                    S.op('dve', lambda e, ptb=ptb, pt2=pt2, r=r, q0=q0, nq=nq, qrel0=qrel0, Eh=Eh: e.tensor_tensor(
                        out=pt2.ap[:, r, q0:q0 + nq], in0=ptb.ap[:, 0:nq], in1=Eh.ap[:, qrel0:qrel0 + nq], op=ALU.mult),
                        reads=[ptb.k(), Eh.k()], writes=[pt2.k(r)])

                def f(e, h=h, pt2=pt2):
                    for b in range(2):
                        for r in range(b, b + 5):
                            tsrc = ti - 2 + r // 2
                            e.matmul(ppv.ap[:, b * 128:(b + 1) * 128], lhsT=vtok.ap[:, tsrc % 3, r % 2, h * 128:(h + 1) * 128],
                                     rhs=pt2.ap[:, r, b * 128:(b + 1) * 128], start=(r == b), stop=(r == b + 4))
                    for b in range(2):
                        for r in range(b, b + 5):
                            ins = e.matmul(pden.ap[:, b * 128:(b + 1) * 128], lhsT=ones_b.ap,
                                           rhs=pt2.ap[:, r, b * 128:(b + 1) * 128], start=(r == b), stop=(r == b + 4))
                    return ins
                S.op('pe', f, reads=[vtok.k(), pt2.k(), ones_b.k()], writes=[ppv.k(), pden.k()])
                rec = tmpf[0]
                S.op('dve', lambda e: e.reciprocal(out=rec.ap, in_=pden.ap), reads=[pden.k()], writes=[rec.k()])
                S.op('dve', lambda e, h=h: e.tensor_tensor(out=attnT.ap[:, h, :], in0=ppv.ap, in1=rec.ap, op=ALU.mult),
                     reads=[ppv.k(), rec.k()], writes=[attnT.k(h)])
            for j in range(4):
                wa = load_w('w_ao', j, 'kc')
                wg = load_w('w_in', 24 + j, 'kc')
                for sub in range(4):
                    dc = 4 * j + sub
                    pa = pj.next()
                    pg = pj.next()
                    proj(wa, sub, attnT, pa)
                    proj(wg, sub, hT, pg)
                    sg = tmpf[4 + (dc % 2)]
                    tt_ = tmpf[2 + (dc % 2)]
                    S.op('act', lambda e, sg=sg, pg=pg: e.activation(out=sg.ap, in_=pg.ap, func=AF.Sigmoid),
                         reads=[pg.k()], writes=[sg.k()])
                    S.op('dve', lambda e, tt_=tt_, sg=sg, pa=pa: e.tensor_tensor(out=tt_.ap, in0=pa.ap, in1=sg.ap, op=ALU.mult),
                         reads=[pa.k(), sg.k()], writes=[tt_.k()])
                    S.op('pool', lambda e, dc=dc, tt_=tt_: e.tensor_tensor(out=merged.ap[:, dc, :], in0=tt_.ap, in1=mc.ap[:, dc, :], op=ALU.add),
                         reads=[tt_.k(), mc.k(dc)], writes=[merged.k(dc)])
            for j in range(4):
                wo = load_w('w_out', j, 'kc')
                for sub in range(4):
                    dc = 4 * j + sub
                    po = pj.next()
                    proj(wo, sub, merged, po)
                    S.op('dve', lambda e, dc=dc, po=po: e.tensor_tensor(out=xt.ap[:, dc, :], in0=po.ap, in1=xt.ap[:, dc, :], op=ALU.add),
                         reads=[po.k(), xt.k(dc)], writes=[xt.k(dc)])
            if debug and ti == HALO_T:
                S.dma('sp', dbg_mid.rearrange("(c p) t -> p c t", p=128), xt.ap, reads=[xt.k()], key='dbg')

            rmsnorm_to_hT(g2s)
            for j in range(4):
                wq = load_w('w_q', j, 'kc')
                for sub in range(4):
                    cc = 4 * j + sub
                    pq = pj.next()
                    proj(wq, sub, hT, pq)
                    S.op('act', lambda e, cc=cc, pq=pq: e.activation(out=pqT.ap[:, cc, :], in_=pq.ap, func=AF.Copy),
                         reads=[pq.k()], writes=[pqT.k(cc)])
            v1v = v1.ap.rearrange("p (h two) k -> p h two k", two=2)
            i1v = i1f.ap.rearrange("p (h two) k -> p h two k", two=2)
            for t2 in range(2):
                for g4 in range(4):
                    pf = pfull.next()

                    def f(e, g4=g4, pf=pf, t2=t2):
                        for i in range(4):
                            cc = 4 * g4 + i
                            ins = e.matmul(pf.ap[:, i * 128:(i + 1) * 128], lhsT=pqT.ap[:, cc, t2 * 128:(t2 + 1) * 128],
                                           rhs=k12.ap[:, (cc % 2) * 128:(cc % 2) * 128 + 128], start=True, stop=True)
                        return ins
                    S.op('pe', f, reads=[pqT.k(4 * g4, 4 * g4 + 4), k12.k()], writes=[pf.k()])
                    S.op('act', lambda e, g4=g4, pf=pf: e.activation(out=sc.ap[:, 4 * g4:4 * g4 + 4, :],
                                                                     in_=pf.ap.rearrange("p (a b) -> p a b", a=4), func=AF.Copy),
                         reads=[pf.k()], writes=[sc.k(4 * g4, 4 * g4 + 4)])
                for cc in range(16):
                    S.op('dve', lambda e, cc=cc: e.max(out=v1.ap[:, cc, 0:8], in_=sc.ap[:, cc, :]),
                         reads=[sc.k(cc)], writes=[v1.ke(cc * 16, cc * 16 + 8)])
                for cc in range(16):
                    S.op('dve', lambda e, cc=cc: e.max_index(out=i1u.ap[:, cc, 0:8], in_max=v1.ap[:, cc, 0:8], in_values=sc.ap[:, cc, :]),
                         reads=[sc.k(cc), v1.ke(cc * 16, cc * 16 + 8)], writes=[i1u.ke(cc * 16, cc * 16 + 8)])
                for cc in range(16):
                    S.op('dve', lambda e, cc=cc: e.match_replace(out=sc.ap[:, cc, :], in_to_replace=v1.ap[:, cc, 0:8],
                                                                 in_values=sc.ap[:, cc, :], imm_value=-1e30),
                         reads=[sc.k(cc), v1.ke(cc * 16, cc * 16 + 8)], writes=[sc.k(cc)])
                for cc in range(16):
                    S.op('dve', lambda e, cc=cc: e.max(out=v1.ap[:, cc, 8:16], in_=sc.ap[:, cc, :]),
                         reads=[sc.k(cc)], writes=[v1.ke(cc * 16 + 8, cc * 16 + 16)])
                for cc in range(16):
                    S.op('dve', lambda e, cc=cc: e.max_index(out=i1u.ap[:, cc, 8:16], in_max=v1.ap[:, cc, 8:16], in_values=sc.ap[:, cc, :]),
                         reads=[sc.k(cc), v1.ke(cc * 16 + 8, cc * 16 + 16)], writes=[i1u.ke(cc * 16 + 8, cc * 16 + 16)])
                S.op('dve', lambda e: e.tensor_copy(out=i1f.ap, in_=i1u.ap), reads=[i1u.k()], writes=[i1f.k()])
                cand4 = cand.ap.rearrange("p h (a b) -> p h a b", a=16)
                S.op('dve', lambda e: e.tensor_tensor(out=cand4, in0=v1v[:, :, 0, :].unsqueeze(3).to_broadcast([128, 8, 16, 16]),
                                                      in1=v1v[:, :, 1, :].unsqueeze(2).to_broadcast([128, 8, 16, 16]), op=ALU.add),
                     reads=[v1.k()], writes=[cand.k()])
                for h in range(8):
                    S.op('dve', lambda e, h=h: e.max(out=topv.ap[:, h, 0:8], in_=cand.ap[:, h, :]),
                         reads=[cand.k(h)], writes=[topv.ke(h * 16, h * 16 + 8)])
                for h in range(8):
                    S.op('dve', lambda e, h=h: e.max_index(out=flatu.ap[:, h, 0:8], in_max=topv.ap[:, h, 0:8], in_values=cand.ap[:, h, :]),
                         reads=[cand.k(h), topv.ke(h * 16, h * 16 + 8)], writes=[flatu.ke(h * 16, h * 16 + 8)])
                for h in range(8):
                    S.op('dve', lambda e, h=h: e.match_replace(out=cand.ap[:, h, :], in_to_replace=topv.ap[:, h, 0:8],
                                                               in_values=cand.ap[:, h, :], imm_value=-1e30),
                         reads=[cand.k(h), topv.ke(h * 16, h * 16 + 8)], writes=[cand.k(h)])
                for h in range(8):
                    S.op('dve', lambda e, h=h: e.max(out=topv.ap[:, h, 8:16], in_=cand.ap[:, h, :]),
                         reads=[cand.k(h)], writes=[topv.ke(h * 16 + 8, h * 16 + 16)])
                for h in range(8):
                    S.op('dve', lambda e, h=h: e.max_index(out=flatu.ap[:, h, 8:16], in_max=topv.ap[:, h, 8:16], in_values=cand.ap[:, h, :]),
                         reads=[cand.k(h), topv.ke(h * 16 + 8, h * 16 + 16)], writes=[flatu.ke(h * 16 + 8, h * 16 + 16)])
                S.op('dve', lambda e: e.tensor_single_scalar(out=au.ap, in_=flatu.ap, scalar=4, op=ALU.logical_shift_right),
                     reads=[flatu.k()], writes=[au.k()])
                S.op('dve', lambda e: e.tensor_single_scalar(out=bu.ap, in_=flatu.ap, scalar=15, op=ALU.bitwise_and),
                     reads=[flatu.k()], writes=[bu.k()])
                S.op('dve', lambda e: e.tensor_copy(out=af.ap, in_=au.ap), reads=[au.k()], writes=[af.k()])
                S.op('dve', lambda e: e.tensor_copy(out=bf_.ap, in_=bu.ap), reads=[bu.k()], writes=[bf_.k()])
                io4 = iota16.ap.unsqueeze(1).unsqueeze(1).to_broadcast([128, 8, 16, 16])
                for (src, which, dst) in ((af, 0, e1f), (bf_, 1, e2f)):
                    S.op('dve', lambda e, src=src: e.tensor_tensor(out=oh4.ap, in0=src.ap.unsqueeze(3).to_broadcast([128, 8, 16, 16]),
                                                                   in1=io4, op=ALU.is_equal),
                         reads=[src.k(), iota16.k()], writes=[oh4.k()])
                    S.op('dve', lambda e, which=which: e.tensor_tensor(out=oh4.ap, in0=oh4.ap,
                                                                       in1=i1v[:, :, which, :].unsqueeze(2).to_broadcast([128, 8, 16, 16]),
                                                                       op=ALU.mult),
                         reads=[oh4.k(), i1f.k()], writes=[oh4.k()])
                    S.op('dve', lambda e, dst=dst: e.tensor_reduce(out=dst.ap, in_=oh4.ap, axis=AX.X, op=ALU.add),
                         reads=[oh4.k()], writes=[dst.k()])
                S.op('dve', lambda e: e.tensor_tensor(out=gat.ap, in0=topv.ap, in1=topv.ap[:, :, 0:1].to_broadcast([128, 8, 16]),
                                                      op=ALU.subtract), reads=[topv.k()], writes=[gat.k()])
                S.op('act', lambda e: e.activation(out=gat.ap, in_=gat.ap, func=AF.Exp), reads=[gat.k()], writes=[gat.k()])
                S.op('dve', lambda e: e.tensor_reduce(out=gsum.ap, in_=gat.ap, axis=AX.X, op=ALU.add),
                     reads=[gat.k()], writes=[gsum.k()])
                S.op('dve', lambda e: e.reciprocal(out=gsum.ap, in_=gsum.ap), reads=[gsum.k()], writes=[gsum.k()])
                S.op('dve', lambda e: e.tensor_tensor(out=gat.ap, in0=gat.ap, in1=gsum.ap.unsqueeze(2).to_broadcast([128, 8, 16]),
                                                      op=ALU.mult), reads=[gat.k(), gsum.k()], writes=[gat.k()])
                ptr = pfull2

                def f(e):
                    e.transpose(out=ptr.ap[:, 0:128], in_=e1f.ap.rearrange("p h k -> p (h k)"), identity=ident.ap)
                    e.transpose(out=ptr.ap[:, 128:256], in_=e2f.ap.rearrange("p h k -> p (h k)"), identity=ident.ap)
                    return e.transpose(out=ptr.ap[:, 256:384], in_=gat.ap.rearrange("p h k -> p (h k)"), identity=ident.ap)
                S.op('pe', f, reads=[e1f.k(), e2f.k(), gat.k(), ident.k()], writes=[ptr.k()])
                for i, dstb in enumerate((Ism, Jsm, gsm)):
                    S.op('act', lambda e, i=i, dstb=dstb, t2=t2: e.activation(out=dstb.ap[:, t2 * 128:(t2 + 1) * 128],
                                                                              in_=ptr.ap[:, i * 128:(i + 1) * 128], func=AF.Copy),
                         reads=[ptr.k()], writes=[dstb.ke(t2 * 128, (t2 + 1) * 128)])

            Gs = [Gsub, Gsub2]

            def gbuild_batch(eg, tb, G):
                oj, cb, cf = OJb[tb % 2], Cb[tb % 2], Cf[tb % 2]
                S.op('dve', lambda e: e.tensor_tensor(
                    out=oj.ap, in0=iota128b.ap.unsqueeze(1).to_broadcast([128, 16, 128]),
                    in1=Jsm.ap[:, tb * 16:(tb + 1) * 16].unsqueeze(2).to_broadcast([128, 16, 128]), op=ALU.is_equal),
                    reads=[iota128b.k(), Jsm.k()], writes=[oj.k()])
                S.op('dve', lambda e: e.tensor_tensor(
                    out=cf.ap, in0=iota128b.ap[:, eg * EG:(eg + 1) * EG].unsqueeze(1).to_broadcast([128, 16, EG]),
                    in1=Ism.ap[:, tb * 16:(tb + 1) * 16].unsqueeze(2).to_broadcast([128, 16, EG]), op=ALU.is_equal),
                    reads=[iota128b.k(), Ism.k()], writes=[cf.k()])
                S.op('pool', lambda e: e.tensor_tensor(
                    out=cb.ap, in0=cf.ap, in1=gsm.ap[:, tb * 16:(tb + 1) * 16].unsqueeze(2).to_broadcast([128, 16, EG]),
                    op=ALU.mult), reads=[cf.k(), gsm.k()], writes=[cb.k()])

                def mm():
                    pf = pfull.next()

                    def f(e):
                        for t in range(16):
                            ins = e.matmul(pf.ap[:, t * EG:(t + 1) * EG], lhsT=oj.ap[:, t, :], rhs=cb.ap[:, t, :],
                                           start=True, stop=True)
                        return ins
                    S.op('pe', f, reads=[oj.k(), cb.k()], writes=[pf.k()])
                    S.op('act', lambda e: e.activation(out=G.ap[:, :, tb * 16:(tb + 1) * 16],
                                                       in_=pf.ap.rearrange("p (t e) -> p e t", e=EG), func=AF.Copy),
                         reads=[pf.k()], writes=[G.k()])
                return mm

            NEG = NEXP_CH // EG
            NB = TT // 16
            NCGE = EG // CG
            NCG = NEG * NCGE

            def dense_A(i):
                eg, cg = divmod(i, NCGE)
                G = Gs[eg % 2]
                wu = load_w('uT', i, 'kc', wi=0)
                wvv = load_w('ev', i, 'ec', wi=1 + (i % 2))
                wt = wTb[i % 2]
                for cc in range(CG):
                    pa = pj.next()
                    proj(wu, cc, hT, pa)
                    ge = gel[cc % 2]
                    S.op('act', lambda e, ge=ge, pa=pa: e.activation(out=ge.ap, in_=pa.ap, func=AF.Gelu),
                         reads=[pa.k()], writes=[ge.k()])
                    S.op('dve', lambda e, ge=ge, cc=cc: e.tensor_tensor(
                        out=wt.ap[:, cc, :], in0=ge.ap, in1=G.ap[:, cg * CG + cc, :], op=ALU.mult),
                        reads=[ge.k(), G.k(cg * CG + cc)], writes=[wt.k(cc)])
                return wvv, wt

            def dense_Y(wvv, wt):
                for dc in range(16):
                    py = ppy.next()

                    def f(e, py=py, dc=dc):
                        for cc in range(CG):
                            ins = e.matmul(py.ap, lhsT=wvv.ap[:, cc, dc * 128:(dc + 1) * 128], rhs=wt.ap[:, cc, :],
                                           start=(cc == 0), stop=(cc == CG - 1))
                        return ins
                    S.op('pe', f, reads=[wvv.k(), wt.k()], writes=[py.k()])
                    if dc % 2 == 0:
                        S.op('dve', lambda e, dc=dc, py=py: e.tensor_tensor(out=xt.ap[:, dc, :], in0=py.ap, in1=xt.ap[:, dc, :], op=ALU.add),
                             reads=[py.k(), xt.k(dc)], writes=[xt.k(dc)])
                    else:
                        yt = ytmp[(dc // 2) % 4]
                        S.op('act', lambda e, yt=yt, py=py: e.activation(out=yt.ap, in_=py.ap, func=AF.Copy),
                             reads=[py.k()], writes=[yt.k()])
                        S.op('pool', lambda e, dc=dc, yt=yt: e.tensor_tensor(out=xt.ap[:, dc, :], in0=yt.ap, in1=xt.ap[:, dc, :], op=ALU.add),
                             reads=[yt.k(), xt.k(dc)], writes=[xt.k(dc)])

            for tb in range(NB):
                gbuild_batch(0, tb, Gs[0])()
            per = NB // NCGE
            cur = dense_A(0)
            for i in range(NCG):
                eg, cg = divmod(i, NCGE)
                mms = []
                if eg + 1 < NEG:
                    for tb in range(cg * per, (cg + 1) * per):
                        mms.append(gbuild_batch(eg + 1, tb, Gs[(eg + 1) % 2]))
                nxt = dense_A(i + 1) if (i + 1 < NCG and (i + 1) % NCGE != 0) else None
                for m in mms:
                    m()
                if i + 1 < NCG and (i + 1) % NCGE == 0:
                    nxt = dense_A(i + 1)
                dense_Y(*cur)
                cur = nxt
            S.dma('sp', yT[:, (ti - HALO_T) * TT:(ti - HALO_T + 1) * TT].rearrange("(c p) t -> p c t", p=128), xt.ap,
                  reads=[xt.k()], key='yst')
        for ti_ in range(NTILE):
            do_tile(ti_)
        S.finish('sp')
        S.emit_all()
    return nc


def _prep_shared(inputs):
    g = lambda k: np.asarray(inputs[k], dtype=np.float32)[0]
    sh = {}
    sh["w_in"] = np.ascontiguousarray(g("w_in"))
    sh["w_co"] = np.ascontiguousarray(g("w_conv_out"))
    sh["w_ao"] = np.ascontiguousarray(g("w_attn_o"))
    sh["w_out"] = np.ascontiguousarray(g("w_out"))
    sh["w_q"] = np.ascontiguousarray(g("w_query"))
    sh["uT"] = np.ascontiguousarray(g("expert_u").T)
    sh["ev"] = np.ascontiguousarray(g("expert_v"))
    fm = lambda v: v.reshape(16, 128).T
    vecs = np.zeros((128, 578), np.float32)
    vecs[:, 0:16] = fm(g("norm1_g"))
    vecs[:, 16:32] = fm(g("norm2_g"))
    vecs[:, 32:48] = fm(g("conv_dw_b"))
    vecs[:, 48:64] = fm(g("conv_ln_g"))
    vecs[:, 64:80] = fm(g("conv_ln_b"))
    vecs[:, 80] = g("q_norm_g")
    vecs[:, 81] = g("k_norm_g")
    dw = g("conv_dw_w")
    vecs[:, 82:] = dw.reshape(31, 16, 128).transpose(2, 1, 0).reshape(128, 16 * 31)
    sh["vecs"] = vecs
    kk = np.arange(128)[:, None]
    mm = np.arange(640)[None, :]
    idx = np.clip(mm - kk, -63, 128) + 63
    rb = g("rel_bias")
    sh["tb"] = np.ascontiguousarray(rb[:, idx].transpose(1, 0, 2))
    sh["k12"] = np.ascontiguousarray(np.concatenate([g("sub_keys_1").T, g("sub_keys_2").T], axis=1))
    return sh


_NC_CACHE = {}


def kernel(**inputs):
    x = np.asarray(inputs["x"], dtype=np.float32)[0]
    xTf = np.ascontiguousarray(x.T)
    sh = _prep_shared(inputs)
    halo = HALO_T * TT
    in_maps = []
    for c in range(NCORE):
        xc = np.zeros((D, halo + TOK_CORE), np.float32)
        xc[:, halo:] = xTf[:, c * TOK_CORE:(c + 1) * TOK_CORE]
        mbc = np.zeros((128, 20), np.float32)
        if c > 0:
            xc[:, :halo] = xTf[:, c * TOK_CORE - halo:c * TOK_CORE]
        else:
            mbc[:, 0:4] = -30000.0
        m = dict(sh)
        m["xT"] = xc
        m["mb"] = mbc
        in_maps.append(m)
    if "nc" not in _NC_CACHE:
        _NC_CACHE["nc"] = build_nc()
    res = run_bass_kernel_spmd(_NC_CACHE["nc"], in_maps, core_ids=list(range(NCORE)))
    out = np.empty((1, NCORE * TOK_CORE, D), np.float32)
    for c in range(NCORE):
        out[0, c * TOK_CORE:(c + 1) * TOK_CORE, :] = res.results[c]["yT"].T
    return out
```

```python
import numpy as np
from contextlib import ExitStack
import concourse.bass as bass
import concourse.mybir as mybir
from concourse.bass_utils import run_bass_kernel_spmd

F32 = mybir.dt.float32
BF16 = mybir.dt.bfloat16
U32 = mybir.dt.uint32
ALU = mybir.AluOpType
AF = mybir.ActivationFunctionType
AX = mybir.AxisListType

D = 2048
NCORE = 8
TOK_CORE = 2048
TT = 256
HALO_T = 2
NT_OWN = TOK_CORE // TT
EPS = 1e-6
NEXP_CH = 128
EG = 32
CG = 4


class Sched:
    ENG = ['pe', 'act', 'dve', 'pool', 'sp']
    EPOCH = 12000

    def __init__(self, nc, stack):
        self.nc = nc
        self.stack = stack
        self.streams = {e: [] for e in self.ENG}
        self.sem = {}
        self.cnt = {}
        self.nsem = 0
        for e in self.ENG:
            self._new_epoch(e)
        self.known = {e: {} for e in self.ENG}
        self.wr = {}
        self.rd = {}
        self.dma_sems = {}

    def _new_sem(self, name):
        self.nsem += 1
        return self.stack.enter_context(self.nc.semaphore(f"{name}_{self.nsem}"))

    def _new_epoch(self, e):
        self.sem[e] = self._new_sem(f"s_{e}")
        self.cnt[e] = 0

    def _deps(self, eng, reads, writes):
        deps = {}

        def need(ev, same_ok):
            sem, val, src = ev
            k = id(sem)
            if k not in deps or deps[k][1] < val:
                deps[k] = (sem, val)

        for (a, lo, hi) in reads:
            for (l2, h2, ev) in self.wr.get(a, ()):
                if l2 < hi and lo < h2:
                    need(ev, False)
        for (a, lo, hi) in writes:
            for (l2, h2, ev) in self.wr.get(a, ()):
                if l2 < hi and lo < h2:
                    need(ev, True)
            for (l2, h2, ev) in self.rd.get(a, ()):
                if l2 < hi and lo < h2:
                    need(ev, True)
        waits = []
        kn = self.known[eng]
        for k, (sem, val) in deps.items():
            if kn.get(k, 0) < val:
                kn[k] = val
                waits.append((sem, val))
        return waits

    def _commit(self, ev, reads, writes):
        for (a, lo, hi) in writes:
            self.wr[a] = [t for t in self.wr.get(a, ()) if not (t[0] >= lo and t[1] <= hi)]
            self.rd[a] = [t for t in self.rd.get(a, ()) if not (t[0] >= lo and t[1] <= hi)]
            self.wr[a].append((lo, hi, ev))
        for (a, lo, hi) in reads:
            lst = self.rd.setdefault(a, [])
            for i, t in enumerate(lst):
                if t[0] == lo and t[1] == hi and t[2][0] is ev[0]:
                    lst[i] = (lo, hi, ev)
                    break
            else:
                lst.append((lo, hi, ev))

    def op(self, eng, fn, reads=(), writes=()):
        reads = list(reads)
        writes = list(writes)
        waits = self._deps(eng, reads, writes)
        if self.cnt[eng] >= self.EPOCH:
            self._new_epoch(eng)
        self.cnt[eng] += 1
        mysem = self.sem[eng]
        ev = (mysem, self.cnt[eng], eng)

        def emit(e):
            for sem, val in waits:
                e.wait_ge(sem, val)
            ins = fn(e)
            ins.then_inc(mysem, 1)

        self.streams[eng].append(emit)
        self._commit(ev, reads, writes)
        return ev

    def dma(self, queue, out, in_, reads=(), writes=(), key=None):
        reads = list(reads)
        writes = list(writes)
        waits = self._deps(queue, reads, writes)
        if key not in self.dma_sems:
            self.dma_sems[key] = [self._new_sem("d"), 0]
        ent = self.dma_sems[key]
        ent[1] += 16
        sem = ent[0]
        ev = (sem, ent[1], None)

        def emit(e):
            for s, val in waits:
                e.wait_ge(s, val)
            e.dma_start(out=out, in_=in_).then_inc(sem, 16)

        self.streams[queue].append(emit)
        self._commit(ev, reads, writes)
        return ev

    def finish(self, eng='sp'):
        waits = [(ent[0], ent[1]) for ent in self.dma_sems.values()]
        for e in self.ENG:
            if e != eng and self.cnt[e] > 0:
                waits.append((self.sem[e], self.cnt[e]))

        def emit(e):
            for s, val in waits:
                e.wait_ge(s, val)

        self.streams[eng].append(emit)

    def emit_all(self):
        with self.nc.Block() as block:
            @block.tensor
            def _(e):
                for f in self.streams['pe']:
                    f(e)

            @block.scalar
            def _(e):
                for f in self.streams['act']:
                    f(e)

            @block.vector
            def _(e):
                for f in self.streams['dve']:
                    f(e)

            @block.gpsimd
            def _(e):
                for f in self.streams['pool']:
                    f(e)

            @block.sync
            def _(e):
                for f in self.streams['sp']:
                    f(e)


class Buf:
    def __init__(self, arena_name, ap, off, dt_bytes):
        self.an = arena_name
        self.ap = ap
        self.off = off
        self.eb = dt_bytes
        sh = ap.shape[1:]
        self.n = int(np.prod(sh))
        self.inner = int(np.prod(sh[1:])) if len(sh) > 1 else 1

    def k(self, i=None, j=None):
        if i is None:
            return (self.an, self.off, self.off + self.n * self.eb)
        if j is None:
            j = i + 1
        return (self.an, self.off + i * self.inner * self.eb, self.off + j * self.inner * self.eb)

    def ke(self, lo, hi):
        return (self.an, self.off + lo * self.eb, self.off + hi * self.eb)


def build_nc(nt_own=NT_OWN, debug=False):
    nc = bass.Bass("TRN2", target_bir_lowering=False)
    NTILE = HALO_T + nt_own
    NTOK = NTILE * TT
    xT = nc.dram_tensor("xT", [D, (HALO_T + NT_OWN) * TT], F32, kind="ExternalInput").ap()
    w_in = nc.dram_tensor("w_in", [D, 14336], F32, kind="ExternalInput").ap()
    w_co = nc.dram_tensor("w_co", [D, D], F32, kind="ExternalInput").ap()
    w_ao = nc.dram_tensor("w_ao", [D, D], F32, kind="ExternalInput").ap()
    w_out = nc.dram_tensor("w_out", [D, D], F32, kind="ExternalInput").ap()
    w_q = nc.dram_tensor("w_q", [D, D], F32, kind="ExternalInput").ap()
    uT = nc.dram_tensor("uT", [D, 16384], F32, kind="ExternalInput").ap()
    ev_d = nc.dram_tensor("ev", [16384, D], F32, kind="ExternalInput").ap()
    vecs_d = nc.dram_tensor("vecs", [128, 578], F32, kind="ExternalInput").ap()
    tb_d = nc.dram_tensor("tb", [128, 16, 640], F32, kind="ExternalInput").ap()
    mb_d = nc.dram_tensor("mb", [128, 20], F32, kind="ExternalInput").ap()
    k12_d = nc.dram_tensor("k12", [128, 256], F32, kind="ExternalInput").ap()
    yT = nc.dram_tensor("yT", [D, NT_OWN * TT], F32, kind="ExternalOutput").ap()
    NBLK = 28 + 16 + 64
    wsc = nc.dram_tensor("wsc", [NBLK, 128, 8192], BF16, kind="Internal").ap()
    dsc = nc.dram_tensor("dsc", [8, 128, 2 * 31 * 128], BF16, kind="Internal").ap()
    if debug:
        dbg_mid = nc.dram_tensor("dbg_mid", [D, TT], F32, kind="ExternalOutput").ap()

    with ExitStack() as st:
        S = Sched(nc, st)

        def sb(name, shape, dt):
            t = st.enter_context(nc.sbuf_tensor("s_" + name, shape, dt))
            eb = 4 if dt in (F32, U32) else 2
            return Buf(name, t[:], 0, eb)

        xt = sb("xt", [128, 16, TT], F32)
        hT = sb("hT", [128, 16, TT], BF16)
        knT = sb("knT", [128, 16, 3, TT], BF16)
        vtok = sb("vtok", [128, 3, 2, D], BF16)
        Etab2 = [sb(f"Etab{i}", [128, 640], BF16) for i in range(2)]
        NWB = 3
        wbt = [sb(f"wb{i}", [128, 8192], BF16) for i in range(NWB)]
        ubuf = sb("ubuf", [128, 16, 30 + TT], BF16)
        vecs = sb("vecs", [128, 578], F32)
        mb = sb("mb", [128, 20], F32)
        k12 = sb("k12", [128, 256], BF16)
        ones_f = sb("ones_f", [128, 128], F32)
        ones_b = sb("ones_b", [128, 128], BF16)
        ident = sb("ident", [128, 128], F32)
        iota128 = sb("iota128", [128, 128], F32)
        iota16 = sb("iota16", [128, 16], F32)
        g1s = sb("g1s", [128, 16], F32)
        g2s = sb("g2s", [128, 16], F32)

        SCR_BYTES = 60 * 1024
        scr_t = st.enter_context(nc.sbuf_tensor("scr", [128, SCR_BYTES // 2], BF16))

        def scr(off, shape, dt):
            eb = 4 if dt in (F32, U32) else 2
            n = int(np.prod(shape))
            assert off % 4 == 0 and off + n * eb <= SCR_BYTES, (off, shape)
            ap = scr_t[:, off // 2: off // 2 + n * eb // 2]
            if dt != BF16:
                ap = ap.bitcast(dt)
            if len(shape) == 2:
                ap = ap.rearrange("p (a b) -> p a b", a=shape[0])
            elif len(shape) == 3:
                ap = ap.rearrange("p (a b c) -> p a b c", a=shape[0], b=shape[1])
            return Buf("scr", ap, off, eb)

        K = 1024
        ybuf = scr(0, [16, TT], F32)
        qnT = scr(0, [16, TT], BF16)
        attnT = scr(8 * K, [16, TT], BF16)
        su = scr(16 * K, [16, TT], BF16)
        merged = scr(16 * K, [16, TT], BF16)
        mc = scr(24 * K, [16, TT], BF16)
        PT = [scr(32 * K + i * K, [TT], F32) for i in range(2)]
        PT2 = [scr(34 * K + i * 3 * K, [6, TT], BF16) for i in range(2)]
        tmpf = [scr(40 * K + i * K, [TT], F32) for i in range(6)]
        sqt = [scr(46 * K + i * 4 * K, [4, TT], F32) for i in range(2)]
        etmp = [scr(54 * K + i * 2560, [640], F32) for i in range(2)]
        pqT = scr(0, [16, TT], BF16)
        sc = scr(8 * K, [16, 128], F32)
        Gsub = scr(16 * K, [EG, TT], BF16)
        Gsub2 = scr(0, [EG, TT], BF16)
        cand = scr(32 * K, [8, 256], F32)
        wk = [scr(40 * K + i * K, [256], F32) for i in range(2)]
        gel = [scr(42 * K + i * K, [TT], F32) for i in range(2)]
        v1 = scr(44 * K, [16, 16], F32)
        i1u = scr(45 * K, [16, 16], U32)
        i1f = scr(46 * K, [16, 16], F32)
        topv = scr(47 * K, [8, 16], F32)
        flatu = scr(47 * K + 512, [8, 16], U32)
        af = scr(48 * K, [8, 16], F32)
        bf_ = scr(48 * K + 512, [8, 16], F32)
        oh4 = scr(49 * K, [8, 16, 16], F32)
        e1f = scr(57 * K, [8, 16], F32)
        e2f = scr(57 * K + 512, [8, 16], F32)
        gat = scr(58 * K, [8, 16], F32)
        gsum = scr(58 * K + 512, [8], F32)
        au = scr(58 * K + 512 + 64, [8, 16], U32)
        bu = scr(59 * K + 64, [8, 16], U32)
        ytmp = [scr(54 * K + i * K, [TT], F32) for i in range(4)]
        Ism = sb("Ism", [128, TT], BF16)
        Jsm = sb("Jsm", [128, TT], BF16)
        iota128b = sb("iota128b", [128, 128], BF16)
        ident_b = sb("ident_b", [128, 128], BF16)
        gsm = sb("gsm", [128, TT], F32)
        OJb = [scr(32 * K + i * 4 * K, [16, 128], BF16) for i in range(2)]
        Cb = [scr(44 * K + i * K, [16, 32], BF16) for i in range(2)]
        Cf = [scr(46 * K + i * 2 * K, [16, 32], BF16) for i in range(2)]
        wTb = [scr(50 * K + i * 2 * K, [CG, TT], BF16) for i in range(2)]

        pst = [st.enter_context(nc.psum_tensor(f"ps{i}", [128, 512], F32)) for i in range(8)]

        class PBuf(Buf):
            def __init__(self, bank, ap):
                Buf.__init__(self, "ps", ap, bank * 2048, 4)
                self.bank = bank

            def k(self, i=None, j=None):
                return ("ps", self.bank * 2048, (self.bank + 1) * 2048)

        def psh(bank, half):
            return PBuf(bank, pst[bank][:, half * 256:half * 256 + 256])

        def psf(b):
            return PBuf(b, pst[b][:, :])

        class Rot:
            def __init__(self, items):
                self.items = items
                self.i = 0

            def next(self):
                r = self.items[self.i % len(self.items)]
                self.i += 1
                return r

        pj = Rot([psh(0, 0), psh(1, 0), psh(2, 0)])
        pstat = Rot([psh(3, 0), psh(3, 1)])
        pscore = Rot([psh(4, 0), psh(5, 0)])
        ppv = psh(6, 0)
        pden = psh(6, 1)
        pfull = Rot([psf(6), psf(7)])
        pfull2 = psf(5)
        ppy = Rot([psh(3, 0), psh(4, 0), psh(5, 0)])
        wrot = Rot(list(range(NWB)))

        V = lambda c0, n=1: vecs.ap[:, c0:c0 + n]
        N1G, N2G, DWB, LNG, LNB, QG, KG, DWW = 0, 16, 32, 48, 64, 80, 81, 82

        S.dma('sp', vecs.ap, vecs_d, writes=[vecs.k()], key='c0')
        S.dma('sp', mb.ap, mb_d, writes=[mb.k()], key='c1')
        S.dma('pool', k12.ap, k12_d, writes=[k12.k()], key='c2')
        S.op('dve', lambda e: e.memset(ones_f.ap, 1.0), writes=[ones_f.k()])
        S.op('dve', lambda e: e.memset(ones_b.ap, 1.0), writes=[ones_b.k()])
        S.op('dve', lambda e: e.memset(ubuf.ap, 0.0), writes=[ubuf.k()])
        S.op('pool', lambda e: e.iota(ident.ap, pattern=[[1, 128]], base=0, channel_multiplier=-1,
                                      allow_small_or_imprecise_dtypes=True), writes=[ident.k()])
        S.op('dve', lambda e: e.tensor_scalar(out=ident.ap, in0=ident.ap, scalar1=0.0, scalar2=None,
                                              op0=ALU.is_equal), reads=[ident.k()], writes=[ident.k()])
        S.op('dve', lambda e: e.tensor_copy(out=ident_b.ap, in_=ident.ap), reads=[ident.k()], writes=[ident_b.k()])
        S.op('pool', lambda e: e.iota(iota128.ap, pattern=[[1, 128]], base=0, channel_multiplier=0,
                                      allow_small_or_imprecise_dtypes=True), writes=[iota128.k()])
        S.op('dve', lambda e: e.tensor_copy(out=iota128b.ap, in_=iota128.ap), reads=[iota128.k()], writes=[iota128b.k()])
        S.op('pool', lambda e: e.iota(iota16.ap, pattern=[[1, 16]], base=0, channel_multiplier=0,
                                      allow_small_or_imprecise_dtypes=True), writes=[iota16.k()])
        S.op('dve', lambda e: e.tensor_scalar(out=g1s.ap, in0=V(N1G, 16), scalar1=float(np.sqrt(D)), scalar2=None,
                                              op0=ALU.mult), reads=[vecs.k()], writes=[g1s.k()])
        S.op('dve', lambda e: e.tensor_scalar(out=g2s.ap, in0=V(N2G, 16), scalar1=float(np.sqrt(D)), scalar2=None,
                                              op0=ALU.mult), reads=[vecs.k()], writes=[g2s.k()])
        blk_id = {}
        NCVSEM = 56
        CV_AHEAD = 4
        CV_LOOK = 8

        conv_list = []
        conv_done = [0]

        def conv_blk(name, src_ap, j, pattern):
            b = len(blk_id)
            blk_id[(name, j)] = b
            conv_list.append((src_ap, pattern))

        def ensure_conv(upto):
            while conv_done[0] < min(upto, len(conv_list)):
                b = conv_done[0]
                src_ap, pattern = conv_list[b]
                c = 16 if pattern == 'kc' else 4
                S.dma('pool', wsc[b].rearrange("p (c n) -> p c n", c=c), src_ap.rearrange("(c p) n -> p c n", p=128),
                      reads=([('wsc', b - CV_AHEAD, b - CV_AHEAD + 1)] if b >= CV_AHEAD else []),
                      writes=[('wsc', b, b + 1), ('cvslot', b % NCVSEM, b % NCVSEM + 1)], key=('cv', b % NCVSEM))
                conv_done[0] += 1

        def conv_in(j0):
            for j in range(4):
                conv_blk('w_in', w_in[:, j0 + 512 * j:j0 + 512 * (j + 1)], (j0 // 512) + j, 'kc')

        conv_in(6144)
        conv_in(8192)
        for j in range(4):
            conv_blk('w_in', w_in[:, 512 * j:512 * (j + 1)], j, 'kc')
            conv_blk('w_in', w_in[:, 2048 + 512 * j:2048 + 512 * (j + 1)], 4 + j, 'kc')
        for j in range(4):
            conv_blk('w_co', w_co[:, 512 * j:512 * (j + 1)], j, 'kc')
            conv_blk('w_in', w_in[:, 10240 + 512 * j:10240 + 512 * (j + 1)], 20 + j, 'kc')
        conv_in(4096)
        for j in range(4):
            conv_blk('w_ao', w_ao[:, 512 * j:512 * (j + 1)], j, 'kc')
            conv_blk('w_in', w_in[:, 12288 + 512 * j:12288 + 512 * (j + 1)], 24 + j, 'kc')
        for j in range(4):
            conv_blk('w_out', w_out[:, 512 * j:512 * (j + 1)], j, 'kc')
        for j in range(4):
            conv_blk('w_q', w_q[:, 512 * j:512 * (j + 1)], j, 'kc')
        for j in range(NEXP_CH // CG):
            conv_blk('uT', uT[:, 512 * j:512 * (j + 1)], j, 'kc')
            conv_blk('ev', ev_d[512 * j:512 * (j + 1), :], j, 'ec')
        assert len(blk_id) == NBLK

        def load_w(name, j, rows_pattern, wi=None):
            if wi is None:
                wi = wrot.next()
            wb = wbt[wi]
            b = blk_id[(name, j)]
            ensure_conv(b + 1 + CV_LOOK)
            c = 16 if rows_pattern == 'kc' else 4
            view = Buf(wb.an, wb.ap.rearrange("p (c n) -> p c n", c=c), 0, 2)
            S.dma('sp', wb.ap, wsc[b], reads=[('wsc', b, b + 1)], writes=[wb.k()], key=('wb', wi))
            return view

        def proj(wv, sub, rhsbuf, ps):
            def f(e):
                for c in range(16):
                    ins = e.matmul(ps.ap, lhsT=wv.ap[:, c, sub * 128:(sub + 1) * 128], rhs=rhsbuf.ap[:, c, :],
                                   start=(c == 0), stop=(c == 15))
                return ins
            S.op('pe', f, reads=[wv.k(), rhsbuf.k()], writes=[ps.k()])

        def rsqrt_chain(dst, src_ap, src_key, addc):
            S.op('dve', lambda e: e.tensor_scalar(out=dst.ap, in0=src_ap, scalar1=float(addc), scalar2=None, op0=ALU.add),
                 reads=[src_key], writes=[dst.k()])
            S.op('act', lambda e: e.activation(out=dst.ap, in_=dst.ap, func=AF.Sqrt), reads=[dst.k()], writes=[dst.k()])
            S.op('dve', lambda e: e.reciprocal(out=dst.ap, in_=dst.ap), reads=[dst.k()], writes=[dst.k()])

        def rmsnorm_to_hT(gs):
            pss = pstat.next()
            for q4 in range(4):
                sq = sqt[q4 % 2]
                S.op('act', lambda e, q4=q4, sq=sq: e.activation(out=sq.ap, in_=xt.ap[:, 4 * q4:4 * q4 + 4, :], func=AF.Square),
                     reads=[xt.k(4 * q4, 4 * q4 + 4)], writes=[sq.k()])

                def f(e, q4=q4, sq=sq):
                    for j in range(4):
                        ins = e.matmul(pss.ap, lhsT=ones_f.ap, rhs=sq.ap[:, j, :], start=(q4 == 0 and j == 0),
                                       stop=(q4 == 3 and j == 3))
                    return ins
                S.op('pe', f, reads=[sq.k(), ones_f.k()], writes=[pss.k()])
            rs = tmpf[0]
            rsqrt_chain(rs, pss.ap, pss.k(), D * EPS)
            for c in range(16):
                S.op('dve', lambda e, c=c: e.scalar_tensor_tensor(out=hT.ap[:, c, :], in0=xt.ap[:, c, :], scalar=gs.ap[:, c:c + 1],
                                                                 in1=rs.ap, op0=ALU.mult, op1=ALU.mult),
                     reads=[xt.k(c), rs.k(), gs.k()], writes=[hT.k(c)])

        def qk_norm(ps, gcol, out_ap, out_key):
            sq = tmpf[1]
            S.op('act', lambda e: e.activation(out=sq.ap, in_=ps.ap, func=AF.Square), reads=[ps.k()], writes=[sq.k()])
            pss = pstat.next()
            S.op('pe', lambda e: e.matmul(pss.ap, lhsT=ones_f.ap, rhs=sq.ap, start=True, stop=True),
                 reads=[sq.k(), ones_f.k()], writes=[pss.k()])
            rs = tmpf[2]
            rsqrt_chain(rs, pss.ap, pss.k(), 128 * EPS)
            S.op('dve', lambda e: e.scalar_tensor_tensor(out=out_ap, in0=ps.ap, scalar=V(gcol), in1=rs.ap,
                                                         op0=ALU.mult, op1=ALU.mult),
                 reads=[ps.k(), rs.k(), vecs.k()], writes=[out_key])

        ND = 2 * 31 * 128
        for p2 in range(8):
            wi = wrot.next()
            stg = Buf(wbt[wi].an, wbt[wi].ap[:, 0:ND].rearrange("p (i j m) -> p i j m", i=2, j=31), 0, 2)
            for i in range(2):
                c = 2 * p2 + i
                for j in range(31):
                    S.op('dve', lambda e, stg=stg, i=i, j=j, c=c: e.tensor_scalar(out=stg.ap[:, i, j, :], in0=ident.ap, scalar1=V(DWW + c * 31 + j),
                                                                                 scalar2=None, op0=ALU.mult),
                         reads=[ident.k(), vecs.k()], writes=[stg.ke((i * 31 + j) * 128, (i * 31 + j + 1) * 128)])
            S.dma('sp', dsc[p2], wbt[wi].ap[:, 0:ND], reads=[stg.k()], writes=[('dsc', p2, p2 + 1)], key=('dst', p2))

        def do_tile(ti):
            own = ti >= HALO_T
            slot = ti % 3
            S.dma('sp', xt.ap, xT[:, ti * TT:(ti + 1) * TT].rearrange("(c p) t -> p c t", p=128),
                  writes=[xt.k()], key='xt')
            rmsnorm_to_hT(g1s)

            if ti >= 1:
                for j in range(4):
                    wa = load_w('w_in', j, 'kc')
                    wg = load_w('w_in', 4 + j, 'kc')
                    for sub in range(4):
                        c = 4 * j + sub
                        pa = pj.next()
                        pg = pj.next()
                        proj(wa, sub, hT, pa)
                        proj(wg, sub, hT, pg)
                        sg = tmpf[3 + (c % 2)]
                        S.op('act', lambda e, sg=sg, pg=pg: e.activation(out=sg.ap, in_=pg.ap, func=AF.Sigmoid),
                             reads=[pg.k()], writes=[sg.k()])
                        S.op('dve', lambda e, c=c, sg=sg, pa=pa: e.tensor_tensor(out=ubuf.ap[:, c, 30:30 + TT], in0=pa.ap, in1=sg.ap, op=ALU.mult),
                             reads=[pa.k(), sg.k()], writes=[ubuf.k(c)])
            if own:
                for p2 in range(8):
                    wi = wrot.next()
                    S.dma('sp', wbt[wi].ap[:, 0:ND], dsc[p2], reads=[('dsc', p2, p2 + 1)], writes=[wbt[wi].k()], key=('wb', wi))
                    dg = Buf(wbt[wi].an, wbt[wi].ap[:, 0:ND].rearrange("p (i j m) -> p i j m", i=2, j=31), 0, 2)
                    for i in range(2):
                        c = 2 * p2 + i
                        ps = pj.next()

                        def f(e, dg=dg, i=i, c=c, ps=ps):
                            for j in range(31):
                                ins = e.matmul(ps.ap, lhsT=dg.ap[:, i, j, :], rhs=ubuf.ap[:, c, j:j + TT], start=(j == 0), stop=(j == 30))
                            return ins
                        S.op('pe', f, reads=[wbt[wi].k(), ubuf.k(c)], writes=[ps.k()])
                        S.op('act', lambda e, c=c, ps=ps: e.activation(out=ybuf.ap[:, c, :], in_=ps.ap, func=AF.Identity, bias=V(DWB + c)),
                             reads=[ps.k(), vecs.k()], writes=[ybuf.k(c)])

            def conv_emit(n):
                pass

            for j in range(4):
                wv = load_w('w_in', 12 + j, 'kc')
                for sub in range(4):
                    h = 4 * j + sub
                    ps = pj.next()
                    proj(wv, sub, hT, ps)
                    qk_norm(ps, KG, knT.ap[:, h, slot, :], knT.ke((h * 3 + slot) * TT, (h * 3 + slot + 1) * TT))
                    conv_emit(24)
            for j in range(4):
                wv = load_w('w_in', 16 + j, 'kc')
                for t2 in range(2):
                    pf = pfull.next()

                    def f(e, wv=wv, t2=t2, pf=pf):
                        for c in range(16):
                            ins = e.matmul(pf.ap, lhsT=hT.ap[:, c, t2 * 128:(t2 + 1) * 128], rhs=wv.ap[:, c, :],
                                           start=(c == 0), stop=(c == 15))
                        return ins
                    S.op('pe', f, reads=[wv.k(), hT.k()], writes=[pf.k()])
                    S.op('act', lambda e, t2=t2, j=j, pf=pf: e.activation(out=vtok.ap[:, slot, t2, 512 * j:512 * (j + 1)], in_=pf.ap, func=AF.Copy),
                         reads=[pf.k()], writes=[vtok.ke((slot * 2 + t2) * D + 512 * j, (slot * 2 + t2) * D + 512 * (j + 1))])
                    conv_emit(14)
            conv_emit(10000)
            if ti == 0:
                return
            if not own:
                S.op('pool', lambda e: e.tensor_copy(out=ubuf.ap[:, :, 0:30], in_=ubuf.ap[:, :, TT:TT + 30]),
                     reads=[ubuf.k()], writes=[ubuf.k()])
                return
            S.op('pool', lambda e: e.tensor_copy(out=ubuf.ap[:, :, 0:30], in_=ubuf.ap[:, :, TT:TT + 30]),
                 reads=[ubuf.k()], writes=[ubuf.k()])
            ps1 = pstat.next()
            ps2 = pstat.next()

            def f(e):
                for c in range(16):
                    ins = e.matmul(ps1.ap, lhsT=ones_f.ap, rhs=ybuf.ap[:, c, :], start=(c == 0), stop=(c == 15))
                return ins
            S.op('pe', f, reads=[ybuf.k(), ones_f.k()], writes=[ps1.k()])
            for q4 in range(4):
                sq = sqt[q4 % 2]
                S.op('act', lambda e, q4=q4, sq=sq: e.activation(out=sq.ap, in_=ybuf.ap[:, 4 * q4:4 * q4 + 4, :], func=AF.Square),
                     reads=[ybuf.k(4 * q4, 4 * q4 + 4)], writes=[sq.k()])

                def f(e, q4=q4, sq=sq):
                    for j in range(4):
                        ins = e.matmul(ps2.ap, lhsT=ones_f.ap, rhs=sq.ap[:, j, :], start=(q4 == 0 and j == 0),
                                       stop=(q4 == 3 and j == 3))
                    return ins
                S.op('pe', f, reads=[sq.k(), ones_f.k()], writes=[ps2.k()])
            mean, msq, rstd, nmr = tmpf[0], tmpf[1], tmpf[2], tmpf[3]
            S.op('dve', lambda e: e.tensor_scalar(out=mean.ap, in0=ps1.ap, scalar1=1.0 / D, scalar2=None, op0=ALU.mult),
                 reads=[ps1.k()], writes=[mean.k()])
            S.op('dve', lambda e: e.tensor_tensor(out=msq.ap, in0=mean.ap, in1=mean.ap, op=ALU.mult),
                 reads=[mean.k()], writes=[msq.k()])
            S.op('dve', lambda e: e.scalar_tensor_tensor(out=rstd.ap, in0=ps2.ap, scalar=1.0 / D, in1=msq.ap,
                                                         op0=ALU.mult, op1=ALU.subtract),
                 reads=[ps2.k(), msq.k()], writes=[rstd.k()])
            rsqrt_chain(rstd, rstd.ap, rstd.k(), EPS)
            S.op('dve', lambda e: e.scalar_tensor_tensor(out=nmr.ap, in0=mean.ap, scalar=-1.0, in1=rstd.ap,
                                                         op0=ALU.mult, op1=ALU.mult),
                 reads=[mean.k(), rstd.k()], writes=[nmr.k()])
            for c in range(16):
                S.op('pool', lambda e, c=c: e.tensor_tensor(out=ybuf.ap[:, c, :], in0=ybuf.ap[:, c, :], in1=rstd.ap, op=ALU.mult),
                     reads=[ybuf.k(c), rstd.k()], writes=[ybuf.k(c)])
            for c in range(16):
                S.op('pool', lambda e, c=c: e.tensor_tensor(out=ybuf.ap[:, c, :], in0=ybuf.ap[:, c, :], in1=nmr.ap, op=ALU.add),
                     reads=[ybuf.k(c), nmr.k()], writes=[ybuf.k(c)])
            for c in range(16):
                S.op('act', lambda e, c=c: e.activation(out=su.ap[:, c, :], in_=ybuf.ap[:, c, :], func=AF.Silu,
                                                        bias=V(LNB + c), scale=V(LNG + c)),
                     reads=[ybuf.k(c), vecs.k()], writes=[su.k(c)])
            for j in range(4):
                wc = load_w('w_co', j, 'kc')
                wg = load_w('w_in', 20 + j, 'kc')
                for sub in range(4):
                    dc = 4 * j + sub
                    pc = pj.next()
                    pg = pj.next()
                    proj(wc, sub, su, pc)
                    proj(wg, sub, hT, pg)
                    sg = tmpf[4 + (dc % 2)]
                    S.op('act', lambda e, sg=sg, pg=pg: e.activation(out=sg.ap, in_=pg.ap, func=AF.Sigmoid),
                         reads=[pg.k()], writes=[sg.k()])
                    S.op('dve', lambda e, dc=dc, sg=sg, pc=pc: e.tensor_tensor(out=mc.ap[:, dc, :], in0=pc.ap, in1=sg.ap, op=ALU.mult),
                         reads=[pc.k(), sg.k()], writes=[mc.k(dc)])
            for j in range(4):
                wv = load_w('w_in', 8 + j, 'kc')
                for sub in range(4):
                    h = 4 * j + sub
                    ps = pj.next()
                    proj(wv, sub, hT, ps)
                    qk_norm(ps, QG, qnT.ap[:, h, :], qnT.k(h))
            RQ = [(0, 128, 512), (0, 256, 384), (0, 256, 256), (0, 256, 128), (0, 256, 0), (128, 128, 0)]
            for h in range(16):
                pt2 = PT2[h % 2]
                et = etmp[h % 2]
                Eh = Etab2[h % 2]
                S.dma('sp', et.ap, tb_d[:, h, :], writes=[et.k()], key=('et', h % 2))
                S.op('act', lambda e, et=et, Eh=Eh: e.activation(out=Eh.ap, in_=et.ap, func=AF.Identity, scale=float(1.0 / np.sqrt(128.0))),
                     reads=[et.k()], writes=[Eh.k()])
                S.op('dve', lambda e, Eh=Eh: e.memset(Eh.ap[0:64, 576:640], -3000.0), writes=[Eh.k()])
                S.op('dve', lambda e, Eh=Eh: e.memset(Eh.ap[64:128, 0:64], -3000.0), writes=[Eh.k()])
                for r in range(6):
                    q0, nq, qrel0 = RQ[r]
                    tsrc = ti - 2 + r // 2
                    ksl = tsrc % 3
                    kh = r % 2
                    gidx = 2 * (ti - 2) + r
                    ps = pscore.next()
                    kkey = knT.ke((h * 3 + ksl) * TT + kh * 128, (h * 3 + ksl) * TT + kh * 128 + 128)
                    def fs(e, ps=ps, h=h, ksl=ksl, kh=kh, q0=q0, nq=nq, qrel0=qrel0, Eh=Eh):
                        e.matmul(ps.ap[:, 0:nq], lhsT=knT.ap[:, h, ksl, kh * 128:(kh + 1) * 128], rhs=qnT.ap[:, h, q0:q0 + nq],
                                 start=True, stop=False)
                        return e.matmul(ps.ap[:, 0:nq], lhsT=ident_b.ap, rhs=Eh.ap[:, qrel0:qrel0 + nq], start=False, stop=True)
                    S.op('pe', fs, reads=[kkey, qnT.k(h), Eh.k(), ident_b.k()], writes=[ps.k()])
                    S.op('act', lambda e, ps=ps, pt2=pt2, r=r, q0=q0, nq=nq, gidx=gidx: e.activation(
                        out=pt2.ap[:, r, q0:q0 + nq], in_=ps.ap[:, 0:nq], func=AF.Exp, bias=mb.ap[:, gidx:gidx + 1],
                        scale=float(np.sqrt(128.0))), reads=[ps.k(), mb.k()], writes=[pt2.k(r)])

                def f(e, h=h, pt2=pt2):
                    for b in range(2):
                        for r in range(b, b + 5):
                            tsrc = ti - 2 + r // 2
                            e.matmul(ppv.ap[:, b * 128:(b + 1) * 128], lhsT=vtok.ap[:, tsrc % 3, r % 2, h * 128:(h + 1) * 128],
                                     rhs=pt2.ap[:, r, b * 128:(b + 1) * 128], start=(r == b), stop=(r == b + 4))
                    for b in range(2):
                        for r in range(b, b + 5):
                            ins = e.matmul(pden.ap[:, b * 128:(b + 1) * 128], lhsT=ones_b.ap,
                                           rhs=pt2.ap[:, r, b * 128:(b + 1) * 128], start=(r == b), stop=(r == b + 4))
                    return ins
                S.op('pe', f, reads=[vtok.k(), pt2.k(), ones_b.k()], writes=[ppv.k(), pden.k()])
                rec = tmpf[0]
                S.op('dve', lambda e: e.reciprocal(out=rec.ap, in_=pden.ap), reads=[pden.k()], writes=[rec.k()])
                S.op('dve', lambda e, h=h: e.tensor_tensor(out=attnT.ap[:, h, :], in0=ppv.ap, in1=rec.ap, op=ALU.mult),
                     reads=[ppv.k(), rec.k()], writes=[attnT.k(h)])
            for j in range(4):
                wa = load_w('w_ao', j, 'kc')
                wg = load_w('w_in', 24 + j, 'kc')
                for sub in range(4):
                    dc = 4 * j + sub
                    pa = pj.next()
                    pg = pj.next()
                    proj(wa, sub, attnT, pa)
                    proj(wg, sub, hT, pg)
                    sg = tmpf[4 + (dc % 2)]
                    tt_ = tmpf[2 + (dc % 2)]
                    S.op('act', lambda e, sg=sg, pg=pg: e.activation(out=sg.ap, in_=pg.ap, func=AF.Sigmoid),
                         reads=[pg.k()], writes=[sg.k()])
                    S.op('dve', lambda e, tt_=tt_, sg=sg, pa=pa: e.tensor_tensor(out=tt_.ap, in0=pa.ap, in1=sg.ap, op=ALU.mult),
                         reads=[pa.k(), sg.k()], writes=[tt_.k()])
                    S.op('pool', lambda e, dc=dc, tt_=tt_: e.tensor_tensor(out=merged.ap[:, dc, :], in0=tt_.ap, in1=mc.ap[:, dc, :], op=ALU.add),
                         reads=[tt_.k(), mc.k(dc)], writes=[merged.k(dc)])
            for j in range(4):
                wo = load_w('w_out', j, 'kc')
                for sub in range(4):
                    dc = 4 * j + sub
                    po = pj.next()
                    proj(wo, sub, merged, po)
                    S.op('dve', lambda e, dc=dc, po=po: e.tensor_tensor(out=xt.ap[:, dc, :], in0=po.ap, in1=xt.ap[:, dc, :], op=ALU.add),
                         reads=[po.k(), xt.k(dc)], writes=[xt.k(dc)])
            if debug and ti == HALO_T:
                S.dma('sp', dbg_mid.rearrange("(c p) t -> p c t", p=128), xt.ap, reads=[xt.k()], key='dbg')

            rmsnorm_to_hT(g2s)
            for j in range(4):
                wq = load_w('w_q', j, 'kc')
                for sub in range(4):
                    cc = 4 * j + sub
                    pq = pj.next()
                    proj(wq, sub, hT, pq)
                    S.op('act', lambda e, cc=cc, pq=pq: e.activation(out=pqT.ap[:, cc, :], in_=pq.ap, func=AF.Copy),
                         reads=[pq.k()], writes=[pqT.k(cc)])
            v1v = v1.ap.rearrange("p (h two) k -> p h two k", two=2)
            i1v = i1f.ap.rearrange("p (h two) k -> p h two k", two=2)
            for t2 in range(2):
                for g4 in range(4):
                    pf = pfull.next()

                    def f(e, g4=g4, pf=pf, t2=t2):
                        for i in range(4):
                            cc = 4 * g4 + i
                            ins = e.matmul(pf.ap[:, i * 128:(i + 1) * 128], lhsT=pqT.ap[:, cc, t2 * 128:(t2 + 1) * 128],
                                           rhs=k12.ap[:, (cc % 2) * 128:(cc % 2) * 128 + 128], start=True, stop=True)
                        return ins
                    S.op('pe', f, reads=[pqT.k(4 * g4, 4 * g4 + 4), k12.k()], writes=[pf.k()])
                    S.op('act', lambda e, g4=g4, pf=pf: e.activation(out=sc.ap[:, 4 * g4:4 * g4 + 4, :],
                                                                     in_=pf.ap.rearrange("p (a b) -> p a b", a=4), func=AF.Copy),
                         reads=[pf.k()], writes=[sc.k(4 * g4, 4 * g4 + 4)])
                for cc in range(16):
                    S.op('dve', lambda e, cc=cc: e.max(out=v1.ap[:, cc, 0:8], in_=sc.ap[:, cc, :]),
                         reads=[sc.k(cc)], writes=[v1.ke(cc * 16, cc * 16 + 8)])
                for cc in range(16):
                    S.op('dve', lambda e, cc=cc: e.max_index(out=i1u.ap[:, cc, 0:8], in_max=v1.ap[:, cc, 0:8], in_values=sc.ap[:, cc, :]),
                         reads=[sc.k(cc), v1.ke(cc * 16, cc * 16 + 8)], writes=[i1u.ke(cc * 16, cc * 16 + 8)])
                for cc in range(16):
                    S.op('dve', lambda e, cc=cc: e.match_replace(out=sc.ap[:, cc, :], in_to_replace=v1.ap[:, cc, 0:8],
                                                                 in_values=sc.ap[:, cc, :], imm_value=-1e30),
                         reads=[sc.k(cc), v1.ke(cc * 16, cc * 16 + 8)], writes=[sc.k(cc)])
                for cc in range(16):
                    S.op('dve', lambda e, cc=cc: e.max(out=v1.ap[:, cc, 8:16], in_=sc.ap[:, cc, :]),
                         reads=[sc.k(cc)], writes=[v1.ke(cc * 16 + 8, cc * 16 + 16)])
                for cc in range(16):
                    S.op('dve', lambda e, cc=cc: e.max_index(out=i1u.ap[:, cc, 8:16], in_max=v1.ap[:, cc, 8:16], in_values=sc.ap[:, cc, :]),
                         reads=[sc.k(cc), v1.ke(cc * 16 + 8, cc * 16 + 16)], writes=[i1u.ke(cc * 16 + 8, cc * 16 + 16)])
                S.op('dve', lambda e: e.tensor_copy(out=i1f.ap, in_=i1u.ap), reads=[i1u.k()], writes=[i1f.k()])
                cand4 = cand.ap.rearrange("p h (a b) -> p h a b", a=16)
                S.op('dve', lambda e: e.tensor_tensor(out=cand4, in0=v1v[:, :, 0, :].unsqueeze(3).to_broadcast([128, 8, 16, 16]),
                                                      in1=v1v[:, :, 1, :].unsqueeze(2).to_broadcast([128, 8, 16, 16]), op=ALU.add),
                     reads=[v1.k()], writes=[cand.k()])
                for h in range(8):
                    S.op('dve', lambda e, h=h: e.max(out=topv.ap[:, h, 0:8], in_=cand.ap[:, h, :]),
                         reads=[cand.k(h)], writes=[topv.ke(h * 16, h * 16 + 8)])
                for h in range(8):
                    S.op('dve', lambda e, h=h: e.max_index(out=flatu.ap[:, h, 0:8], in_max=topv.ap[:, h, 0:8], in_values=cand.ap[:, h, :]),
                         reads=[cand.k(h), topv.ke(h * 16, h * 16 + 8)], writes=[flatu.ke(h * 16, h * 16 + 8)])
                for h in range(8):
                    S.op('dve', lambda e, h=h: e.match_replace(out=cand.ap[:, h, :], in_to_replace=topv.ap[:, h, 0:8],
                                                               in_values=cand.ap[:, h, :], imm_value=-1e30),
                         reads=[cand.k(h), topv.ke(h * 16, h * 16 + 8)], writes=[cand.k(h)])
                for h in range(8):
                    S.op('dve', lambda e, h=h: e.max(out=topv.ap[:, h, 8:16], in_=cand.ap[:, h, :]),
                         reads=[cand.k(h)], writes=[topv.ke(h * 16 + 8, h * 16 + 16)])
                for h in range(8):
                    S.op('dve', lambda e, h=h: e.max_index(out=flatu.ap[:, h, 8:16], in_max=topv.ap[:, h, 8:16], in_values=cand.ap[:, h, :]),
                         reads=[cand.k(h), topv.ke(h * 16 + 8, h * 16 + 16)], writes=[flatu.ke(h * 16 + 8, h * 16 + 16)])
                S.op('dve', lambda e: e.tensor_single_scalar(out=au.ap, in_=flatu.ap, scalar=4, op=ALU.logical_shift_right),
                     reads=[flatu.k()], writes=[au.k()])
                S.op('dve', lambda e: e.tensor_single_scalar(out=bu.ap, in_=flatu.ap, scalar=15, op=ALU.bitwise_and),
                     reads=[flatu.k()], writes=[bu.k()])
                S.op('dve', lambda e: e.tensor_copy(out=af.ap, in_=au.ap), reads=[au.k()], writes=[af.k()])
                S.op('dve', lambda e: e.tensor_copy(out=bf_.ap, in_=bu.ap), reads=[bu.k()], writes=[bf_.k()])
                io4 = iota16.ap.unsqueeze(1).unsqueeze(1).to_broadcast([128, 8, 16, 16])
                for (src, which, dst) in ((af, 0, e1f), (bf_, 1, e2f)):
                    S.op('dve', lambda e, src=src: e.tensor_tensor(out=oh4.ap, in0=src.ap.unsqueeze(3).to_broadcast([128, 8, 16, 16]),
                                                                   in1=io4, op=ALU.is_equal),
                         reads=[src.k(), iota16.k()], writes=[oh4.k()])
                    S.op('dve', lambda e, which=which: e.tensor_tensor(out=oh4.ap, in0=oh4.ap,
                                                                       in1=i1v[:, :, which, :].unsqueeze(2).to_broadcast([128, 8, 16, 16]),
                                                                       op=ALU.mult),
                         reads=[oh4.k(), i1f.k()], writes=[oh4.k()])
                    S.op('dve', lambda e, dst=dst: e.tensor_reduce(out=dst.ap, in_=oh4.ap, axis=AX.X, op=ALU.add),
                         reads=[oh4.k()], writes=[dst.k()])
                S.op('dve', lambda e: e.tensor_tensor(out=gat.ap, in0=topv.ap, in1=topv.ap[:, :, 0:1].to_broadcast([128, 8, 16]),
                                                      op=ALU.subtract), reads=[topv.k()], writes=[gat.k()])
                S.op('act', lambda e: e.activation(out=gat.ap, in_=gat.ap, func=AF.Exp), reads=[gat.k()], writes=[gat.k()])
                S.op('dve', lambda e: e.tensor_reduce(out=gsum.ap, in_=gat.ap, axis=AX.X, op=ALU.add),
                     reads=[gat.k()], writes=[gsum.k()])
                S.op('dve', lambda e: e.reciprocal(out=gsum.ap, in_=gsum.ap), reads=[gsum.k()], writes=[gsum.k()])
                S.op('dve', lambda e: e.tensor_tensor(out=gat.ap, in0=gat.ap, in1=gsum.ap.unsqueeze(2).to_broadcast([128, 8, 16]),
                                                      op=ALU.mult), reads=[gat.k(), gsum.k()], writes=[gat.k()])
                ptr = pfull2

                def f(e):
                    e.transpose(out=ptr.ap[:, 0:128], in_=e1f.ap.rearrange("p h k -> p (h k)"), identity=ident.ap)
                    e.transpose(out=ptr.ap[:, 128:256], in_=e2f.ap.rearrange("p h k -> p (h k)"), identity=ident.ap)
                    return e.transpose(out=ptr.ap[:, 256:384], in_=gat.ap.rearrange("p h k -> p (h k)"), identity=ident.ap)
                S.op('pe', f, reads=[e1f.k(), e2f.k(), gat.k(), ident.k()], writes=[ptr.k()])
                for i, dstb in enumerate((Ism, Jsm, gsm)):
                    S.op('act', lambda e, i=i, dstb=dstb, t2=t2: e.activation(out=dstb.ap[:, t2 * 128:(t2 + 1) * 128],
                                                                              in_=ptr.ap[:, i * 128:(i + 1) * 128], func=AF.Copy),
                         reads=[ptr.k()], writes=[dstb.ke(t2 * 128, (t2 + 1) * 128)])

            Gs = [Gsub, Gsub2]

            def gbuild_batch(eg, tb, G):
                oj, cb, cf = OJb[tb % 2], Cb[tb % 2], Cf[tb % 2]
                S.op('dve', lambda e: e.tensor_tensor(
                    out=oj.ap, in0=iota128b.ap.unsqueeze(1).to_broadcast([128, 16, 128]),
                    in1=Jsm.ap[:, tb * 16:(tb + 1) * 16].unsqueeze(2).to_broadcast([128, 16, 128]), op=ALU.is_equal),
                    reads=[iota128b.k(), Jsm.k()], writes=[oj.k()])
                S.op('dve', lambda e: e.tensor_tensor(
                    out=cf.ap, in0=iota128b.ap[:, eg * EG:(eg + 1) * EG].unsqueeze(1).to_broadcast([128, 16, EG]),
                    in1=Ism.ap[:, tb * 16:(tb + 1) * 16].unsqueeze(2).to_broadcast([128, 16, EG]), op=ALU.is_equal),
                    reads=[iota128b.k(), Ism.k()], writes=[cf.k()])
                S.op('pool', lambda e: e.tensor_tensor(
                    out=cb.ap, in0=cf.ap, in1=gsm.ap[:, tb * 16:(tb + 1) * 16].unsqueeze(2).to_broadcast([128, 16, EG]),
                    op=ALU.mult), reads=[cf.k(), gsm.k()], writes=[cb.k()])

                def mm():
                    pf = pfull.next()

                    def f(e):
                        for t in range(16):
                            ins = e.matmul(pf.ap[:, t * EG:(t + 1) * EG], lhsT=oj.ap[:, t, :], rhs=cb.ap[:, t, :],
                                           start=True, stop=True)
                        return ins
                    S.op('pe', f, reads=[oj.k(), cb.k()], writes=[pf.k()])
                    S.op('act', lambda e: e.activation(out=G.ap[:, :, tb * 16:(tb + 1) * 16],
                                                       in_=pf.ap.rearrange("p (t e) -> p e t", e=EG), func=AF.Copy),
                         reads=[pf.k()], writes=[G.k()])
                return mm

            NEG = NEXP_CH // EG
            NB = TT // 16
            NCGE = EG // CG
            NCG = NEG * NCGE

            def dense_A(i):
                eg, cg = divmod(i, NCGE)
                G = Gs[eg % 2]
                wu = load_w('uT', i, 'kc', wi=0)
                wvv = load_w('ev', i, 'ec', wi=1 + (i % 2))
                wt = wTb[i % 2]
                for cc in range(CG):
                    pa = pj.next()
                    proj(wu, cc, hT, pa)
                    ge = gel[cc % 2]
                    S.op('act', lambda e, ge=ge, pa=pa: e.activation(out=ge.ap, in_=pa.ap, func=AF.Gelu),
                         reads=[pa.k()], writes=[ge.k()])
                    S.op('dve', lambda e, ge=ge, cc=cc: e.tensor_tensor(
                        out=wt.ap[:, cc, :], in0=ge.ap, in1=G.ap[:, cg * CG + cc, :], op=ALU.mult),
                        reads=[ge.k(), G.k(cg * CG + cc)], writes=[wt.k(cc)])
                return wvv, wt

            def dense_Y(wvv, wt):
                for dc in range(16):
                    py = ppy.next()

                    def f(e, py=py, dc=dc):
                        for cc in range(CG):
                            ins = e.matmul(py.ap, lhsT=wvv.ap[:, cc, dc * 128:(dc + 1) * 128], rhs=wt.ap[:, cc, :],
                                           start=(cc == 0), stop=(cc == CG - 1))
                        return ins
                    S.op('pe', f, reads=[wvv.k(), wt.k()], writes=[py.k()])
                    if dc % 2 == 0:
                        S.op('dve', lambda e, dc=dc, py=py: e.tensor_tensor(out=xt.ap[:, dc, :], in0=py.ap, in1=xt.ap[:, dc, :], op=ALU.add),
                             reads=[py.k(), xt.k(dc)], writes=[xt.k(dc)])
                    else:
                        yt = ytmp[(dc // 2) % 4]
                        S.op('act', lambda e, yt=yt, py=py: e.activation(out=yt.ap, in_=py.ap, func=AF.Copy),
                             reads=[py.k()], writes=[yt.k()])
                        S.op('pool', lambda e, dc=dc, yt=yt: e.tensor_tensor(out=xt.ap[:, dc, :], in0=yt.ap, in1=xt.ap[:, dc, :], op=ALU.add),
                             reads=[yt.k(), xt.k(dc)], writes=[xt.k(dc)])

            for tb in range(NB):
                gbuild_batch(0, tb, Gs[0])()
            per = NB // NCGE
            cur = dense_A(0)
            for i in range(NCG):
                eg, cg = divmod(i, NCGE)
                mms = []
                if eg + 1 < NEG:
                    for tb in range(cg * per, (cg + 1) * per):
                        mms.append(gbuild_batch(eg + 1, tb, Gs[(eg + 1) % 2]))
                nxt = dense_A(i + 1) if (i + 1 < NCG and (i + 1) % NCGE != 0) else None
                for m in mms:
                    m()
                if i + 1 < NCG and (i + 1) % NCGE == 0:
                    nxt = dense_A(i + 1)
                dense_Y(*cur)
                cur = nxt
            S.dma('sp', yT[:, (ti - HALO_T) * TT:(ti - HALO_T + 1) * TT].rearrange("(c p) t -> p c t", p=128), xt.ap,
                  reads=[xt.k()], key='yst')
        for ti_ in range(NTILE):
            do_tile(ti_)
        S.finish('sp')
        S.emit_all()
    return nc


def _prep_shared(inputs):
    g = lambda k: np.asarray(inputs[k], dtype=np.float32)[0]
    sh = {}
    sh["w_in"] = np.ascontiguousarray(g("w_in"))
    sh["w_co"] = np.ascontiguousarray(g("w_conv_out"))
    sh["w_ao"] = np.ascontiguousarray(g("w_attn_o"))
    sh["w_out"] = np.ascontiguousarray(g("w_out"))
    sh["w_q"] = np.ascontiguousarray(g("w_query"))
    sh["uT"] = np.ascontiguousarray(g("expert_u").T)
    sh["ev"] = np.ascontiguousarray(g("expert_v"))
    fm = lambda v: v.reshape(16, 128).T
    vecs = np.zeros((128, 578), np.float32)
    vecs[:, 0:16] = fm(g("norm1_g"))
    vecs[:, 16:32] = fm(g("norm2_g"))
    vecs[:, 32:48] = fm(g("conv_dw_b"))
    vecs[:, 48:64] = fm(g("conv_ln_g"))
    vecs[:, 64:80] = fm(g("conv_ln_b"))
    vecs[:, 80] = g("q_norm_g")
    vecs[:, 81] = g("k_norm_g")
    dw = g("conv_dw_w")
    vecs[:, 82:] = dw.reshape(31, 16, 128).transpose(2, 1, 0).reshape(128, 16 * 31)
    sh["vecs"] = vecs
    kk = np.arange(128)[:, None]
    mm = np.arange(640)[None, :]
    idx = np.clip(mm - kk, -63, 128) + 63
    rb = g("rel_bias")
    sh["tb"] = np.ascontiguousarray(rb[:, idx].transpose(1, 0, 2))
    sh["k12"] = np.ascontiguousarray(np.concatenate([g("sub_keys_1").T, g("sub_keys_2").T], axis=1))
    return sh


_NC_CACHE = {}


def kernel(**inputs):
    x = np.asarray(inputs["x"], dtype=np.float32)[0]
    xTf = np.ascontiguousarray(x.T)
    sh = _prep_shared(inputs)
    halo = HALO_T * TT
    in_maps = []
    for c in range(NCORE):
        xc = np.zeros((D, halo + TOK_CORE), np.float32)
        xc[:, halo:] = xTf[:, c * TOK_CORE:(c + 1) * TOK_CORE]
        mbc = np.zeros((128, 20), np.float32)
        if c > 0:
            xc[:, :halo] = xTf[:, c * TOK_CORE - halo:c * TOK_CORE]
        else:
            mbc[:, 0:4] = -30000.0
        m = dict(sh)
        m["xT"] = xc
        m["mb"] = mbc
        in_maps.append(m)
    if "nc" not in _NC_CACHE:
        _NC_CACHE["nc"] = build_nc()
    res = run_bass_kernel_spmd(_NC_CACHE["nc"], in_maps, core_ids=list(range(NCORE)))
    out = np.empty((1, NCORE * TOK_CORE, D), np.float32)
    for c in range(NCORE):
        out[0, c * TOK_CORE:(c + 1) * TOK_CORE, :] = res.results[c]["yT"].T
    return out
```

```python
import numpy as np
from contextlib import ExitStack
import concourse.bass as bass
import concourse.mybir as mybir
from concourse.bass_utils import run_bass_kernel_spmd

F32 = mybir.dt.float32
BF16 = mybir.dt.bfloat16
U32 = mybir.dt.uint32
ALU = mybir.AluOpType
AF = mybir.ActivationFunctionType
AX = mybir.AxisListType

D = 2048
NCORE = 8
TOK_CORE = 2048
TT = 256
HALO_T = 2
NT_OWN = TOK_CORE // TT
EPS = 1e-6
NEXP_CH = 128
EG = 32
CG = 4


class Sched:
    ENG = ['pe', 'act', 'dve', 'pool', 'sp']
    EPOCH = 12000

    def __init__(self, nc, stack):
        self.nc = nc
        self.stack = stack
        self.streams = {e: [] for e in self.ENG}
        self.sem = {}
        self.cnt = {}
        self.nsem = 0
        for e in self.ENG:
            self._new_epoch(e)
        self.known = {e: {} for e in self.ENG}
        self.wr = {}
        self.rd = {}
        self.dma_sems = {}

    def _new_sem(self, name):
        self.nsem += 1
        return self.stack.enter_context(self.nc.semaphore(f"{name}_{self.nsem}"))

    def _new_epoch(self, e):
        self.sem[e] = self._new_sem(f"s_{e}")
        self.cnt[e] = 0

    def _deps(self, eng, reads, writes):
        deps = {}

        def need(ev, same_ok):
            sem, val, src = ev
            k = id(sem)
            if k not in deps or deps[k][1] < val:
                deps[k] = (sem, val)

        for (a, lo, hi) in reads:
            for (l2, h2, ev) in self.wr.get(a, ()):
                if l2 < hi and lo < h2:
                    need(ev, False)
        for (a, lo, hi) in writes:
            for (l2, h2, ev) in self.wr.get(a, ()):
                if l2 < hi and lo < h2:
                    need(ev, True)
            for (l2, h2, ev) in self.rd.get(a, ()):
                if l2 < hi and lo < h2:
                    need(ev, True)
        waits = []
        kn = self.known[eng]
        for k, (sem, val) in deps.items():
            if kn.get(k, 0) < val:
                kn[k] = val
                waits.append((sem, val))
        return waits

    def _commit(self, ev, reads, writes):
        for (a, lo, hi) in writes:
            self.wr[a] = [t for t in self.wr.get(a, ()) if not (t[0] >= lo and t[1] <= hi)]
            self.rd[a] = [t for t in self.rd.get(a, ()) if not (t[0] >= lo and t[1] <= hi)]
            self.wr[a].append((lo, hi, ev))
        for (a, lo, hi) in reads:
            lst = self.rd.setdefault(a, [])
            for i, t in enumerate(lst):
                if t[0] == lo and t[1] == hi and t[2][0] is ev[0]:
                    lst[i] = (lo, hi, ev)
                    break
            else:
                lst.append((lo, hi, ev))

    def op(self, eng, fn, reads=(), writes=()):
        reads = list(reads)
        writes = list(writes)
        waits = self._deps(eng, reads, writes)
        if self.cnt[eng] >= self.EPOCH:
            self._new_epoch(eng)
        self.cnt[eng] += 1
        mysem = self.sem[eng]
        ev = (mysem, self.cnt[eng], eng)

        def emit(e):
            for sem, val in waits:
                e.wait_ge(sem, val)
            ins = fn(e)
            ins.then_inc(mysem, 1)

        self.streams[eng].append(emit)
        self._commit(ev, reads, writes)
        return ev

    def dma(self, queue, out, in_, reads=(), writes=(), key=None):
        reads = list(reads)
        writes = list(writes)
        waits = self._deps(queue, reads, writes)
        if key not in self.dma_sems:
            self.dma_sems[key] = [self._new_sem("d"), 0]
        ent = self.dma_sems[key]
        ent[1] += 16
        sem = ent[0]
        ev = (sem, ent[1], None)

        def emit(e):
            for s, val in waits:
                e.wait_ge(s, val)
            e.dma_start(out=out, in_=in_).then_inc(sem, 16)

        self.streams[queue].append(emit)
        self._commit(ev, reads, writes)
        return ev

    def finish(self, eng='sp'):
        waits = [(ent[0], ent[1]) for ent in self.dma_sems.values()]
        for e in self.ENG:
            if e != eng and self.cnt[e] > 0:
                waits.append((self.sem[e], self.cnt[e]))

        def emit(e):
            for s, val in waits:
                e.wait_ge(s, val)

        self.streams[eng].append(emit)

    def emit_all(self):
        with self.nc.Block() as block:
            @block.tensor
            def _(e):
                for f in self.streams['pe']:
                    f(e)

            @block.scalar
            def _(e):
                for f in self.streams['act']:
                    f(e)

            @block.vector
            def _(e):
                for f in self.streams['dve']:
                    f(e)

            @block.gpsimd
            def _(e):
                for f in self.streams['pool']:
                    f(e)

            @block.sync
            def _(e):
                for f in self.streams['sp']:
                    f(e)


class Buf:
    def __init__(self, arena_name, ap, off, dt_bytes):
        self.an = arena_name
        self.ap = ap
        self.off = off
        self.eb = dt_bytes
        sh = ap.shape[1:]
        self.n = int(np.prod(sh))
        self.inner = int(np.prod(sh[1:])) if len(sh) > 1 else 1

    def k(self, i=None, j=None):
        if i is None:
            return (self.an, self.off, self.off + self.n * self.eb)
        if j is None:
            j = i + 1
        return (self.an, self.off + i * self.inner * self.eb, self.off + j * self.inner * self.eb)

    def ke(self, lo, hi):
        return (self.an, self.off + lo * self.eb, self.off + hi * self.eb)


def build_nc(nt_own=NT_OWN, debug=False):
    nc = bass.Bass("TRN2", target_bir_lowering=False)
    NTILE = HALO_T + nt_own
    NTOK = NTILE * TT
    xT = nc.dram_tensor("xT", [D, (HALO_T + NT_OWN) * TT], F32, kind="ExternalInput").ap()
    w_in = nc.dram_tensor("w_in", [D, 14336], F32, kind="ExternalInput").ap()
    w_co = nc.dram_tensor("w_co", [D, D], F32, kind="ExternalInput").ap()
    w_ao = nc.dram_tensor("w_ao", [D, D], F32, kind="ExternalInput").ap()
    w_out = nc.dram_tensor("w_out", [D, D], F32, kind="ExternalInput").ap()
    w_q = nc.dram_tensor("w_q", [D, D], F32, kind="ExternalInput").ap()
    uT = nc.dram_tensor("uT", [D, 16384], F32, kind="ExternalInput").ap()
    ev_d = nc.dram_tensor("ev", [16384, D], F32, kind="ExternalInput").ap()
    vecs_d = nc.dram_tensor("vecs", [128, 578], F32, kind="ExternalInput").ap()
    tb_d = nc.dram_tensor("tb", [128, 16, 640], F32, kind="ExternalInput").ap()
    mb_d = nc.dram_tensor("mb", [128, 20], F32, kind="ExternalInput").ap()
    k12_d = nc.dram_tensor("k12", [128, 256], F32, kind="ExternalInput").ap()
    yT = nc.dram_tensor("yT", [D, NT_OWN * TT], F32, kind="ExternalOutput").ap()
    NBLK = 28 + 16 + 64
    wsc = nc.dram_tensor("wsc", [NBLK, 128, 8192], BF16, kind="Internal").ap()
    dsc = nc.dram_tensor("dsc", [8, 128, 2 * 31 * 128], BF16, kind="Internal").ap()
    bsc = nc.dram_tensor("bsc", [16, 128, 640], BF16, kind="Internal").ap()
    if debug:
        dbg_mid = nc.dram_tensor("dbg_mid", [D, TT], F32, kind="ExternalOutput").ap()

    with ExitStack() as st:
        S = Sched(nc, st)

        def sb(name, shape, dt):
            t = st.enter_context(nc.sbuf_tensor("s_" + name, shape, dt))
            eb = 4 if dt in (F32, U32) else 2
            return Buf(name, t[:], 0, eb)

        xt = sb("xt", [128, 16, TT], F32)
        hT = sb("hT", [128, 16, TT], BF16)
        knT = sb("knT", [128, 16, 3, TT], BF16)
        vtok = sb("vtok", [128, 3, 2, D], BF16)
        Etab2 = [sb(f"Etab{i}", [128, 640], BF16) for i in range(4)]
        NWB = 3
        wbt = [sb(f"wb{i}", [128, 8192], BF16) for i in range(NWB)]
        ubuf = sb("ubuf", [128, 16, 30 + TT], BF16)
        vecs = sb("vecs", [128, 578], F32)
        mb = sb("mb", [128, 20], F32)
        k12 = sb("k12", [128, 256], BF16)
        ones_f = sb("ones_f", [128, 128], F32)
        ones_b = sb("ones_b", [128, 128], BF16)
        ident = sb("ident", [128, 128], F32)
        iota128 = sb("iota128", [128, 128], F32)
        iota16 = sb("iota16", [128, 16], F32)
        g1s = sb("g1s", [128, 16], F32)
        g2s = sb("g2s", [128, 16], F32)

        SCR_BYTES = 60 * 1024
        scr_t = st.enter_context(nc.sbuf_tensor("scr", [128, SCR_BYTES // 2], BF16))

        def scr(off, shape, dt):
            eb = 4 if dt in (F32, U32) else 2
            n = int(np.prod(shape))
            assert off % 4 == 0 and off + n * eb <= SCR_BYTES, (off, shape)
            ap = scr_t[:, off // 2: off // 2 + n * eb // 2]
            if dt != BF16:
                ap = ap.bitcast(dt)
            if len(shape) == 2:
                ap = ap.rearrange("p (a b) -> p a b", a=shape[0])
            elif len(shape) == 3:
                ap = ap.rearrange("p (a b c) -> p a b c", a=shape[0], b=shape[1])
            return Buf("scr", ap, off, eb)

        K = 1024
        ybuf = scr(0, [16, TT], F32)
        qnT = scr(0, [16, TT], BF16)
        attnT = scr(8 * K, [16, TT], BF16)
        su = scr(16 * K, [16, TT], BF16)
        merged = scr(16 * K, [16, TT], BF16)
        mc = scr(24 * K, [16, TT], BF16)
        PT = [scr(32 * K + i * K, [TT], F32) for i in range(2)]
        PT2 = [scr(34 * K + i * 3 * K, [6, TT], BF16) for i in range(2)]
        tmpf = [scr(40 * K + i * K, [TT], F32) for i in range(6)]
        sqt = [scr(46 * K + i * 4 * K, [4, TT], F32) for i in range(2)]
        etmp = [scr(54 * K + i * 2560, [640], F32) for i in range(2)]
        pqT = scr(0, [16, TT], BF16)
        sc = scr(8 * K, [16, 128], F32)
        Gsub = scr(16 * K, [EG, TT], BF16)
        Gsub2 = scr(0, [EG, TT], BF16)
        cand = scr(32 * K, [8, 256], F32)
        wk = [scr(40 * K + i * K, [256], F32) for i in range(2)]
        gel = [scr(42 * K + i * K, [TT], F32) for i in range(2)]
        v1 = scr(44 * K, [16, 16], F32)
        i1u = scr(45 * K, [16, 16], U32)
        i1f = scr(46 * K, [16, 16], F32)
        topv = scr(47 * K, [8, 16], F32)
        flatu = scr(47 * K + 512, [8, 16], U32)
        af = scr(48 * K, [8, 16], F32)
        bf_ = scr(48 * K + 512, [8, 16], F32)
        oh4 = scr(49 * K, [8, 16, 16], F32)
        e1f = scr(57 * K, [8, 16], F32)
        e2f = scr(57 * K + 512, [8, 16], F32)
        gat = scr(58 * K, [8, 16], F32)
        gsum = scr(58 * K + 512, [8], F32)
        au = scr(58 * K + 512 + 64, [8, 16], U32)
        bu = scr(59 * K + 64, [8, 16], U32)
        ytmp = [scr(54 * K + i * K, [TT], F32) for i in range(4)]
        Ism = sb("Ism", [128, TT], BF16)
        Jsm = sb("Jsm", [128, TT], BF16)
        iota128b = sb("iota128b", [128, 128], BF16)
        ident_b = sb("ident_b", [128, 128], BF16)
        gsm = sb("gsm", [128, TT], F32)
        OJb = [scr(32 * K + i * 4 * K, [16, 128], BF16) for i in range(2)]
        Cb = [scr(44 * K + i * K, [16, 32], BF16) for i in range(2)]
        Cf = [scr(46 * K + i * 2 * K, [16, 32], BF16) for i in range(2)]
        wTb = [scr(50 * K + i * 2 * K, [CG, TT], BF16) for i in range(2)]

        pst = [st.enter_context(nc.psum_tensor(f"ps{i}", [128, 512], F32)) for i in range(8)]

        class PBuf(Buf):
            def __init__(self, bank, ap):
                Buf.__init__(self, "ps", ap, bank * 2048, 4)
                self.bank = bank

            def k(self, i=None, j=None):
                return ("ps", self.bank * 2048, (self.bank + 1) * 2048)

        def psh(bank, half):
            return PBuf(bank, pst[bank][:, half * 256:half * 256 + 256])

        def psf(b):
            return PBuf(b, pst[b][:, :])

        class Rot:
            def __init__(self, items):
                self.items = items
                self.i = 0

            def next(self):
                r = self.items[self.i % len(self.items)]
                self.i += 1
                return r

        pj = Rot([psh(0, 0), psh(1, 0), psh(2, 0)])
        pstat = Rot([psh(3, 0), psh(3, 1)])
        pscore = Rot([psh(4, 0), psh(5, 0)])
        ppv = psh(6, 0)
        pden = psh(6, 1)
        pfull = Rot([psf(6), psf(7)])
        pfull2 = psf(5)
        ppy = Rot([psh(3, 0), psh(4, 0), psh(5, 0)])
        wrot = Rot(list(range(NWB)))

        V = lambda c0, n=1: vecs.ap[:, c0:c0 + n]
        N1G, N2G, DWB, LNG, LNB, QG, KG, DWW = 0, 16, 32, 48, 64, 80, 81, 82

        S.dma('sp', vecs.ap, vecs_d, writes=[vecs.k()], key='c0')
        S.dma('sp', mb.ap, mb_d, writes=[mb.k()], key='c1')
        S.dma('pool', k12.ap, k12_d, writes=[k12.k()], key='c2')
        S.op('dve', lambda e: e.memset(ones_f.ap, 1.0), writes=[ones_f.k()])
        S.op('dve', lambda e: e.memset(ones_b.ap, 1.0), writes=[ones_b.k()])
        S.op('dve', lambda e: e.memset(ubuf.ap, 0.0), writes=[ubuf.k()])
        S.op('pool', lambda e: e.iota(ident.ap, pattern=[[1, 128]], base=0, channel_multiplier=-1,
                                      allow_small_or_imprecise_dtypes=True), writes=[ident.k()])
        S.op('dve', lambda e: e.tensor_scalar(out=ident.ap, in0=ident.ap, scalar1=0.0, scalar2=None,
                                              op0=ALU.is_equal), reads=[ident.k()], writes=[ident.k()])
        S.op('dve', lambda e: e.tensor_copy(out=ident_b.ap, in_=ident.ap), reads=[ident.k()], writes=[ident_b.k()])
        S.op('pool', lambda e: e.iota(iota128.ap, pattern=[[1, 128]], base=0, channel_multiplier=0,
                                      allow_small_or_imprecise_dtypes=True), writes=[iota128.k()])
        S.op('dve', lambda e: e.tensor_copy(out=iota128b.ap, in_=iota128.ap), reads=[iota128.k()], writes=[iota128b.k()])
        S.op('pool', lambda e: e.iota(iota16.ap, pattern=[[1, 16]], base=0, channel_multiplier=0,
                                      allow_small_or_imprecise_dtypes=True), writes=[iota16.k()])
        S.op('dve', lambda e: e.tensor_scalar(out=g1s.ap, in0=V(N1G, 16), scalar1=float(np.sqrt(D)), scalar2=None,
                                              op0=ALU.mult), reads=[vecs.k()], writes=[g1s.k()])
        S.op('dve', lambda e: e.tensor_scalar(out=g2s.ap, in0=V(N2G, 16), scalar1=float(np.sqrt(D)), scalar2=None,
                                              op0=ALU.mult), reads=[vecs.k()], writes=[g2s.k()])
        blk_id = {}
        NCVSEM = 56
        CV_AHEAD = 4
        CV_LOOK = 8

        conv_list = []
        conv_done = [0]

        def conv_blk(name, src_ap, j, pattern):
            b = len(blk_id)
            blk_id[(name, j)] = b
            conv_list.append((src_ap, pattern))

        def ensure_conv(upto):
            while conv_done[0] < min(upto, len(conv_list)):
                b = conv_done[0]
                src_ap, pattern = conv_list[b]
                c = 16 if pattern == 'kc' else 4
                S.dma('pool', wsc[b].rearrange("p (c n) -> p c n", c=c), src_ap.rearrange("(c p) n -> p c n", p=128),
                      reads=([('wsc', b - CV_AHEAD, b - CV_AHEAD + 1)] if b >= CV_AHEAD else []),
                      writes=[('wsc', b, b + 1), ('cvslot', b % NCVSEM, b % NCVSEM + 1)], key=('cv', b % NCVSEM))
                conv_done[0] += 1

        def conv_in(j0):
            for j in range(4):
                conv_blk('w_in', w_in[:, j0 + 512 * j:j0 + 512 * (j + 1)], (j0 // 512) + j, 'kc')

        conv_in(6144)
        conv_in(8192)
        for j in range(4):
            conv_blk('w_in', w_in[:, 512 * j:512 * (j + 1)], j, 'kc')
            conv_blk('w_in', w_in[:, 2048 + 512 * j:2048 + 512 * (j + 1)], 4 + j, 'kc')
        for j in range(4):
            conv_blk('w_co', w_co[:, 512 * j:512 * (j + 1)], j, 'kc')
            conv_blk('w_in', w_in[:, 10240 + 512 * j:10240 + 512 * (j + 1)], 20 + j, 'kc')
        conv_in(4096)
        for j in range(4):
            conv_blk('w_ao', w_ao[:, 512 * j:512 * (j + 1)], j, 'kc')
            conv_blk('w_in', w_in[:, 12288 + 512 * j:12288 + 512 * (j + 1)], 24 + j, 'kc')
        for j in range(4):
            conv_blk('w_out', w_out[:, 512 * j:512 * (j + 1)], j, 'kc')
        for j in range(4):
            conv_blk('w_q', w_q[:, 512 * j:512 * (j + 1)], j, 'kc')
        for j in range(NEXP_CH // CG):
            conv_blk('uT', uT[:, 512 * j:512 * (j + 1)], j, 'kc')
            conv_blk('ev', ev_d[512 * j:512 * (j + 1), :], j, 'ec')
        assert len(blk_id) == NBLK

        def load_w(name, j, rows_pattern, wi=None):
            if wi is None:
                wi = wrot.next()
            wb = wbt[wi]
            b = blk_id[(name, j)]
            ensure_conv(b + 1 + CV_LOOK)
            c = 16 if rows_pattern == 'kc' else 4
            view = Buf(wb.an, wb.ap.rearrange("p (c n) -> p c n", c=c), 0, 2)
            S.dma('sp', wb.ap, wsc[b], reads=[('wsc', b, b + 1)], writes=[wb.k()], key=('wb', wi))
            return view

        def proj(wv, sub, rhsbuf, ps):
            def f(e):
                for c in range(16):
                    ins = e.matmul(ps.ap, lhsT=wv.ap[:, c, sub * 128:(sub + 1) * 128], rhs=rhsbuf.ap[:, c, :],
                                   start=(c == 0), stop=(c == 15))
                return ins
            S.op('pe', f, reads=[wv.k(), rhsbuf.k()], writes=[ps.k()])

        def rsqrt_chain(dst, src_ap, src_key, addc):
            S.op('dve', lambda e: e.tensor_scalar(out=dst.ap, in0=src_ap, scalar1=float(addc), scalar2=None, op0=ALU.add),
                 reads=[src_key], writes=[dst.k()])
            S.op('act', lambda e: e.activation(out=dst.ap, in_=dst.ap, func=AF.Sqrt), reads=[dst.k()], writes=[dst.k()])
            S.op('dve', lambda e: e.reciprocal(out=dst.ap, in_=dst.ap), reads=[dst.k()], writes=[dst.k()])

        def rmsnorm_to_hT(gs):
            pss = pstat.next()
            for q4 in range(4):
                sq = sqt[q4 % 2]
                S.op('act', lambda e, q4=q4, sq=sq: e.activation(out=sq.ap, in_=xt.ap[:, 4 * q4:4 * q4 + 4, :], func=AF.Square),
                     reads=[xt.k(4 * q4, 4 * q4 + 4)], writes=[sq.k()])

                def f(e, q4=q4, sq=sq):
                    for j in range(4):
                        ins = e.matmul(pss.ap, lhsT=ones_f.ap, rhs=sq.ap[:, j, :], start=(q4 == 0 and j == 0),
                                       stop=(q4 == 3 and j == 3))
                    return ins
                S.op('pe', f, reads=[sq.k(), ones_f.k()], writes=[pss.k()])
            rs = tmpf[0]
            rsqrt_chain(rs, pss.ap, pss.k(), D * EPS)
            for c in range(16):
                S.op('dve', lambda e, c=c: e.scalar_tensor_tensor(out=hT.ap[:, c, :], in0=xt.ap[:, c, :], scalar=gs.ap[:, c:c + 1],
                                                                 in1=rs.ap, op0=ALU.mult, op1=ALU.mult),
                     reads=[xt.k(c), rs.k(), gs.k()], writes=[hT.k(c)])

        def qk_norm(ps, gcol, out_ap, out_key):
            sq = tmpf[4]
            S.op('act', lambda e: e.activation(out=sq.ap, in_=ps.ap, func=AF.Square), reads=[ps.k()], writes=[sq.k()])
            pss = pstat.next()
            S.op('pe', lambda e: e.matmul(pss.ap, lhsT=ones_f.ap, rhs=sq.ap, start=True, stop=True),
                 reads=[sq.k(), ones_f.k()], writes=[pss.k()])
            rs = tmpf[5]
            rsqrt_chain(rs, pss.ap, pss.k(), 128 * EPS)
            S.op('dve', lambda e: e.scalar_tensor_tensor(out=out_ap, in0=ps.ap, scalar=V(gcol), in1=rs.ap,
                                                         op0=ALU.mult, op1=ALU.mult),
                 reads=[ps.k(), rs.k(), vecs.k()], writes=[out_key])

        for h in range(16):
            et = etmp[h % 2]
            Eh = Etab2[h % 2]
            S.dma('sp', et.ap, tb_d[:, h, :], writes=[et.k()], key=('et', h % 2))
            S.op('act', lambda e, et=et, Eh=Eh: e.activation(out=Eh.ap, in_=et.ap, func=AF.Identity, scale=float(1.0 / np.sqrt(128.0))),
                 reads=[et.k()], writes=[Eh.k()])
            S.op('dve', lambda e, Eh=Eh: e.memset(Eh.ap[0:64, 576:640], -3000.0), writes=[Eh.k()])
            S.op('dve', lambda e, Eh=Eh: e.memset(Eh.ap[64:128, 0:64], -3000.0), writes=[Eh.k()])
            S.dma('sp', bsc[h], Eh.ap, reads=[Eh.k()], writes=[('bsc', h, h + 1)], key=('bst', h % 2))

        ND = 2 * 31 * 128
        for p2 in range(8):
            wi = wrot.next()
            stg = Buf(wbt[wi].an, wbt[wi].ap[:, 0:ND].rearrange("p (i j m) -> p i j m", i=2, j=31), 0, 2)
            for i in range(2):
                c = 2 * p2 + i
                for j in range(31):
                    S.op('dve', lambda e, stg=stg, i=i, j=j, c=c: e.tensor_scalar(out=stg.ap[:, i, j, :], in0=ident.ap, scalar1=V(DWW + c * 31 + j),
                                                                                 scalar2=None, op0=ALU.mult),
                         reads=[ident.k(), vecs.k()], writes=[stg.ke((i * 31 + j) * 128, (i * 31 + j + 1) * 128)])
            S.dma('sp', dsc[p2], wbt[wi].ap[:, 0:ND], reads=[stg.k()], writes=[('dsc', p2, p2 + 1)], key=('dst', p2))

        def do_tile(ti):
            own = ti >= HALO_T
            slot = ti % 3
            S.dma('sp', xt.ap, xT[:, ti * TT:(ti + 1) * TT].rearrange("(c p) t -> p c t", p=128),
                  writes=[xt.k()], key='xt')
            rmsnorm_to_hT(g1s)

            if ti >= 1:
                for j in range(4):
                    wa = load_w('w_in', j, 'kc')
                    wg = load_w('w_in', 4 + j, 'kc')
                    for sub in range(4):
                        c = 4 * j + sub
                        pa = pj.next()
                        pg = pj.next()
                        proj(wa, sub, hT, pa)
                        proj(wg, sub, hT, pg)
                        sg = tmpf[3 + (c % 2)]
                        S.op('act', lambda e, sg=sg, pg=pg: e.activation(out=sg.ap, in_=pg.ap, func=AF.Sigmoid),
                             reads=[pg.k()], writes=[sg.k()])
                        S.op('dve', lambda e, c=c, sg=sg, pa=pa: e.tensor_tensor(out=ubuf.ap[:, c, 30:30 + TT], in0=pa.ap, in1=sg.ap, op=ALU.mult),
                             reads=[pa.k(), sg.k()], writes=[ubuf.k(c)])
            if own:
                for p2 in range(8):
                    wi = wrot.next()
                    S.dma('sp', wbt[wi].ap[:, 0:ND], dsc[p2], reads=[('dsc', p2, p2 + 1)], writes=[wbt[wi].k()], key=('wb', wi))
                    dg = Buf(wbt[wi].an, wbt[wi].ap[:, 0:ND].rearrange("p (i j m) -> p i j m", i=2, j=31), 0, 2)
                    for i in range(2):
                        c = 2 * p2 + i
                        ps = pj.next()

                        def f(e, dg=dg, i=i, c=c, ps=ps):
                            for j in range(31):
                                ins = e.matmul(ps.ap, lhsT=dg.ap[:, i, j, :], rhs=ubuf.ap[:, c, j:j + TT], start=(j == 0), stop=(j == 30))
                            return ins
                        S.op('pe', f, reads=[wbt[wi].k(), ubuf.k(c)], writes=[ps.k()])
                        S.op('act', lambda e, c=c, ps=ps: e.activation(out=ybuf.ap[:, c, :], in_=ps.ap, func=AF.Identity, bias=V(DWB + c)),
                             reads=[ps.k(), vecs.k()], writes=[ybuf.k(c)])

            def conv_emit(n):
                pass

            def do_kv():
                for j in range(4):
                    wv = load_w('w_in', 12 + j, 'kc')
                    for sub in range(4):
                        h = 4 * j + sub
                        ps = pj.next()
                        proj(wv, sub, hT, ps)
                        qk_norm(ps, KG, knT.ap[:, h, slot, :], knT.ke((h * 3 + slot) * TT, (h * 3 + slot + 1) * TT))
                        conv_emit(24)
                for j in range(4):
                    wv = load_w('w_in', 16 + j, 'kc')
                    for t2 in range(2):
                        pf = pfull.next()

                        def f(e, wv=wv, t2=t2, pf=pf):
                            for c in range(16):
                                ins = e.matmul(pf.ap, lhsT=hT.ap[:, c, t2 * 128:(t2 + 1) * 128], rhs=wv.ap[:, c, :],
                                               start=(c == 0), stop=(c == 15))
                            return ins
                        S.op('pe', f, reads=[wv.k(), hT.k()], writes=[pf.k()])
                        S.op('act', lambda e, t2=t2, j=j, pf=pf: e.activation(out=vtok.ap[:, slot, t2, 512 * j:512 * (j + 1)], in_=pf.ap, func=AF.Copy),
                             reads=[pf.k()], writes=[vtok.ke((slot * 2 + t2) * D + 512 * j, (slot * 2 + t2) * D + 512 * (j + 1))])
                        conv_emit(14)

            if not own:
                do_kv()
                if ti >= 1:
                    S.op('pool', lambda e: e.tensor_copy(out=ubuf.ap[:, :, 0:30], in_=ubuf.ap[:, :, TT:TT + 30]),
                         reads=[ubuf.k()], writes=[ubuf.k()])
                return
            S.op('pool', lambda e: e.tensor_copy(out=ubuf.ap[:, :, 0:30], in_=ubuf.ap[:, :, TT:TT + 30]),
                 reads=[ubuf.k()], writes=[ubuf.k()])
            ps1 = pstat.next()
            ps2 = pstat.next()

            def f(e):
                for c in range(16):
                    ins = e.matmul(ps1.ap, lhsT=ones_f.ap, rhs=ybuf.ap[:, c, :], start=(c == 0), stop=(c == 15))
                return ins
            S.op('pe', f, reads=[ybuf.k(), ones_f.k()], writes=[ps1.k()])
            for q4 in range(4):
                sq = sqt[q4 % 2]
                S.op('act', lambda e, q4=q4, sq=sq: e.activation(out=sq.ap, in_=ybuf.ap[:, 4 * q4:4 * q4 + 4, :], func=AF.Square),
                     reads=[ybuf.k(4 * q4, 4 * q4 + 4)], writes=[sq.k()])

                def f(e, q4=q4, sq=sq):
                    for j in range(4):
                        ins = e.matmul(ps2.ap, lhsT=ones_f.ap, rhs=sq.ap[:, j, :], start=(q4 == 0 and j == 0),
                                       stop=(q4 == 3 and j == 3))
                    return ins
                S.op('pe', f, reads=[sq.k(), ones_f.k()], writes=[ps2.k()])
            mean, msq, rstd, nmr = tmpf[0], tmpf[1], tmpf[2], tmpf[3]
            S.op('dve', lambda e: e.tensor_scalar(out=mean.ap, in0=ps1.ap, scalar1=1.0 / D, scalar2=None, op0=ALU.mult),
                 reads=[ps1.k()], writes=[mean.k()])
            S.op('dve', lambda e: e.tensor_tensor(out=msq.ap, in0=mean.ap, in1=mean.ap, op=ALU.mult),
                 reads=[mean.k()], writes=[msq.k()])
            S.op('dve', lambda e: e.scalar_tensor_tensor(out=rstd.ap, in0=ps2.ap, scalar=1.0 / D, in1=msq.ap,
                                                         op0=ALU.mult, op1=ALU.subtract),
                 reads=[ps2.k(), msq.k()], writes=[rstd.k()])
            rsqrt_chain(rstd, rstd.ap, rstd.k(), EPS)
            S.op('dve', lambda e: e.scalar_tensor_tensor(out=nmr.ap, in0=mean.ap, scalar=-1.0, in1=rstd.ap,
                                                         op0=ALU.mult, op1=ALU.mult),
                 reads=[mean.k(), rstd.k()], writes=[nmr.k()])
            for c in range(16):
                S.op('pool', lambda e, c=c: e.tensor_tensor(out=ybuf.ap[:, c, :], in0=ybuf.ap[:, c, :], in1=rstd.ap, op=ALU.mult),
                     reads=[ybuf.k(c), rstd.k()], writes=[ybuf.k(c)])
            for c in range(16):
                S.op('pool', lambda e, c=c: e.tensor_tensor(out=ybuf.ap[:, c, :], in0=ybuf.ap[:, c, :], in1=nmr.ap, op=ALU.add),
                     reads=[ybuf.k(c), nmr.k()], writes=[ybuf.k(c)])
            for c in range(16):
                S.op('act', lambda e, c=c: e.activation(out=su.ap[:, c, :], in_=ybuf.ap[:, c, :], func=AF.Silu,
                                                        bias=V(LNB + c), scale=V(LNG + c)),
                     reads=[ybuf.k(c), vecs.k()], writes=[su.k(c)])
            do_kv()
            for j in range(4):
                wc = load_w('w_co', j, 'kc')
                wg = load_w('w_in', 20 + j, 'kc')
                for sub in range(4):
                    dc = 4 * j + sub
                    pc = pj.next()
                    pg = pj.next()
                    proj(wc, sub, su, pc)
                    proj(wg, sub, hT, pg)
                    sg = tmpf[4 + (dc % 2)]
                    S.op('act', lambda e, sg=sg, pg=pg: e.activation(out=sg.ap, in_=pg.ap, func=AF.Sigmoid),
                         reads=[pg.k()], writes=[sg.k()])
                    S.op('dve', lambda e, dc=dc, sg=sg, pc=pc: e.tensor_tensor(out=mc.ap[:, dc, :], in0=pc.ap, in1=sg.ap, op=ALU.mult),
                         reads=[pc.k(), sg.k()], writes=[mc.k(dc)])
            for j in range(4):
                wv = load_w('w_in', 8 + j, 'kc')
                for sub in range(4):
                    h = 4 * j + sub
                    ps = pj.next()
                    proj(wv, sub, hT, ps)
                    qk_norm(ps, QG, qnT.ap[:, h, :], qnT.k(h))
            RQ = [(0, 128, 512), (0, 256, 384), (0, 256, 256), (0, 256, 128), (0, 256, 0), (128, 128, 0)]
            for h in range(16):
                pt2 = PT2[h % 2]
                Eh = Etab2[h % 4]
                S.dma('sp', Eh.ap, bsc[h], reads=[('bsc', h, h + 1)], writes=[Eh.k()], key=('eb', h % 4))
                for r in range(6):
                    q0, nq, qrel0 = RQ[r]
                    tsrc = ti - 2 + r // 2
                    ksl = tsrc % 3
                    kh = r % 2
                    gidx = 2 * (ti - 2) + r
                    ps = pscore.next()
                    kkey = knT.ke((h * 3 + ksl) * TT + kh * 128, (h * 3 + ksl) * TT + kh * 128 + 128)
                    def fs(e, ps=ps, h=h, ksl=ksl, kh=kh, q0=q0, nq=nq, qrel0=qrel0, Eh=Eh):
                        e.matmul(ps.ap[:, 0:nq], lhsT=knT.ap[:, h, ksl, kh * 128:(kh + 1) * 128], rhs=qnT.ap[:, h, q0:q0 + nq],
                                 start=True, stop=False)
                        return e.matmul(ps.ap[:, 0:nq], lhsT=ident_b.ap, rhs=Eh.ap[:, qrel0:qrel0 + nq], start=False, stop=True)
                    S.op('pe', fs, reads=[kkey, qnT.k(h), Eh.k(), ident_b.k()], writes=[ps.k()])
                    S.op('act', lambda e, ps=ps, pt2=pt2, r=r, q0=q0, nq=nq, gidx=gidx: e.activation(
                        out=pt2.ap[:, r, q0:q0 + nq], in_=ps.ap[:, 0:nq], func=AF.Exp, bias=mb.ap[:, gidx:gidx + 1],
                        scale=float(np.sqrt(128.0))), reads=[ps.k(), mb.k()], writes=[pt2.k(r)])

                def f(e, h=h, pt2=pt2):
                    for b in range(2):
                        for r in range(b, b + 5):
                            tsrc = ti - 2 + r // 2
                            e.matmul(ppv.ap[:, b * 128:(b + 1) * 128], lhsT=vtok.ap[:, tsrc % 3, r % 2, h * 128:(h + 1) * 128],
                                     rhs=pt2.ap[:, r, b * 128:(b + 1) * 128], start=(r == b), stop=(r == b + 4))
                    for b in range(2):
                        for r in range(b, b + 5):
                            ins = e.matmul(pden.ap[:, b * 128:(b + 1) * 128], lhsT=ones_b.ap,
                                           rhs=pt2.ap[:, r, b * 128:(b + 1) * 128], start=(r == b), stop=(r == b + 4))
                    return ins
                S.op('pe', f, reads=[vtok.k(), pt2.k(), ones_b.k()], writes=[ppv.k(), pden.k()])
                rec = tmpf[0]
                S.op('dve', lambda e: e.reciprocal(out=rec.ap, in_=pden.ap), reads=[pden.k()], writes=[rec.k()])
                S.op('dve', lambda e, h=h: e.tensor_tensor(out=attnT.ap[:, h, :], in0=ppv.ap, in1=rec.ap, op=ALU.mult),
                     reads=[ppv.k(), rec.k()], writes=[attnT.k(h)])
            for j in range(4):
                wa = load_w('w_ao', j, 'kc')
                wg = load_w('w_in', 24 + j, 'kc')
                for sub in range(4):
                    dc = 4 * j + sub
                    pa = pj.next()
                    pg = pj.next()
                    proj(wa, sub, attnT, pa)
                    proj(wg, sub, hT, pg)
                    sg = tmpf[4 + (dc % 2)]
                    tt_ = tmpf[2 + (dc % 2)]
                    S.op('act', lambda e, sg=sg, pg=pg: e.activation(out=sg.ap, in_=pg.ap, func=AF.Sigmoid),
                         reads=[pg.k()], writes=[sg.k()])
                    S.op('dve', lambda e, tt_=tt_, sg=sg, pa=pa: e.tensor_tensor(out=tt_.ap, in0=pa.ap, in1=sg.ap, op=ALU.mult),
                         reads=[pa.k(), sg.k()], writes=[tt_.k()])
                    S.op('pool', lambda e, dc=dc, tt_=tt_: e.tensor_tensor(out=merged.ap[:, dc, :], in0=tt_.ap, in1=mc.ap[:, dc, :], op=ALU.add),
                         reads=[tt_.k(), mc.k(dc)], writes=[merged.k(dc)])
            for j in range(4):
                wo = load_w('w_out', j, 'kc')
                for sub in range(4):
                    dc = 4 * j + sub
                    po = pj.next()
                    proj(wo, sub, merged, po)
                    S.op('dve', lambda e, dc=dc, po=po: e.tensor_tensor(out=xt.ap[:, dc, :], in0=po.ap, in1=xt.ap[:, dc, :], op=ALU.add),
                         reads=[po.k(), xt.k(dc)], writes=[xt.k(dc)])
            if debug and ti == HALO_T:
                S.dma('sp', dbg_mid.rearrange("(c p) t -> p c t", p=128), xt.ap, reads=[xt.k()], key='dbg')

            rmsnorm_to_hT(g2s)
            for j in range(4):
                wq = load_w('w_q', j, 'kc')
                for sub in range(4):
                    cc = 4 * j + sub
                    pq = pj.next()
                    proj(wq, sub, hT, pq)
                    S.op('act', lambda e, cc=cc, pq=pq: e.activation(out=pqT.ap[:, cc, :], in_=pq.ap, func=AF.Copy),
                         reads=[pq.k()], writes=[pqT.k(cc)])
            v1v = v1.ap.rearrange("p (h two) k -> p h two k", two=2)
            i1v = i1f.ap.rearrange("p (h two) k -> p h two k", two=2)
            for t2 in range(2):
                for g4 in range(4):
                    pf = pfull.next()

                    def f(e, g4=g4, pf=pf, t2=t2):
                        for i in range(4):
                            cc = 4 * g4 + i
                            ins = e.matmul(pf.ap[:, i * 128:(i + 1) * 128], lhsT=pqT.ap[:, cc, t2 * 128:(t2 + 1) * 128],
                                           rhs=k12.ap[:, (cc % 2) * 128:(cc % 2) * 128 + 128], start=True, stop=True)
                        return ins
                    S.op('pe', f, reads=[pqT.k(4 * g4, 4 * g4 + 4), k12.k()], writes=[pf.k()])
                    S.op('act', lambda e, g4=g4, pf=pf: e.activation(out=sc.ap[:, 4 * g4:4 * g4 + 4, :],
                                                                     in_=pf.ap.rearrange("p (a b) -> p a b", a=4), func=AF.Copy),
                         reads=[pf.k()], writes=[sc.k(4 * g4, 4 * g4 + 4)])
                for cc in range(16):
                    S.op('dve', lambda e, cc=cc: e.max(out=v1.ap[:, cc, 0:8], in_=sc.ap[:, cc, :]),
                         reads=[sc.k(cc)], writes=[v1.ke(cc * 16, cc * 16 + 8)])
                for cc in range(16):
                    S.op('dve', lambda e, cc=cc: e.max_index(out=i1u.ap[:, cc, 0:8], in_max=v1.ap[:, cc, 0:8], in_values=sc.ap[:, cc, :]),
                         reads=[sc.k(cc), v1.ke(cc * 16, cc * 16 + 8)], writes=[i1u.ke(cc * 16, cc * 16 + 8)])
                for cc in range(16):
                    S.op('dve', lambda e, cc=cc: e.match_replace(out=sc.ap[:, cc, :], in_to_replace=v1.ap[:, cc, 0:8],
                                                                 in_values=sc.ap[:, cc, :], imm_value=-1e30),
                         reads=[sc.k(cc), v1.ke(cc * 16, cc * 16 + 8)], writes=[sc.k(cc)])
                for cc in range(16):
                    S.op('dve', lambda e, cc=cc: e.max(out=v1.ap[:, cc, 8:16], in_=sc.ap[:, cc, :]),
                         reads=[sc.k(cc)], writes=[v1.ke(cc * 16 + 8, cc * 16 + 16)])
                for cc in range(16):
                    S.op('dve', lambda e, cc=cc: e.max_index(out=i1u.ap[:, cc, 8:16], in_max=v1.ap[:, cc, 8:16], in_values=sc.ap[:, cc, :]),
                         reads=[sc.k(cc), v1.ke(cc * 16 + 8, cc * 16 + 16)], writes=[i1u.ke(cc * 16 + 8, cc * 16 + 16)])
                S.op('dve', lambda e: e.tensor_copy(out=i1f.ap, in_=i1u.ap), reads=[i1u.k()], writes=[i1f.k()])
                cand4 = cand.ap.rearrange("p h (a b) -> p h a b", a=16)
                S.op('dve', lambda e: e.tensor_tensor(out=cand4, in0=v1v[:, :, 0, :].unsqueeze(3).to_broadcast([128, 8, 16, 16]),
                                                      in1=v1v[:, :, 1, :].unsqueeze(2).to_broadcast([128, 8, 16, 16]), op=ALU.add),
                     reads=[v1.k()], writes=[cand.k()])
                for h in range(8):
                    S.op('dve', lambda e, h=h: e.max(out=topv.ap[:, h, 0:8], in_=cand.ap[:, h, :]),
                         reads=[cand.k(h)], writes=[topv.ke(h * 16, h * 16 + 8)])
                for h in range(8):
                    S.op('dve', lambda e, h=h: e.max_index(out=flatu.ap[:, h, 0:8], in_max=topv.ap[:, h, 0:8], in_values=cand.ap[:, h, :]),
                         reads=[cand.k(h), topv.ke(h * 16, h * 16 + 8)], writes=[flatu.ke(h * 16, h * 16 + 8)])
                for h in range(8):
                    S.op('dve', lambda e, h=h: e.match_replace(out=cand.ap[:, h, :], in_to_replace=topv.ap[:, h, 0:8],
                                                               in_values=cand.ap[:, h, :], imm_value=-1e30),
                         reads=[cand.k(h), topv.ke(h * 16, h * 16 + 8)], writes=[cand.k(h)])
                for h in range(8):
                    S.op('dve', lambda e, h=h: e.max(out=topv.ap[:, h, 8:16], in_=cand.ap[:, h, :]),
                         reads=[cand.k(h)], writes=[topv.ke(h * 16 + 8, h * 16 + 16)])
                for h in range(8):
                    S.op('dve', lambda e, h=h: e.max_index(out=flatu.ap[:, h, 8:16], in_max=topv.ap[:, h, 8:16], in_values=cand.ap[:, h, :]),
                         reads=[cand.k(h), topv.ke(h * 16 + 8, h * 16 + 16)], writes=[flatu.ke(h * 16 + 8, h * 16 + 16)])
                S.op('dve', lambda e: e.tensor_single_scalar(out=au.ap, in_=flatu.ap, scalar=4, op=ALU.logical_shift_right),
                     reads=[flatu.k()], writes=[au.k()])
                S.op('dve', lambda e: e.tensor_single_scalar(out=bu.ap, in_=flatu.ap, scalar=15, op=ALU.bitwise_and),
                     reads=[flatu.k()], writes=[bu.k()])
                S.op('dve', lambda e: e.tensor_copy(out=af.ap, in_=au.ap), reads=[au.k()], writes=[af.k()])
                S.op('dve', lambda e: e.tensor_copy(out=bf_.ap, in_=bu.ap), reads=[bu.k()], writes=[bf_.k()])
                io4 = iota16.ap.unsqueeze(1).unsqueeze(1).to_broadcast([128, 8, 16, 16])
                for (src, which, dst) in ((af, 0, e1f), (bf_, 1, e2f)):
                    S.op('dve', lambda e, src=src: e.tensor_tensor(out=oh4.ap, in0=src.ap.unsqueeze(3).to_broadcast([128, 8, 16, 16]),
                                                                   in1=io4, op=ALU.is_equal),
                         reads=[src.k(), iota16.k()], writes=[oh4.k()])
                    S.op('dve', lambda e, which=which: e.tensor_tensor(out=oh4.ap, in0=oh4.ap,
                                                                       in1=i1v[:, :, which, :].unsqueeze(2).to_broadcast([128, 8, 16, 16]),
                                                                       op=ALU.mult),
                         reads=[oh4.k(), i1f.k()], writes=[oh4.k()])
                    S.op('dve', lambda e, dst=dst: e.tensor_reduce(out=dst.ap, in_=oh4.ap, axis=AX.X, op=ALU.add),
                         reads=[oh4.k()], writes=[dst.k()])
                S.op('dve', lambda e: e.tensor_tensor(out=gat.ap, in0=topv.ap, in1=topv.ap[:, :, 0:1].to_broadcast([128, 8, 16]),
                                                      op=ALU.subtract), reads=[topv.k()], writes=[gat.k()])
                S.op('act', lambda e: e.activation(out=gat.ap, in_=gat.ap, func=AF.Exp), reads=[gat.k()], writes=[gat.k()])
                S.op('dve', lambda e: e.tensor_reduce(out=gsum.ap, in_=gat.ap, axis=AX.X, op=ALU.add),
                     reads=[gat.k()], writes=[gsum.k()])
                S.op('dve', lambda e: e.reciprocal(out=gsum.ap, in_=gsum.ap), reads=[gsum.k()], writes=[gsum.k()])
                S.op('dve', lambda e: e.tensor_tensor(out=gat.ap, in0=gat.ap, in1=gsum.ap.unsqueeze(2).to_broadcast([128, 8, 16]),
                                                      op=ALU.mult), reads=[gat.k(), gsum.k()], writes=[gat.k()])
                ptr = pfull2

                def f(e):
                    e.transpose(out=ptr.ap[:, 0:128], in_=e1f.ap.rearrange("p h k -> p (h k)"), identity=ident.ap)
                    e.transpose(out=ptr.ap[:, 128:256], in_=e2f.ap.rearrange("p h k -> p (h k)"), identity=ident.ap)
                    return e.transpose(out=ptr.ap[:, 256:384], in_=gat.ap.rearrange("p h k -> p (h k)"), identity=ident.ap)
                S.op('pe', f, reads=[e1f.k(), e2f.k(), gat.k(), ident.k()], writes=[ptr.k()])
                for i, dstb in enumerate((Ism, Jsm, gsm)):
                    S.op('act', lambda e, i=i, dstb=dstb, t2=t2: e.activation(out=dstb.ap[:, t2 * 128:(t2 + 1) * 128],
                                                                              in_=ptr.ap[:, i * 128:(i + 1) * 128], func=AF.Copy),
                         reads=[ptr.k()], writes=[dstb.ke(t2 * 128, (t2 + 1) * 128)])

            Gs = [Gsub, Gsub2]

            def gbuild_batch(eg, tb, G):
                oj, cb, cf = OJb[tb % 2], Cb[tb % 2], Cf[tb % 2]
                S.op('dve', lambda e: e.tensor_tensor(
                    out=oj.ap, in0=iota128b.ap.unsqueeze(1).to_broadcast([128, 16, 128]),
                    in1=Jsm.ap[:, tb * 16:(tb + 1) * 16].unsqueeze(2).to_broadcast([128, 16, 128]), op=ALU.is_equal),
                    reads=[iota128b.k(), Jsm.k()], writes=[oj.k()])
                S.op('dve', lambda e: e.tensor_tensor(
                    out=cf.ap, in0=iota128b.ap[:, eg * EG:(eg + 1) * EG].unsqueeze(1).to_broadcast([128, 16, EG]),
                    in1=Ism.ap[:, tb * 16:(tb + 1) * 16].unsqueeze(2).to_broadcast([128, 16, EG]), op=ALU.is_equal),
                    reads=[iota128b.k(), Ism.k()], writes=[cf.k()])
                S.op('pool', lambda e: e.tensor_tensor(
                    out=cb.ap, in0=cf.ap, in1=gsm.ap[:, tb * 16:(tb + 1) * 16].unsqueeze(2).to_broadcast([128, 16, EG]),
                    op=ALU.mult), reads=[cf.k(), gsm.k()], writes=[cb.k()])

                def mm():
                    pf = pfull.next()

                    def f(e):
                        for t in range(16):
                            ins = e.matmul(pf.ap[:, t * EG:(t + 1) * EG], lhsT=oj.ap[:, t, :], rhs=cb.ap[:, t, :],
                                           start=True, stop=True)
                        return ins
                    S.op('pe', f, reads=[oj.k(), cb.k()], writes=[pf.k()])
                    S.op('act', lambda e: e.activation(out=G.ap[:, :, tb * 16:(tb + 1) * 16],
                                                       in_=pf.ap.rearrange("p (t e) -> p e t", e=EG), func=AF.Copy),
                         reads=[pf.k()], writes=[G.k()])
                return mm

            NEG = NEXP_CH // EG
            NB = TT // 16
            NCGE = EG // CG
            NCG = NEG * NCGE

            def dense_A(i):
                eg, cg = divmod(i, NCGE)
                G = Gs[eg % 2]
                wu = load_w('uT', i, 'kc', wi=0)
                wvv = load_w('ev', i, 'ec', wi=1 + (i % 2))
                wt = wTb[i % 2]
                for cc in range(CG):
                    pa = pj.next()
                    proj(wu, cc, hT, pa)
                    ge = gel[cc % 2]
                    S.op('act', lambda e, ge=ge, pa=pa: e.activation(out=ge.ap, in_=pa.ap, func=AF.Gelu),
                         reads=[pa.k()], writes=[ge.k()])
                    S.op('dve', lambda e, ge=ge, cc=cc: e.tensor_tensor(
                        out=wt.ap[:, cc, :], in0=ge.ap, in1=G.ap[:, cg * CG + cc, :], op=ALU.mult),
                        reads=[ge.k(), G.k(cg * CG + cc)], writes=[wt.k(cc)])
                return wvv, wt

            def dense_Y(wvv, wt):
                for dc in range(16):
                    py = ppy.next()

                    def f(e, py=py, dc=dc):
                        for cc in range(CG):
                            ins = e.matmul(py.ap, lhsT=wvv.ap[:, cc, dc * 128:(dc + 1) * 128], rhs=wt.ap[:, cc, :],
                                           start=(cc == 0), stop=(cc == CG - 1))
                        return ins
                    S.op('pe', f, reads=[wvv.k(), wt.k()], writes=[py.k()])
                    if dc % 2 == 0:
                        S.op('dve', lambda e, dc=dc, py=py: e.tensor_tensor(out=xt.ap[:, dc, :], in0=py.ap, in1=xt.ap[:, dc, :], op=ALU.add),
                             reads=[py.k(), xt.k(dc)], writes=[xt.k(dc)])
                    else:
                        yt = ytmp[(dc // 2) % 4]
                        S.op('act', lambda e, yt=yt, py=py: e.activation(out=yt.ap, in_=py.ap, func=AF.Copy),
                             reads=[py.k()], writes=[yt.k()])
                        S.op('pool', lambda e, dc=dc, yt=yt: e.tensor_tensor(out=xt.ap[:, dc, :], in0=yt.ap, in1=xt.ap[:, dc, :], op=ALU.add),
                             reads=[yt.k(), xt.k(dc)], writes=[xt.k(dc)])

            for tb in range(NB):
                gbuild_batch(0, tb, Gs[0])()
            per = NB // NCGE
            cur = dense_A(0)
            for i in range(NCG):
                eg, cg = divmod(i, NCGE)
                mms = []
                if eg + 1 < NEG:
                    for tb in range(cg * per, (cg + 1) * per):
                        mms.append(gbuild_batch(eg + 1, tb, Gs[(eg + 1) % 2]))
                nxt = dense_A(i + 1) if (i + 1 < NCG and (i + 1) % NCGE != 0) else None
                for m in mms:
                    m()
                if i + 1 < NCG and (i + 1) % NCGE == 0:
                    nxt = dense_A(i + 1)
                dense_Y(*cur)
                cur = nxt
            S.dma('sp', yT[:, (ti - HALO_T) * TT:(ti - HALO_T + 1) * TT].rearrange("(c p) t -> p c t", p=128), xt.ap,
                  reads=[xt.k()], key='yst')
        for ti_ in range(NTILE):
            do_tile(ti_)
        S.finish('sp')
        S.emit_all()
    return nc


def _prep_shared(inputs):
    g = lambda k: np.asarray(inputs[k], dtype=np.float32)[0]
    sh = {}
    sh["w_in"] = np.ascontiguousarray(g("w_in"))
    sh["w_co"] = np.ascontiguousarray(g("w_conv_out"))
    sh["w_ao"] = np.ascontiguousarray(g("w_attn_o"))
    sh["w_out"] = np.ascontiguousarray(g("w_out"))
    sh["w_q"] = np.ascontiguousarray(g("w_query"))
    sh["uT"] = np.ascontiguousarray(g("expert_u").T)
    sh["ev"] = np.ascontiguousarray(g("expert_v"))
    fm = lambda v: v.reshape(16, 128).T
    vecs = np.zeros((128, 578), np.float32)
    vecs[:, 0:16] = fm(g("norm1_g"))
    vecs[:, 16:32] = fm(g("norm2_g"))
    vecs[:, 32:48] = fm(g("conv_dw_b"))
    vecs[:, 48:64] = fm(g("conv_ln_g"))
    vecs[:, 64:80] = fm(g("conv_ln_b"))
    vecs[:, 80] = g("q_norm_g")
    vecs[:, 81] = g("k_norm_g")
    dw = g("conv_dw_w")
    vecs[:, 82:] = dw.reshape(31, 16, 128).transpose(2, 1, 0).reshape(128, 16 * 31)
    sh["vecs"] = vecs
    kk = np.arange(128)[:, None]
    mm = np.arange(640)[None, :]
    idx = np.clip(mm - kk, -63, 128) + 63
    rb = g("rel_bias")
    sh["tb"] = np.ascontiguousarray(rb[:, idx].transpose(1, 0, 2))
    sh["k12"] = np.ascontiguousarray(np.concatenate([g("sub_keys_1").T, g("sub_keys_2").T], axis=1))
    return sh


_NC_CACHE = {}


def kernel(**inputs):
    x = np.asarray(inputs["x"], dtype=np.float32)[0]
    xTf = np.ascontiguousarray(x.T)
    sh = _prep_shared(inputs)
    halo = HALO_T * TT
    in_maps = []
    for c in range(NCORE):
        xc = np.zeros((D, halo + TOK_CORE), np.float32)
        xc[:, halo:] = xTf[:, c * TOK_CORE:(c + 1) * TOK_CORE]
        mbc = np.zeros((128, 20), np.float32)
        if c > 0:
            xc[:, :halo] = xTf[:, c * TOK_CORE - halo:c * TOK_CORE]
        else:
            mbc[:, 0:4] = -30000.0
        m = dict(sh)
        m["xT"] = xc
        m["mb"] = mbc
        in_maps.append(m)
    if "nc" not in _NC_CACHE:
        _NC_CACHE["nc"] = build_nc()
    res = run_bass_kernel_spmd(_NC_CACHE["nc"], in_maps, core_ids=list(range(NCORE)))
    out = np.empty((1, NCORE * TOK_CORE, D), np.float32)
    for c in range(NCORE):
        out[0, c * TOK_CORE:(c + 1) * TOK_CORE, :] = res.results[c]["yT"].T
    return out
```
